# Optimizing a Trainium2 kernel written in Bass

```python
import math
import jax
import jax.numpy as jnp
from jax import lax
import numpy as np

D_MODEL = 1024
BATCH = 2
SEQ = 16384
DEPTH = 4

CHUNK = 64
D_MIX = D_MODEL
GROUP_W = D_MIX // 4
NORM_EPS = 1e-6
MASK_VALUE = -1e30
GATE_FLOOR = 1e-20

A_KERNEL = 31
B_HEADS = 4
B_DK = GROUP_W // B_HEADS
B_DV = GROUP_W // B_HEADS
C_HEADS = 4
C_HEADDIM = GROUP_W // C_HEADS
C_GROUPS = 2
C_STATE = 128
C_CONV = 4
C_XBC = GROUP_W + 2 * C_GROUPS * C_STATE
D_HEADS = 4
D_Q_RANK = 256
D_KV_RANK = 128
D_NOPE = 64
D_ROPE = 32
D_VDIM = GROUP_W // D_HEADS
ROPE_THETA = 10000.0
Q_BLOCK = 128

IN_SPLITS = (GROUP_W, GROUP_W, GROUP_W,
             GROUP_W, GROUP_W, GROUP_W, GROUP_W,
             C_XBC, C_HEADS, GROUP_W,
             D_Q_RANK, D_KV_RANK, D_ROPE, GROUP_W)
N_IN = sum(IN_SPLITS)

kernel_name = 'hymba_style_streaming_hybrid_trunk'


def rms_norm(x, g, eps=NORM_EPS):
    xf = x.astype(jnp.float32)
    y = xf * lax.rsqrt(jnp.mean(jnp.square(xf), axis=-1, keepdims=True) + eps)
    return (y * g.astype(jnp.float32)).astype(x.dtype)


def layer_norm(x, g, b, eps=1e-5):
    xf = x.astype(jnp.float32)
    mu = jnp.mean(xf, axis=-1, keepdims=True)
    var = jnp.mean(jnp.square(xf - mu), axis=-1, keepdims=True)
    y = (xf - mu) * lax.rsqrt(var + eps)
    return (y * g.astype(jnp.float32) + b.astype(jnp.float32)).astype(x.dtype)


def masked_exp(mask, logd):
    return jnp.where(mask, jnp.exp(jnp.where(mask, logd, 0.0)), 0.0)


def causal_depthwise_conv(x, w, b):
    k, c = w.shape
    y = lax.conv_general_dilated(
        x, w[:, None, :].astype(x.dtype), window_strides=(1,), padding=[(k - 1, 0)],
        dimension_numbers=('NWC', 'WIO', 'NWC'), feature_group_count=c)
    return y + b.astype(x.dtype)


def apply_rotary(x, positions):
    r = x.shape[-1]
    inv_freq = ROPE_THETA ** (-jnp.arange(0, r, 2, dtype=jnp.float32) / r)
    ang = positions.astype(jnp.float32)[..., None] * inv_freq
    if x.ndim == 4:
        ang = ang[:, :, None, :]
    cos, sin = jnp.cos(ang), jnp.sin(ang)
    xf = x.astype(jnp.float32)
    x1, x2 = xf[..., : r // 2], xf[..., r // 2:]
    out = jnp.concatenate([x1 * cos - x2 * sin, x2 * cos + x1 * sin], axis=-1)
    return out.astype(x.dtype)


def conformer_conv_branch(val, glu_gate, gate, dw_w, dw_b, ln_g, ln_b, pw_w, pw_b):
    h = val * jax.nn.sigmoid(glu_gate)
    h = causal_depthwise_conv(h, dw_w, dw_b)
    h = jax.nn.silu(layer_norm(h, ln_g, ln_b))
    h = h @ pw_w + pw_b
    return h * jax.nn.silu(gate)


def hgrn2_branch(q, f_raw, v, gate, lb, norm_g):
    bsz, s, _ = q.shape
    nc = s // CHUNK
    f32 = jnp.float32
    zf = f_raw.astype(f32)
    lbf = lb.astype(f32)
    fgate = lbf + (1.0 - lbf) * jax.nn.sigmoid(zf)
    log_f = jnp.log(jnp.maximum(fgate, GATE_FLOOR))
    k = (1.0 - lbf) * jax.nn.sigmoid(-zf)

    def to_chunks(t, d):
        return t.astype(f32).reshape(bsz, nc, CHUNK, B_HEADS, d).transpose(1, 0, 3, 2, 4)

    qc, kc, gc = to_chunks(q, B_DK), to_chunks(k, B_DK), to_chunks(log_f, B_DK)
    vc = to_chunks(v, B_DV)
    causal = jnp.tril(jnp.ones((CHUNK, CHUNK), dtype=bool))[:, :, None]

    def step(state, inp):
        qt, kt, vt, gt = inp
        b = jnp.cumsum(gt, axis=2)
        o_inter = jnp.einsum('bhtk,bhkv->bhtv', qt * jnp.exp(b), state)
        diff = b[:, :, :, None, :] - b[:, :, None, :, :]
        decay = masked_exp(causal, diff)
        scores = jnp.einsum('bhtk,bhsk,bhtsk->bhts', qt, kt, decay)
        o_intra = jnp.einsum('bhts,bhsv->bhtv', scores, vt)
        b_last = b[:, :, -1:, :]
        new_state = (jnp.exp(b_last[:, :, 0, :])[..., None] * state
                     + jnp.einsum('bhsk,bhsv->bhkv', kt * jnp.exp(b_last - b), vt))
        return new_state, o_inter + o_intra

    state0 = jnp.zeros((bsz, B_HEADS, B_DK, B_DV), f32)
    _, o = lax.scan(step, state0, (qc, kc, vc, gc))
    o = o.transpose(1, 0, 3, 2, 4).reshape(bsz, s, B_HEADS, B_DV)
    o = rms_norm(o, norm_g.reshape(B_HEADS, B_DV)).astype(q.dtype)
    return o.reshape(bsz, s, B_HEADS * B_DV) * jax.nn.silu(gate)


def ssd_branch(xbc, dt_raw, z, conv_w, conv_b, dt_bias, a_log, d_skip, norm_g):
    bsz, s, _ = xbc.shape
    nc = s // CHUNK
    e = C_HEADS // C_GROUPS
    f32 = jnp.float32
    xbc = jax.nn.silu(causal_depthwise_conv(xbc, conv_w, conv_b))
    xs, bm, cm = jnp.split(xbc, [GROUP_W, GROUP_W + C_GROUPS * C_STATE], axis=-1)
    xs = xs.astype(f32).reshape(bsz, nc, CHUNK, C_GROUPS, e, C_HEADDIM)
    bm = bm.astype(f32).reshape(bsz, nc, CHUNK, C_GROUPS, C_STATE)
    cm = cm.astype(f32).reshape(bsz, nc, CHUNK, C_GROUPS, C_STATE)
    dt = jax.nn.softplus(dt_raw.astype(f32) + dt_bias.astype(f32))
    a = (dt * (-jnp.exp(a_log.astype(f32)))).reshape(bsz, nc, CHUNK, C_GROUPS, e)
    dt = dt.reshape(bsz, nc, CHUNK, C_GROUPS, e)
    a_cs = jnp.cumsum(a, axis=2)
    xdt = xs * dt[..., None]
    causal = jnp.tril(jnp.ones((CHUNK, CHUNK), dtype=bool))[:, :, None, None]
    seg = a_cs[:, :, :, None] - a_cs[:, :, None, :]
    lmat = masked_exp(causal, seg)
    cb = jnp.einsum('bclgn,bcsgn->bclsg', cm, bm)
    scores = cb[..., None] * lmat
    y_diag = jnp.einsum('bclsge,bcsgep->bclgep', scores, xdt)
    wx = xdt * jnp.exp(a_cs[:, :, -1:] - a_cs)[..., None]
    states = jnp.einsum('bclgn,bclgep->bcgepn', bm, wx)
    chunk_decay = jnp.exp(a_cs[:, :, -1])

    def step(h, inp):
        st, dc = inp
        return dc[..., None, None] * h + st, h

    h0 = jnp.zeros((bsz, C_GROUPS, e, C_HEADDIM, C_STATE), f32)
    _, prev = lax.scan(step, h0, (states.transpose(1, 0, 2, 3, 4, 5),
                                  chunk_decay.transpose(1, 0, 2, 3)))
    prev = prev.transpose(1, 0, 2, 3, 4, 5)
    y_off = jnp.einsum('bclgn,bcgepn->bclgep', cm, prev) * jnp.exp(a_cs)[..., None]
    y = y_diag + y_off + xs * d_skip.astype(f32).reshape(C_GROUPS, e)[..., None]
    y = y.reshape(bsz, s, C_GROUPS, e * C_HEADDIM)
    zg = jax.nn.silu(z.astype(f32)).reshape(bsz, s, C_GROUPS, e * C_HEADDIM)
    y = rms_norm(y * zg, norm_g.reshape(C_GROUPS, e * C_HEADDIM))
    return y.reshape(bsz, s, GROUP_W).astype(z.dtype)


def mla_branch(cq, ckv, k_rope, gate, positions, qa_g, qb_w, kva_g, kvb_w):
    bsz, s, _ = cq.shape
    q = (rms_norm(cq, qa_g) @ qb_w).reshape(bsz, s, D_HEADS, D_NOPE + D_ROPE)
    q = jnp.concatenate([q[..., :D_NOPE], apply_rotary(q[..., D_NOPE:], positions)], axis=-1)
    kv = (rms_norm(ckv, kva_g) @ kvb_w).reshape(bsz, s, D_HEADS, D_NOPE + D_VDIM)
    k_nope, v = kv[..., :D_NOPE], kv[..., D_NOPE:]
    k_r = apply_rotary(k_rope, positions)
    k = jnp.concatenate(
        [k_nope, jnp.broadcast_to(k_r[:, :, None, :], (bsz, s, D_HEADS, D_ROPE))], axis=-1)
    scale = (D_NOPE + D_ROPE) ** -0.5
    nqb = s // Q_BLOCK
    key_chunk = jnp.arange(s) // CHUNK
    q_blocks = q.reshape(bsz, nqb, Q_BLOCK, D_HEADS, D_NOPE + D_ROPE).transpose(1, 0, 2, 3, 4)
    q_chunk = key_chunk.reshape(nqb, Q_BLOCK)

    def attend(inp):
        qb, qch = inp
        sc = jnp.einsum('bqhd,bkhd->bhqk', qb, k).astype(jnp.float32) * scale
        mask = key_chunk[None, :] <= qch[:, None]
        p = jax.nn.softmax(jnp.where(mask, sc, MASK_VALUE), axis=-1).astype(v.dtype)
        return jnp.einsum('bhqk,bkhd->bqhd', p, v)

    o = lax.map(attend, (q_blocks, q_chunk))
    o = o.transpose(1, 0, 2, 3, 4).reshape(bsz, s, D_HEADS * D_VDIM)
    return o * jax.nn.silu(gate)


def setup_inputs(seed: int = 0) -> dict:
    key = jax.random.key(seed)
    ks = jax.random.split(key, 24)
    f32 = jnp.float32

    def nrm(k, shape, scale):
        return jax.random.normal(k, shape, f32) * scale

    def gain(k, shape):
        return 1.0 + 0.02 * jax.random.normal(k, shape, f32)

    x = nrm(ks[0], (BATCH, SEQ, D_MODEL), 1.0)
    offsets = jax.random.randint(ks[1], (BATCH, 1), 0, 4096, dtype=jnp.int32)
    positions = offsets + jnp.arange(SEQ, dtype=jnp.int32)[None, :]
    dt0 = jnp.exp(jax.random.uniform(ks[16], (DEPTH, C_HEADS), f32,
                                     math.log(1e-3), math.log(1e-1)))
    return {
        'x': x,
        'positions': positions,
        'pre_norm_g': gain(ks[2], (DEPTH, D_MODEL)),
        'post_norm_g': gain(ks[3], (DEPTH, D_MODEL)),
        'w_in': nrm(ks[4], (DEPTH, D_MODEL, N_IN), D_MODEL ** -0.5),
        'w_out': nrm(ks[5], (DEPTH, D_MIX, D_MODEL), D_MIX ** -0.5),
        'a_dw_w': nrm(ks[6], (DEPTH, A_KERNEL, GROUP_W), A_KERNEL ** -0.5),
        'a_dw_b': nrm(ks[7], (DEPTH, GROUP_W), 0.02),
        'a_ln_g': gain(ks[8], (DEPTH, GROUP_W)),
        'a_ln_b': nrm(ks[9], (DEPTH, GROUP_W), 0.02),
        'a_pw_w': nrm(ks[10], (DEPTH, GROUP_W, GROUP_W), GROUP_W ** -0.5),
        'a_pw_b': nrm(ks[11], (DEPTH, GROUP_W), 0.02),
        'b_lb_logits': 1.0 + nrm(ks[12], (DEPTH, B_HEADS * B_DK), 0.1),
        'b_norm_g': gain(ks[13], (DEPTH, B_HEADS * B_DV)),
        'c_conv_w': nrm(ks[14], (DEPTH, C_CONV, C_XBC), C_CONV ** -0.5),
        'c_conv_b': nrm(ks[15], (DEPTH, C_XBC), 0.02),
        'c_dt_bias': dt0 + jnp.log(-jnp.expm1(-dt0)),
        'c_a_log': jnp.log(jax.random.uniform(ks[17], (DEPTH, C_HEADS), f32, 1.0, 16.0)),
        'c_d': 1.0 + nrm(ks[18], (DEPTH, C_HEADS), 0.1),
        'c_norm_g': gain(ks[19], (DEPTH, GROUP_W)),
        'd_qa_g': gain(ks[20], (DEPTH, D_Q_RANK)),
        'd_qb_w': nrm(ks[21], (DEPTH, D_Q_RANK, D_HEADS * (D_NOPE + D_ROPE)), D_Q_RANK ** -0.5),
        'd_kva_g': gain(ks[22], (DEPTH, D_KV_RANK)),
        'd_kvb_w': nrm(ks[23], (DEPTH, D_KV_RANK, D_HEADS * (D_NOPE + D_VDIM)), D_KV_RANK ** -0.5),
    }


def reference(x, positions, pre_norm_g, post_norm_g, w_in, w_out,
              a_dw_w, a_dw_b, a_ln_g, a_ln_b, a_pw_w, a_pw_b,
              b_lb_logits, b_norm_g,
              c_conv_w, c_conv_b, c_dt_bias, c_a_log, c_d, c_norm_g,
              d_qa_g, d_qb_w, d_kva_g, d_kvb_w):
    lb_p = jax.nn.softmax(b_lb_logits.astype(jnp.float32), axis=0)
    lower_bounds = jnp.cumsum(lb_p, axis=0) - lb_p[0:1]
    split_at = [int(v) for v in np.cumsum(IN_SPLITS)[:-1]]
    for l in range(DEPTH):
        h = rms_norm(x, pre_norm_g[l])
        proj = h @ w_in[l]
        (a_val, a_glu, a_gate, b_q, b_f, b_i, b_gate,
         c_xbc, c_dt, c_z, d_cq, d_ckv, d_kr, d_gate) = jnp.split(proj, split_at, axis=-1)
        y_a = conformer_conv_branch(a_val, a_glu, a_gate, a_dw_w[l], a_dw_b[l],
                                    a_ln_g[l], a_ln_b[l], a_pw_w[l], a_pw_b[l])
        y_b = hgrn2_branch(b_q, b_f, b_i, b_gate, lower_bounds[l], b_norm_g[l])
        y_c = ssd_branch(c_xbc, c_dt, c_z, c_conv_w[l], c_conv_b[l], c_dt_bias[l],
                         c_a_log[l], c_d[l], c_norm_g[l])
        y_d = mla_branch(d_cq, d_ckv, d_kr, d_gate, positions, d_qa_g[l], d_qb_w[l],
                         d_kva_g[l], d_kvb_w[l])
        y = jnp.concatenate([y_a, y_b, y_c, y_d], axis=-1) @ w_out[l]
        x = x + rms_norm(y, post_norm_g[l])
    return x
```

```python
import numpy as np
from contextlib import ExitStack
import concourse.bass as bass
import concourse.mybir as mybir
from concourse.bass_utils import run_bass_kernel_spmd

F32 = mybir.dt.float32
BF16 = mybir.dt.bfloat16
I32 = mybir.dt.int32
AF = mybir.ActivationFunctionType
ALU = mybir.AluOpType
AX = mybir.AxisListType

D_MODEL = 1024
SEQ = 16384
DEPTH = 4
NCORE = 8
SEG = 2048
NSEG = 8
TOK = 4096
T = 512
NT = TOK // T
NPROJ = 3584
SEM_ROT = 24000


class Op:
    __slots__ = ("eng", "fn", "waits", "sem", "val", "dma", "idx")


class Prog:
    ENGS = ("pe", "act", "dve", "pool", "sp")

    def __init__(self, nc, es, ndma_sems=14):
        self.nc = nc
        self.es = es
        self.ops = {e: [] for e in self.ENGS}
        self.count = {e: 0 for e in self.ENGS}
        self.last_op = {e: None for e in self.ENGS}
        self.last_w = {}
        self.readers = {}
        self.waited = {e: {} for e in self.ENGS}
        self.eng_sems = {e: [] for e in self.ENGS}
        self.dsems = [es.enter_context(nc.semaphore(f"dma{i}")) for i in range(ndma_sems)]
        self.ndma = 0
        self.dma_ops = []
        self.banks = [es.enter_context(nc.psum_tensor(f"psbank{i}", [128, T], F32)) for i in range(8)]
        self.alias = {}
        self.nbank = 0

    def ps_reset(self):
        self.nbank = 0

    def bank(self, name):
        k = self.nbank % 8
        self.nbank += 1
        self.alias[name] = ("PS", k)
        return self.banks[k]

    def canon(self, k):
        if isinstance(k, str):
            return self.alias.get(k, k)
        if isinstance(k, tuple) and len(k) == 2 and isinstance(k[0], str):
            return self.alias.get(k[0] + str(k[1]), k)
        return k

    def _eng_sem(self, eng, k):
        lst = self.eng_sems[eng]
        while len(lst) <= k:
            lst.append(self.es.enter_context(self.nc.semaphore(f"s_{eng}{len(lst)}")))
        return lst[k]

    def _need(self, op, d, raw):
        if d is None or d is op:
            return
        if not d.dma and d.eng == op.eng:
            if op.eng == "pe" or op.eng == "sp":
                return
        key = id(d.sem)
        if self.waited[op.eng].get(key, 0) >= d.val:
            return
        self.waited[op.eng][key] = d.val
        op.waits.append((d.sem, d.val))

    def add(self, eng, fn, reads=(), writes=(), dma=False):
        op = Op()
        op.eng, op.fn, op.waits, op.dma = eng, fn, [], dma
        op.idx = self.count[eng]
        self.count[eng] += 1
        reads = [self.canon(k) for k in reads]
        writes = [self.canon(k) for k in writes]
        if dma:
            n = self.ndma
            R = len(self.dsems)
            op.sem = self.dsems[n % R]
            op.val = 16 * (n // R + 1)
            if n >= R:
                prev = self.dma_ops[n - R]
                self._need(op, prev, True)
            self.ndma += 1
            self.dma_ops.append(op)
        else:
            k = op.idx // SEM_ROT
            op.sem = self._eng_sem(eng, k)
            op.val = op.idx % SEM_ROT + 1
        for k in reads:
            self._need(op, self.last_w.get(k), True)
        for k in writes:
            self._need(op, self.last_w.get(k), False)
            for r in self.readers.get(k, ()):
                self._need(op, r, False)
        for k in reads:
            lst = self.readers.setdefault(k, [])
            if not dma:
                lst[:] = [r for r in lst if r.dma or r.eng != eng]
            lst.append(op)
        for k in writes:
            self.last_w[k] = op
            self.readers[k] = []
        self.ops[eng].append(op)
        self.last_op[eng] = op
        return op

    def phase_end(self):
        R = len(self.dsems)
        for eng in self.ENGS:
            op = Op()
            op.eng, op.fn, op.waits, op.dma, op.idx = eng, None, [], False, -1
            for x in self.ENGS:
                if x != eng and x != "sp" and self.last_op[x] is not None:
                    self._need(op, self.last_op[x], True)
            for d in self.dma_ops[-R:]:
                self._need(op, d, True)
            self.ops[eng].append(op)
        self.emit()
        self.ops = {e: [] for e in self.ENGS}
        self.last_w = {}
        self.readers = {}
        self.alias = {}
        self.nbank = 0

    def pe(self, fn, reads=(), writes=()):
        return self.add("pe", fn, reads, writes)

    def act(self, fn, reads=(), writes=()):
        return self.add("act", fn, reads, writes)

    def dve(self, fn, reads=(), writes=()):
        return self.add("dve", fn, reads, writes)

    def pool(self, fn, reads=(), writes=()):
        return self.add("pool", fn, reads, writes)

    def dma(self, out, in_, reads=(), writes=(), **kw):
        return self.add("sp", lambda e: e.dma_start(out=out, in_=in_, **kw), reads, writes, dma=True)

    def finish(self):
        pass

    def emit(self):
        nc = self.nc
        with nc.Block() as block:
            def replay(name, e):
                for op in self.ops[name]:
                    for (s, v) in op.waits:
                        e.wait_ge(s, v)
                    if op.fn is None:
                        continue
                    ins = op.fn(e)
                    ins.then_inc(op.sem, 16 if op.dma else 1)

            @block.tensor
            def _(e):
                replay("pe", e)

            @block.scalar
            def _(e):
                replay("act", e)

            @block.vector
            def _(e):
                replay("dve", e)

            @block.gpsimd
            def _(e):
                replay("pool", e)

            @block.sync
            def _(e):
                replay("sp", e)


_UNIQ = [0]


def sb(nc, es, name, shape, dt):
    _UNIQ[0] += 1
    return es.enter_context(nc.sbuf_tensor(f"{name}_{_UNIQ[0]}", list(shape), dt))


def psb(P, name):
    return P.bank(name)


def emit_l1(nc, P, es, xT, w_in, V, projT, tag="l1", ntok=TOK):
    NCC = NPROJ // 128
    Wb = sb(nc, es, tag + "Wb", [128, 8, NPROJ], BF16)
    Wst = [sb(nc, es, tag + f"Wst{i}", [128, NPROJ], F32) for i in range(2)]
    X = [sb(nc, es, tag + f"X{i}", [128, 8, T], F32) for i in range(2)]
    Xsq = sb(nc, es, tag + "Xsq", [128, 8, T], BF16)
    H = [sb(nc, es, tag + f"H{i}", [128, 8, T], BF16) for i in range(2)]
    ones = sb(nc, es, tag + "ones", [128, 128], BF16)
    rs = sb(nc, es, tag + "rs", [128, T], F32)
    O = [sb(nc, es, tag + f"O{i}", [128, T], F32) for i in range(4)]
    ps_ss = psb(P, tag + "ps_ss")
    ps = [psb(P, tag + f"ps{i}") for i in range(4)]

    P.pool(lambda e: e.memset(ones[:], 1.0), writes=["ones"])
    w_v = w_in.rearrange("(kc p) n -> kc p n", p=128)
    for kc in range(8):
        st = Wst[kc % 2]
        P.dma(st[:], w_v[kc], writes=[("Wst", kc % 2)])
        P.pool(lambda e, st=st, kc=kc: e.tensor_copy(out=Wb[:, kc, :], in_=st[:]),
               reads=[("Wst", kc % 2)], writes=[("Wb", kc)])
    x_v = xT.rearrange("(kc p) t -> p kc t", p=128)
    nout = 0
    for it in range(ntok // T):
        xs = X[it % 2]
        hs = H[it % 2]
        ts = slice(it * T, (it + 1) * T)
        P.dma(xs[:], x_v[:, :, ts], reads=[("xT", it)], writes=[("X", it % 2)])
        P.act(lambda e, xs=xs: e.activation(out=Xsq[:], in_=xs[:], func=AF.Square),
              reads=[("X", it % 2)], writes=["Xsq"])
        for kc in range(8):
            P.pe(lambda e, kc=kc: e.matmul(ps_ss[:], lhsT=ones[:], rhs=Xsq[:, kc, :],
                                           start=(kc == 0), stop=(kc == 7)),
                 reads=["ones", "Xsq"], writes=[tag + "ps_ss"])
        P.dve(lambda e: e.tensor_scalar(out=rs[:], in0=ps_ss[:], scalar1=1.0 / D_MODEL, scalar2=1e-6,
                                        op0=ALU.mult, op1=ALU.add),
              reads=[tag + "ps_ss"], writes=["rs"])
        P.act(lambda e: e.activation(out=rs[:], in_=rs[:], func=AF.Sqrt), reads=["rs"], writes=["rs"])
        P.dve(lambda e: e.reciprocal(out=rs[:], in_=rs[:]), reads=["rs"], writes=["rs"])
        for kc in range(8):
            P.dve(lambda e, kc=kc, xs=xs, hs=hs: e.scalar_tensor_tensor(
                out=hs[:, kc, :], in0=xs[:, kc, :], scalar=vcol(V, "pre_g", kc), in1=rs[:],
                op0=ALU.mult, op1=ALU.mult),
                reads=[("X", it % 2), "V", "rs"], writes=[("H", it % 2, kc)])
        for cc in range(NCC):
            pb = ps[cc % 4]
            for kc in range(8):
                P.pe(lambda e, kc=kc, cc=cc, pb=pb, hs=hs: e.matmul(
                    pb[:], lhsT=Wb[:, kc, cc * 128:(cc + 1) * 128], rhs=hs[:, kc, :],
                    start=(kc == 0), stop=(kc == 7)),
                    reads=[("Wb", kc), ("H", it % 2, kc)], writes=[(tag + "ps", cc % 4)])
            ob = O[nout % 4]
            if cc % 2 == 0:
                P.act(lambda e, ob=ob, pb=pb: e.copy(out=ob[:], in_=pb[:]),
                      reads=[(tag + "ps", cc % 4)], writes=[("O", nout % 4)])
            else:
                P.dve(lambda e, ob=ob, pb=pb: e.tensor_copy(out=ob[:], in_=pb[:]),
                      reads=[(tag + "ps", cc % 4)], writes=[("O", nout % 4)])
            P.dma(projT[cc * 128:(cc + 1) * 128, ts], ob[:], reads=[("O", nout % 4)],
                  writes=[("projT", it, cc)])
            nout += 1


VEC_SPEC = [("pre_g", 8), ("post_g", 8), ("a_dw_w", 62), ("a_dw_b", 2), ("a_ln_g", 2), ("a_ln_b", 2),
            ("a_pw_b", 2), ("b_lb", 8), ("b_norm_g", 2), ("c_conv_w", 24), ("c_conv_b", 6),
            ("c_norm_g", 2), ("d_qa_g", 2), ("d_kva_g", 1), ("c_dt_bias", 1), ("c_a_log", 1), ("c_d", 2)]
VEC_OFF = {}
_o = 0
for _n, _w in VEC_SPEC:
    VEC_OFF[_n] = (_o, _w)
    _o += _w
NV = _o


def vcol(V, name, j=0, n=1, rows=128):
    o, w = VEC_OFF[name]
    return V[0:rows, o + j:o + j + n]


def emit_A(nc, P, es, projT, V, pw_w, ycatT, ntok, tag="A"):
    HAL = 32
    val = [sb(nc, es, tag + f"val{i}", [128, 2, T], F32) for i in range(2)]
    glu = [sb(nc, es, tag + f"glu{i}", [128, 2, T], F32) for i in range(2)]
    gat = [sb(nc, es, tag + f"gat{i}", [128, 2, T], F32) for i in range(2)]
    gb = sb(nc, es, tag + "gb", [128, 2, HAL + T], F32)
    acc = sb(nc, es, tag + "acc", [128, 2, T], F32)
    sq = sb(nc, es, tag + "sq", [128, 2, T], F32)
    mean = sb(nc, es, tag + "mean", [128, T], F32)
    rstd = sb(nc, es, tag + "rstd", [128, T], F32)
    hs = sb(nc, es, tag + "hs", [128, 2, T], BF16)
    ya = [sb(nc, es, tag + f"ya{i}", [128, 2, T], BF16) for i in range(2)]
    onesf = sb(nc, es, tag + "onesf", [128, 128], F32)
    pwst = sb(nc, es, tag + "pwst", [128, 2, 256], F32)
    pwb = sb(nc, es, tag + "pwb", [128, 2, 256], BF16)
    ps1 = psb(P, tag + "ps1")
    ps2 = psb(P, tag + "ps2")
    pso = [psb(P, tag + f"pso{i}") for i in range(2)]

    P.pool(lambda e: e.memset(onesf[:], 1.0), writes=[tag + "onesf"])
    P.dma(pwst[:], pw_w.rearrange("(c p) n -> p c n", p=128), writes=[tag + "pwst"])
    P.pool(lambda e: e.tensor_copy(out=pwb[:], in_=pwst[:]), reads=[tag + "pwst"], writes=[tag + "pwb"])
    P.pool(lambda e: e.memset(gb[:, :, 0:HAL], 0.0), writes=[tag + "gb"])
    pv = projT.rearrange("(c p) t -> p c t", p=128)
    for it in range(ntok // T):
        ts = slice(it * T, (it + 1) * T)
        b = it % 2
        P.dma(val[b][:], pv[:, 0:2, ts], reads=[("projT", it)], writes=[(tag + "val", b)])
        P.dma(glu[b][:], pv[:, 2:4, ts], reads=[("projT", it)], writes=[(tag + "glu", b)])
        P.dma(gat[b][:], pv[:, 4:6, ts], reads=[("projT", it)], writes=[(tag + "gat", b)])
        P.act(lambda e, b=b: e.activation(out=glu[b][:], in_=glu[b][:], func=AF.Sigmoid),
              reads=[(tag + "glu", b)], writes=[(tag + "glu", b)])
        P.dve(lambda e, b=b: e.tensor_tensor(out=gb[:, :, HAL:HAL + T], in0=val[b][:], in1=glu[b][:], op=ALU.mult),
              reads=[(tag + "glu", b), (tag + "val", b)], writes=[tag + "gb"])
        P.act(lambda e, b=b: e.activation(out=gat[b][:], in_=gat[b][:], func=AF.Silu),
              reads=[(tag + "gat", b)], writes=[(tag + "gat", b)])
        for c in range(2):
            eng = P.dve
            for j in range(31):
                src = gb[:, c, HAL - 30 + j:HAL - 30 + j + T]
                wj = vcol(V, "a_dw_w", c * 31 + j)
                if j == 0:
                    eng(lambda e, c=c, src=src, wj=wj: e.tensor_scalar(
                        out=acc[:, c, :], in0=src, scalar1=wj, scalar2=vcol(V, "a_dw_b", c),
                        op0=ALU.mult, op1=ALU.add),
                        reads=[tag + "gb", "V"], writes=[(tag + "acc", c)])
                else:
                    eng(lambda e, c=c, src=src, wj=wj: e.scalar_tensor_tensor(
                        out=acc[:, c, :], in0=src, scalar=wj, in1=acc[:, c, :], op0=ALU.mult, op1=ALU.add),
                        reads=[tag + "gb", "V", (tag + "acc", c)], writes=[(tag + "acc", c)])
        P.act(lambda e: e.copy(out=gb[:, :, 0:HAL], in_=gb[:, :, T:T + HAL]),
              reads=[tag + "gb"], writes=[tag + "gb"])
        P.act(lambda e: e.activation(out=sq[:], in_=acc[:], func=AF.Square),
              reads=[(tag + "acc", 0), (tag + "acc", 1)], writes=[tag + "sq"])
        for c in range(2):
            P.pe(lambda e, c=c: e.matmul(ps1[:], lhsT=onesf[:], rhs=acc[:, c, :], start=(c == 0), stop=(c == 1)),
                 reads=[tag + "onesf", (tag + "acc", c)], writes=[tag + "ps1"])
        for c in range(2):
            P.pe(lambda e, c=c: e.matmul(ps2[:], lhsT=onesf[:], rhs=sq[:, c, :], start=(c == 0), stop=(c == 1)),
                 reads=[tag + "onesf", tag + "sq"], writes=[tag + "ps2"])
        P.dve(lambda e: e.tensor_scalar(out=mean[:], in0=ps1[:], scalar1=1.0 / 256, scalar2=None, op0=ALU.mult),
              reads=[tag + "ps1"], writes=[tag + "mean"])
        P.dve(lambda e: e.tensor_tensor(out=rstd[:], in0=mean[:], in1=mean[:], op=ALU.mult),
              reads=[tag + "mean"], writes=[tag + "rstd"])
        P.dve(lambda e: e.scalar_tensor_tensor(out=rstd[:], in0=ps2[:], scalar=1.0 / 256, in1=rstd[:],
                                               op0=ALU.mult, op1=ALU.subtract),
              reads=[tag + "ps2", tag + "rstd"], writes=[tag + "rstd"])
        P.dve(lambda e: e.tensor_scalar(out=rstd[:], in0=rstd[:], scalar1=1e-5, scalar2=None, op0=ALU.add),
              reads=[tag + "rstd"], writes=[tag + "rstd"])
        P.act(lambda e: e.activation(out=rstd[:], in_=rstd[:], func=AF.Sqrt), reads=[tag + "rstd"], writes=[tag + "rstd"])
        P.dve(lambda e: e.reciprocal(out=rstd[:], in_=rstd[:]), reads=[tag + "rstd"], writes=[tag + "rstd"])
        for c in range(2):
            P.dve(lambda e, c=c: e.tensor_tensor(out=acc[:, c, :], in0=acc[:, c, :], in1=mean[:], op=ALU.subtract),
                  reads=[(tag + "acc", c), tag + "mean", tag + "ps1"], writes=[(tag + "acc", c)])
            P.dve(lambda e, c=c: e.tensor_tensor(out=acc[:, c, :], in0=acc[:, c, :], in1=rstd[:], op=ALU.mult),
                  reads=[(tag + "acc", c), tag + "rstd"], writes=[(tag + "acc", c)])
            P.act(lambda e, c=c: e.activation(out=hs[:, c, :], in_=acc[:, c, :], func=AF.Silu,
                                              scale=vcol(V, "a_ln_g", c), bias=vcol(V, "a_ln_b", c)),
                  reads=[(tag + "acc", c), "V"], writes=[(tag + "hs", c)])
        for co in range(2):
            for ci in range(2):
                P.pe(lambda e, co=co, ci=ci: e.matmul(pso[co][:], lhsT=pwb[:, ci, co * 128:(co + 1) * 128],
                                                      rhs=hs[:, ci, :], start=(ci == 0), stop=(ci == 1)),
                     reads=[tag + "pwb", (tag + "hs", ci)], writes=[(tag + "pso", co)])
            P.dve(lambda e, co=co, b=b: e.scalar_tensor_tensor(
                out=ya[b][:, co, :], in0=pso[co][:], scalar=vcol(V, "a_pw_b", co), in1=gat[b][:, co, :],
                op0=ALU.add, op1=ALU.mult),
                reads=[(tag + "pso", co), (tag + "gat", b), "V"], writes=[(tag + "ya", b)])
        P.dma(ycatT.rearrange("(c p) t -> p c t", p=128)[:, 0:2, ts], ya[b][:],
              reads=[(tag + "ya", b)], writes=[("ycatA", it)])


COLMAP = [(0, 768), (768, 1792), (1792, 2560), (2564, 2820), (2820, 3076), (3076, 3204),
          (3236, 3492), (3204, 3236), (2560, 2564)]
R_VAL, R_GLU, R_AG = 0, 256, 512
R_BQ, R_BF, R_BI, R_BG = 768, 1024, 1280, 1536
R_XBC, R_Z, R_CQ, R_CKV, R_DG, R_KR, R_DT = 1792, 2560, 2816, 3072, 3200, 3456, 3488


def pack_w_in(w):
    out = np.zeros((D_MODEL, NPROJ), np.float32)
    o = 0
    for a, b in COLMAP:
        out[:, o:o + b - a] = w[:, a:b]
        o += b - a
    return out


def _pc(v):
    return np.ascontiguousarray(np.asarray(v, np.float32).reshape(-1, 128).T)


def pack_vecs(p, l):
    V = np.zeros((128, NV), np.float32)

    def put(name, arr):
        o, w = VEC_OFF[name]
        V[:arr.shape[0], o:o + arr.shape[1]] = arr

    put("pre_g", _pc(p["pre_norm_g"][l]))
    put("post_g", _pc(p["post_norm_g"][l]))
    w = np.asarray(p["a_dw_w"][l], np.float32)
    put("a_dw_w", np.concatenate([w[:, c * 128:(c + 1) * 128].T for c in range(2)], axis=1))
    put("a_dw_b", _pc(p["a_dw_b"][l]))
    put("a_ln_g", _pc(p["a_ln_g"][l]))
    put("a_ln_b", _pc(p["a_ln_b"][l]))
    put("a_pw_b", _pc(p["a_pw_b"][l]))
    lg = np.asarray(p["b_lb_logits"], np.float32)
    put("b_lb", np.concatenate([lg[:, c * 128:(c + 1) * 128].T for c in range(2)], axis=1))
    put("b_norm_g", _pc(p["b_norm_g"][l]))
    cw = np.asarray(p["c_conv_w"][l], np.float32)
    put("c_conv_w", np.concatenate([cw[:, c * 128:(c + 1) * 128].T for c in range(6)], axis=1))
    put("c_conv_b", _pc(p["c_conv_b"][l]))
    put("c_norm_g", _pc(p["c_norm_g"][l]))
    put("d_qa_g", _pc(p["d_qa_g"][l]))
    put("d_kva_g", _pc(p["d_kva_g"][l]))
    for nm in ("c_dt_bias", "c_a_log"):
        col = np.zeros((128, 1), np.float32)
        col[0:4, 0] = np.asarray(p[nm][l], np.float32)
        col[64:68, 0] = np.asarray(p[nm][l], np.float32)
        put(nm, col)
    cd = np.asarray(p["c_d"][l], np.float32)
    put("c_d", np.stack([np.repeat(cd[2 * g:2 * g + 2], 64) for g in range(2)], axis=1))
    return V


def emit_O(nc, P, es, ycatT, xT_in, V, w_out, xT_out, ntok, tag="O"):
    Wo = sb(nc, es, tag + "Wo", [128, 8, D_MODEL], BF16)
    Wst = [sb(nc, es, tag + f"Wst{i}", [128, D_MODEL], F32) for i in range(2)]
    Y = [sb(nc, es, tag + f"Y{i}", [128, 8, T], BF16) for i in range(2)]
    X = [sb(nc, es, tag + f"X{i}", [128, 8, T], F32) for i in range(2)]
    Yo = sb(nc, es, tag + "Yo", [128, 8, T], F32)
    Ysq = sb(nc, es, tag + "Ysq", [128, 8, T], BF16)
    ones = sb(nc, es, tag + "ones", [128, 128], BF16)
    rs = sb(nc, es, tag + "rs", [128, T], F32)
    ps = [psb(P, tag + f"ps{i}") for i in range(4)]
    ps_ss = psb(P, tag + "ps_ss")
    P.pool(lambda e: e.memset(ones[:], 1.0), writes=[tag + "ones"])
    w_v = w_out.rearrange("(kc p) n -> kc p n", p=128)
    for kc in range(8):
        st = Wst[kc % 2]
        P.dma(st[:], w_v[kc], writes=[(tag + "Wst", kc % 2)])
        P.pool(lambda e, st=st, kc=kc: e.tensor_copy(out=Wo[:, kc, :], in_=st[:]),
               reads=[(tag + "Wst", kc % 2)], writes=[(tag + "Wo", kc)])
    yv = ycatT.rearrange("(c p) t -> p c t", p=128)
    xv = xT_in.rearrange("(c p) t -> p c t", p=128)
    xo = xT_out.rearrange("(c p) t -> p c t", p=128)
    for it in range(ntok // T):
        ts = slice(it * T, (it + 1) * T)
        b = it % 2
        P.dma(Y[b][:], yv[:, :, ts], reads=[("ycatA", it), ("ycatB", it), ("ycatC", it), ("ycatD", it)],
              writes=[(tag + "Y", b)])
        P.dma(X[b][:], xv[:, :, ts], reads=[("xT", it)], writes=[(tag + "X", b)])
        for do in range(8):
            pb = ps[do % 4]
            for kc in range(8):
                P.pe(lambda e, kc=kc, do=do, pb=pb, b=b: e.matmul(
                    pb[:], lhsT=Wo[:, kc, do * 128:(do + 1) * 128], rhs=Y[b][:, kc, :],
                    start=(kc == 0), stop=(kc == 7)),
                    reads=[(tag + "Wo", kc), (tag + "Y", b)], writes=[(tag + "ps", do % 4)])
            P.act(lambda e, do=do, pb=pb: e.copy(out=Yo[:, do, :], in_=pb[:]),
                  reads=[(tag + "ps", do % 4)], writes=[(tag + "Yo", do)])
            P.act(lambda e, do=do, pb=pb: e.activation(out=Ysq[:, do, :], in_=pb[:], func=AF.Square),
                  reads=[(tag + "ps", do % 4)], writes=[(tag + "Ysq", do)])
        for do in range(8):
            P.pe(lambda e, do=do: e.matmul(ps_ss[:], lhsT=ones[:], rhs=Ysq[:, do, :],
                                           start=(do == 0), stop=(do == 7)),
                 reads=[tag + "ones", (tag + "Ysq", do)], writes=[tag + "ps_ss"])
        P.dve(lambda e: e.tensor_scalar(out=rs[:], in0=ps_ss[:], scalar1=1.0 / D_MODEL, scalar2=1e-6,
                                        op0=ALU.mult, op1=ALU.add), reads=[tag + "ps_ss"], writes=[tag + "rs"])
        P.act(lambda e: e.activation(out=rs[:], in_=rs[:], func=AF.Sqrt), reads=[tag + "rs"], writes=[tag + "rs"])
        P.dve(lambda e: e.reciprocal(out=rs[:], in_=rs[:]), reads=[tag + "rs"], writes=[tag + "rs"])
        for do in range(8):
            P.dve(lambda e, do=do: e.scalar_tensor_tensor(
                out=Yo[:, do, :], in0=Yo[:, do, :], scalar=vcol(V, "post_g", do), in1=rs[:],
                op0=ALU.mult, op1=ALU.mult), reads=[(tag + "Yo", do), tag + "rs", "V"], writes=[(tag + "Yo", do)])
            P.dve(lambda e, do=do, b=b: e.tensor_tensor(out=Yo[:, do, :], in0=Yo[:, do, :], in1=X[b][:, do, :],
                                                        op=ALU.add),
                   reads=[(tag + "Yo", do), (tag + "X", b)], writes=[(tag + "Yo", do)])
        P.dma(xo[:, :, ts], Yo[:], reads=[(tag + "Yo", do) for do in range(8)], writes=[("xTo", it)])


NCP = 8
TWO_PI = float(2 * np.pi)


def make_cp():
    cp = np.zeros((128, NCP), np.float32)
    inv = (10000.0 ** (-np.arange(0, 32, 2, dtype=np.float32) / 32)).astype(np.float32)
    cp[64:96, 0] = np.concatenate([inv, inv])
    cp[64:96, 1] = np.concatenate([-np.ones(16), np.ones(16)])
    cp[64:96, 2] = np.concatenate([-np.pi * np.ones(16), np.pi * np.ones(16)])
    cp[:, 3] = np.pi
    cp[:, 4] = np.pi / 2
    return cp


def pack_qb(w):
    w = np.asarray(w, np.float32).reshape(256, 4, 96)
    b = w.copy()
    b[:, :, 64:80] = w[:, :, 80:96]
    b[:, :, 80:96] = w[:, :, 64:80]
    return np.ascontiguousarray(np.stack([w, b], axis=2))


def pack_kvb(w):
    w = np.asarray(w, np.float32).reshape(128, 4, 128)
    return np.ascontiguousarray(np.stack([w[:, :, 0:64].reshape(128, 256), w[:, :, 64:128].reshape(128, 256)], axis=1))


def emit_D(nc, P, es, projT, pos, V, CP, qbw, kvbw, QT, KT, VD, OD, ycatT, ntok, tag="D"):
    nt = ntok // T
    SC = float(96 ** -0.5)
    qst = sb(nc, es, tag + "qst", [128, 2, 768], F32)
    qb = sb(nc, es, tag + "qb", [128, 2, 768], BF16)
    kst = sb(nc, es, tag + "kst", [128, 512], F32)
    kb_ = sb(nc, es, tag + "kb", [128, 512], BF16)
    ones = sb(nc, es, tag + "ones", [128, 128], BF16)
    cq = [sb(nc, es, tag + f"cq{i}", [128, 2, T], F32) for i in range(2)]
    ckv = [sb(nc, es, tag + f"ckv{i}", [128, T], F32) for i in range(2)]
    krA = [sb(nc, es, tag + f"krA{i}", [128, T], F32) for i in range(2)]
    krB = [sb(nc, es, tag + f"krB{i}", [128, T], F32) for i in range(2)]
    posi = [sb(nc, es, tag + f"posi{i}", [128, T], I32) for i in range(2)]
    sqb = sb(nc, es, tag + "sqb", [128, 3, T], BF16)
    rs = sb(nc, es, tag + "rs", [128, T], F32)
    rs2 = sb(nc, es, tag + "rs2", [128, T], F32)
    cqn = sb(nc, es, tag + "cqn", [128, 2, T], BF16)
    ckvn = sb(nc, es, tag + "ckvn", [128, T], BF16)
    ang = sb(nc, es, tag + "ang", [128, T], F32)
    cosT = sb(nc, es, tag + "cos", [128, T], F32)
    sinT = sb(nc, es, tag + "sin", [128, T], F32)
    tmp = sb(nc, es, tag + "tmp", [128, T], F32)
    tmp2 = sb(nc, es, tag + "tmp2", [128, T], F32)
    kf = sb(nc, es, tag + "kf", [128, T], F32)
    ki = sb(nc, es, tag + "ki", [128, T], I32)
    qt = [sb(nc, es, tag + f"qt{i}", [96, 4, T], BF16) for i in range(2)]
    kt = [sb(nc, es, tag + f"kt{i}", [96, 4, T], BF16) for i in range(2)]
    vt = [sb(nc, es, tag + f"vt{i}", [128, 4, 4, 65], BF16) for i in range(2)]
    ps_a = psb(P, tag + "ps_a")
    ps_b = psb(P, tag + "ps_b")
    ps_q = [psb(P, tag + f"ps_q{i}") for i in range(2)]
    ps_q2 = [psb(P, tag + f"ps_q2{i}") for i in range(2)]
    ps_v = psb(P, tag + "ps_v")

    P.pool(lambda e: e.memset(ones[:], 1.0), writes=[tag + "ones"])
    P.dma(qst[:], qbw.rearrange("(c p) n -> p c n", p=128), writes=[tag + "qst"])
    P.pool(lambda e: e.tensor_copy(out=qb[:], in_=qst[:]), reads=[tag + "qst"], writes=[tag + "qb"])
    P.dma(kst[:], kvbw[:, :], writes=[tag + "kst"])
    P.pool(lambda e: e.tensor_copy(out=kb_[:], in_=kst[:]), reads=[tag + "kst"], writes=[tag + "kb"])
    for i in range(2):
        P.pool(lambda e, i=i: e.memset(vt[i][:, :, :, 64:65], 1.0), writes=[(tag + "vt", i)])
    pv = projT.rearrange("(c p) t -> p c t", p=128)
    QTv = QT.rearrange("h d t -> d h t")
    KTv = KT.rearrange("h d t -> d h t")
    for it in range(nt):
        ts = slice(it * T, (it + 1) * T)
        b = it % 2
        P.dma(cq[b][:], pv[:, 22:24, ts], reads=[("projT", it)], writes=[(tag + "cq", b)])
        P.dma(ckv[b][:], projT[R_CKV:R_CKV + 128, ts], reads=[("projT", it)], writes=[(tag + "ckv", b)])
        P.dma(krA[b][64:96, :], projT[R_KR:R_KR + 32, ts], reads=[("projT", it)], writes=[(tag + "krA", b)])
        P.dma(krB[b][64:80, :], projT[R_KR + 16:R_KR + 32, ts], reads=[("projT", it)], writes=[(tag + "krB", b)])
        P.dma(krB[b][80:96, :], projT[R_KR:R_KR + 16, ts], reads=[("projT", it)], writes=[(tag + "krB", b)])
        P.dma(posi[b][64:96, :], pos[0:1, ts].partition_broadcast(32), writes=[(tag + "posi", b)])
        P.act(lambda e, b=b: e.activation(out=sqb[:, 0:2, :], in_=cq[b][:], func=AF.Square),
              reads=[(tag + "cq", b)], writes=[tag + "sqq"])
        P.act(lambda e, b=b: e.activation(out=sqb[:, 2, :], in_=ckv[b][:], func=AF.Square),
              reads=[(tag + "ckv", b)], writes=[tag + "sqk"])
        for c in range(2):
            P.pe(lambda e, c=c: e.matmul(ps_a[:], lhsT=ones[:], rhs=sqb[:, c, :], start=(c == 0), stop=(c == 1)),
                 reads=[tag + "ones", tag + "sqq"], writes=[tag + "ps_a"])
        P.pe(lambda e: e.matmul(ps_b[:], lhsT=ones[:], rhs=sqb[:, 2, :], start=True, stop=True),
             reads=[tag + "ones", tag + "sqk"], writes=[tag + "ps_b"])
        for (pss, r, n) in ((ps_a, rs, 256.0), (ps_b, rs2, 128.0)):
            k1, k2 = (tag + "ps_a", tag + "rs") if pss is ps_a else (tag + "ps_b", tag + "rs2")
            P.dve(lambda e, pss=pss, r=r, n=n: e.tensor_scalar(out=r[:], in0=pss[:], scalar1=1.0 / n, scalar2=1e-6,
                                                             op0=ALU.mult, op1=ALU.add), reads=[k1], writes=[k2])
            P.act(lambda e, r=r: e.activation(out=r[:], in_=r[:], func=AF.Sqrt), reads=[k2], writes=[k2])
            P.dve(lambda e, r=r: e.reciprocal(out=r[:], in_=r[:]), reads=[k2], writes=[k2])
        for c in range(2):
            P.dve(lambda e, c=c, b=b: e.scalar_tensor_tensor(out=cqn[:, c, :], in0=cq[b][:, c, :],
                                                            scalar=vcol(V, "d_qa_g", c), in1=rs[:],
                                                            op0=ALU.mult, op1=ALU.mult),
                  reads=[(tag + "cq", b), tag + "rs", "V"], writes=[tag + "cqn"])
        P.dve(lambda e, b=b: e.scalar_tensor_tensor(out=ckvn[:], in0=ckv[b][:], scalar=vcol(V, "d_kva_g"),
                                                     in1=rs2[:], op0=ALU.mult, op1=ALU.mult),
              reads=[(tag + "ckv", b), tag + "rs2", "V"], writes=[tag + "ckvn"])
        R_ = slice(64, 96)
        P.dve(lambda e, b=b: e.tensor_copy(out=ang[R_, :], in_=posi[b][R_, :]), reads=[(tag + "posi", b)],
              writes=[tag + "ang"])
        P.dve(lambda e: e.tensor_scalar(out=ang[R_, :], in0=ang[R_, :], scalar1=CP[R_, 0:1], scalar2=None,
                                        op0=ALU.mult), reads=[tag + "ang", "CP"], writes=[tag + "ang"])
        def reduce_angle(dst, kd, add_half_pi):
            if add_half_pi:
                P.dve(lambda e: e.tensor_scalar(out=dst[R_, :], in0=ang[R_, :], scalar1=CP[R_, 4:5], scalar2=None,
                                                op0=ALU.add), reads=[tag + "ang", "CP"], writes=[kd])
            else:
                P.dve(lambda e: e.tensor_copy(out=dst[R_, :], in_=ang[R_, :]), reads=[tag + "ang"], writes=[kd])
            P.dve(lambda e: e.tensor_scalar(out=kf[R_, :], in0=dst[R_, :], scalar1=1.0 / TWO_PI, scalar2=None,
                                            op0=ALU.mult), reads=[kd], writes=[tag + "kf"])
            P.dve(lambda e: e.tensor_copy(out=ki[R_, :], in_=kf[R_, :]), reads=[tag + "kf"], writes=[tag + "ki"])
            P.dve(lambda e: e.tensor_copy(out=kf[R_, :], in_=ki[R_, :]), reads=[tag + "ki"], writes=[tag + "kf"])
            P.dve(lambda e: e.scalar_tensor_tensor(out=dst[R_, :], in0=kf[R_, :], scalar=-6.28125, in1=dst[R_, :],
                                                   op0=ALU.mult, op1=ALU.add), reads=[tag + "kf", kd], writes=[kd])
            P.dve(lambda e: e.scalar_tensor_tensor(out=dst[R_, :], in0=kf[R_, :], scalar=-(TWO_PI - 6.28125),
                                                   in1=dst[R_, :], op0=ALU.mult, op1=ALU.add),
                  reads=[tag + "kf", kd], writes=[kd])
            P.dve(lambda e: e.tensor_scalar(out=kf[R_, :], in0=dst[R_, :], scalar1=float(np.pi), scalar2=None,
                                            op0=ALU.is_gt), reads=[kd], writes=[tag + "kf"])
            P.dve(lambda e: e.scalar_tensor_tensor(out=dst[R_, :], in0=kf[R_, :], scalar=-TWO_PI, in1=dst[R_, :],
                                                   op0=ALU.mult, op1=ALU.add), reads=[tag + "kf", kd], writes=[kd])

        reduce_angle(tmp, tag + "tmp", False)
        P.act(lambda e: e.activation(out=sinT[R_, :], in_=tmp[R_, :], func=AF.Sin, scale=CP[R_, 1:2]),
              reads=[tag + "tmp", "CP"], writes=[tag + "sin"])
        reduce_angle(tmp2, tag + "tmp2", True)
        P.act(lambda e: e.activation(out=cosT[R_, :], in_=tmp2[R_, :], func=AF.Sin),
              reads=[tag + "tmp2"], writes=[tag + "cos"])
        P.dve(lambda e, b=b: e.tensor_tensor(out=krA[b][R_, :], in0=krA[b][R_, :], in1=cosT[R_, :], op=ALU.mult),
              reads=[(tag + "krA", b), tag + "cos"], writes=[(tag + "krA", b)])
        P.dve(lambda e, b=b: e.tensor_tensor(out=krB[b][R_, :], in0=krB[b][R_, :], in1=sinT[R_, :], op=ALU.mult),
              reads=[(tag + "krB", b), tag + "sin"], writes=[(tag + "krB", b)])
        for h in range(4):
            P.dve(lambda e, b=b, h=h: e.tensor_tensor(out=kt[b][R_, h, :], in0=krA[b][R_, :], in1=krB[b][R_, :],
                                                       op=ALU.add),
                   reads=[(tag + "krA", b), (tag + "krB", b)], writes=[(tag + "kt", b)])
        for h in range(4):
            P.pe(lambda e, h=h: e.matmul(ps_q[h % 2][0:64, :], lhsT=kb_[:, h * 64:(h + 1) * 64], rhs=ckvn[:],
                                         start=True, stop=True),
                 reads=[tag + "kb", tag + "ckvn"], writes=[(tag + "ps_q", h % 2)])
            P.act(lambda e, h=h, b=b: e.copy(out=kt[b][0:64, h, :], in_=ps_q[h % 2][0:64, :]),
                  reads=[(tag + "ps_q", h % 2)], writes=[(tag + "kt", b)])
        for h in range(4):
            pA, pB = ps_q[h % 2], ps_q2[h % 2]
            for c in range(2):
                P.pe(lambda e, h=h, c=c, pA=pA: e.matmul(pA[0:96, :], lhsT=qb[:, c, (h * 2) * 96:(h * 2 + 1) * 96],
                                                         rhs=cqn[:, c, :], start=(c == 0), stop=(c == 1)),
                     reads=[tag + "qb", tag + "cqn"], writes=[(tag + "ps_q", h % 2)])
            for c in range(2):
                P.pe(lambda e, h=h, c=c, pB=pB: e.matmul(pB[0:96, :], lhsT=qb[:, c, (h * 2 + 1) * 96:(h * 2 + 2) * 96],
                                                         rhs=cqn[:, c, :], start=(c == 0), stop=(c == 1)),
                     reads=[tag + "qb", tag + "cqn"], writes=[(tag + "ps_q2", h % 2)])
            P.act(lambda e, h=h, b=b, pA=pA: e.copy(out=qt[b][0:64, h, :], in_=pA[0:64, :]),
                  reads=[(tag + "ps_q", h % 2)], writes=[(tag + "qt", b)])
            P.dve(lambda e, pA=pA: e.tensor_tensor(out=tmp[R_, :], in0=pA[R_, :], in1=cosT[R_, :], op=ALU.mult),
                  reads=[(tag + "ps_q", h % 2), tag + "cos"], writes=[tag + "tmp"])
            P.dve(lambda e, pB=pB: e.tensor_tensor(out=tmp2[R_, :], in0=pB[R_, :], in1=sinT[R_, :], op=ALU.mult),
                  reads=[(tag + "ps_q2", h % 2), tag + "sin"], writes=[tag + "tmp2"])
            P.dve(lambda e, h=h, b=b: e.tensor_tensor(out=qt[b][R_, h, :], in0=tmp[R_, :], in1=tmp2[R_, :], op=ALU.add),
                  reads=[tag + "tmp", tag + "tmp2"], writes=[(tag + "qt", b)])
        for j in range(4):
            P.pe(lambda e, j=j: e.matmul(ps_v[:, 0:256],
                                         lhsT=ckvn[:, j * 128:(j + 1) * 128], rhs=kb_[:, 256:512],
                                         start=True, stop=True),
                 reads=[tag + "ckvn", tag + "kb"], writes=[tag + "ps_v"])
            P.act(lambda e, j=j, b=b: e.copy(out=vt[b][:, j, :, 0:64],
                                             in_=ps_v[:, 0:256].rearrange("p (h v) -> p h v", h=4)),
                  reads=[tag + "ps_v"], writes=[(tag + "vt", b)])
        P.dma(QTv[:, :, ts], qt[b][:], reads=[(tag + "qt", b)], writes=[("QT", it)])
        P.dma(KTv[:, :, ts], kt[b][:], reads=[(tag + "kt", b)], writes=[("KT", it)])
        for h in range(4):
            P.dma(VD[h, :, it * 4:(it + 1) * 4, :], vt[b][:, :, h, :], reads=[(tag + "vt", b)], writes=[("VD", it)])

    P.ps_reset()
    Kall = sb(nc, es, tag + "Kall", [96, ntok], BF16)
    Vall = sb(nc, es, tag + "Vall", [128, ntok // 128, 65], BF16)
    Qg = [sb(nc, es, tag + f"Qg{i}", [96, T], BF16) for i in range(2)]
    Pt = [sb(nc, es, tag + f"Pt{i}", [128, T], BF16) for i in range(3)]
    Ost = [sb(nc, es, tag + f"Ost{i}", [65, T], F32) for i in range(2)]
    S_ps = [psb(P, tag + f"S_ps{i}") for i in range(3)]
    O_ps = [psb(P, tag + f"O_ps{i}") for i in range(2)]
    allk = [("KT", i) for i in range(nt)]
    allv = [("VD", i) for i in range(nt)]
    n = 0
    ng = 0
    for h in range(4):
        P.dma(Kall[:], KT[h], reads=allk, writes=[tag + "Kall"])
        P.dma(Vall[:], VD[h], reads=allv, writes=[tag + "Vall"])
        for qg in range(nt):
            gb = ng % 2
            ng += 1
            P.dma(Qg[gb][:], QT[h, :, qg * T:(qg + 1) * T], reads=[("QT", qg)], writes=[(tag + "Qg", gb)])
            nkb = 4 * qg + 4
            for kb in range(nkb):
                j = kb - 4 * qg
                c0 = max(0, j) * 128
                sp, pt = S_ps[n % 3], Pt[n % 3]
                kS, kP = (tag + "S_ps", n % 3), (tag + "Pt", n % 3)
                n += 1
                P.pe(lambda e, sp=sp, kb=kb, gb=gb, c0=c0: e.matmul(
                    sp[:, c0:T], lhsT=Kall[:, kb * 128:(kb + 1) * 128], rhs=Qg[gb][:, c0:T], start=True, stop=True),
                    reads=[tag + "Kall", (tag + "Qg", gb)], writes=[kS])
                P.act(lambda e, sp=sp, pt=pt, c0=c0: e.activation(out=pt[:, c0:T], in_=sp[:, c0:T], func=AF.Exp,
                                                                  scale=SC), reads=[kS], writes=[kP])
                if j >= 0:
                    P.pool(lambda e, pt=pt, c0=c0: e.memset(pt[64:128, c0:c0 + 64], 0.0), reads=[kP], writes=[kP])
                P.pe(lambda e, pt=pt, kb=kb, gb=gb, c0=c0, nkb=nkb: e.matmul(
                    O_ps[gb][0:65, c0:T], lhsT=Vall[:, kb, :], rhs=pt[:, c0:T], start=(kb == 0), stop=(kb == nkb - 1)),
                    reads=[tag + "Vall", kP], writes=[(tag + "O_ps", gb)])
            P.dve(lambda e, gb=gb: e.tensor_copy(out=Ost[gb][:], in_=O_ps[gb][0:65, :]),
                  reads=[(tag + "O_ps", gb)], writes=[(tag + "Ost", gb)])
            P.dma(OD[h, :, qg * T:(qg + 1) * T], Ost[gb][:], reads=[(tag + "Ost", gb)], writes=[("OD", qg, h)])

    Oa = [sb(nc, es, tag + f"Oa{i}", [128, 2, T], F32) for i in range(2)]
    La = [sb(nc, es, tag + f"La{i}", [128, 2, T], F32) for i in range(2)]
    Ga = [sb(nc, es, tag + f"Ga{i}", [128, 2, T], F32) for i in range(2)]
    Yd = [sb(nc, es, tag + f"Yd{i}", [128, 2, T], BF16) for i in range(2)]
    yv = ycatT.rearrange("(c p) t -> p c t", p=128)
    for it in range(nt):
        ts = slice(it * T, (it + 1) * T)
        b = it % 2
        odk = [("OD", it, h) for h in range(4)]
        for h in range(4):
            P.dma(Oa[b][(h % 2) * 64:(h % 2) * 64 + 64, h // 2, :], OD[h, 0:64, ts], reads=odk, writes=[(tag + "Oa", b)])
            P.dma(La[b][(h % 2) * 64:(h % 2) * 64 + 64, h // 2, :], OD[h, 64:65, ts].partition_broadcast(64),
                  reads=odk, writes=[(tag + "La", b)])
        P.dma(Ga[b][:], pv[:, 25:27, ts], reads=[("projT", it)], writes=[(tag + "Ga", b)])
        P.act(lambda e, b=b: e.activation(out=Ga[b][:], in_=Ga[b][:], func=AF.Silu), reads=[(tag + "Ga", b)],
              writes=[(tag + "Ga", b)])
        P.dve(lambda e, b=b: e.reciprocal(out=La[b][:], in_=La[b][:]), reads=[(tag + "La", b)], writes=[(tag + "La", b)])
        P.dve(lambda e, b=b: e.tensor_tensor(out=Oa[b][:], in0=Oa[b][:], in1=La[b][:], op=ALU.mult),
              reads=[(tag + "Oa", b), (tag + "La", b)], writes=[(tag + "Oa", b)])
        P.dve(lambda e, b=b: e.tensor_tensor(out=Yd[b][:], in0=Oa[b][:], in1=Ga[b][:], op=ALU.mult),
              reads=[(tag + "Oa", b), (tag + "Ga", b)], writes=[(tag + "Yd", b)])
        P.dma(yv[:, 6:8, ts], Yd[b][:], reads=[(tag + "Yd", b)], writes=[("ycatD", it)])


def emit_lb(nc, P, es, V, LB, l):
    ex = sb(nc, es, "lb_ex", [128, 2, 4], F32)
    sm = sb(nc, es, "lb_sm", [128, 2], F32)
    o, w = VEC_OFF["b_lb"]
    lg = V[:, o:o + 8].rearrange("p (c l) -> p c l", c=2)
    P.act(lambda e: e.activation(out=ex[:], in_=lg, func=AF.Exp), reads=["V"], writes=["lb_ex"])
    P.dve(lambda e: e.tensor_reduce(out=sm[:], in_=ex[:], axis=AX.X, op=ALU.add), reads=["lb_ex"], writes=["lb_sm"])
    P.dve(lambda e: e.reciprocal(out=sm[:], in_=sm[:]), reads=["lb_sm"], writes=["lb_sm"])
    P.pool(lambda e: e.memset(LB[:, 0, :], 0.0), writes=["LB"])
    for j in range(1, l + 1):
        P.dve(lambda e, j=j: e.tensor_tensor(out=LB[:, 0, :], in0=LB[:, 0, :], in1=ex[:, :, j], op=ALU.add),
              reads=["LB", "lb_ex"], writes=["LB"])
    P.dve(lambda e: e.tensor_tensor(out=LB[:, 0, :], in0=LB[:, 0, :], in1=sm[:], op=ALU.mult),
          reads=["LB", "lb_sm"], writes=["LB"])
    P.dve(lambda e: e.tensor_scalar(out=LB[:, 1, :], in0=LB[:, 0, :], scalar1=-1.0, scalar2=1.0, op0=ALU.mult,
                                    op1=ALU.add), reads=["LB"], writes=["LB"])
    P.dve(lambda e: e.tensor_scalar(out=LB[:, 2, :], in0=LB[:, 1, :], scalar1=-1.0, scalar2=None, op0=ALU.mult),
          reads=["LB"], writes=["LB"])


def emit_B(nc, P, es, projT, V, LB, KC, ycatT, ntok, tag="B"):
    nt = ntok // T
    ident, bd, rmask, rm, tri2 = KC["ident"], KC["bd"], KC["rmask"], KC["rm"], KC["tri2"]
    q = [sb(nc, es, tag + f"q{i}", [128, 2, T], F32) for i in range(2)]
    f = [sb(nc, es, tag + f"f{i}", [128, 2, T], F32) for i in range(2)]
    vi = [sb(nc, es, tag + f"vi{i}", [128, 2, T], F32) for i in range(2)]
    g = [sb(nc, es, tag + f"g{i}", [128, 2, T], F32) for i in range(2)]
    kk = sb(nc, es, tag + "kk", [128, 2, T], F32)
    bb = sb(nc, es, tag + "bb", [128, 2, T], F32)
    t1 = sb(nc, es, tag + "t1", [128, 2, T], F32)
    t4 = sb(nc, es, tag + "t4", [128, 2, T], F32)
    e3 = sb(nc, es, tag + "e3", [128, 2, T], F32)
    ee = sb(nc, es, tag + "ee", [128, 2, T], F32)
    qt = sb(nc, es, tag + "qt", [128, 2, T], BF16)
    ktl = sb(nc, es, tag + "ktl", [128, 2, T], BF16)
    qe = sb(nc, es, tag + "qe", [128, 2, T], BF16)
    kh = sb(nc, es, tag + "kh", [128, 2, T], BF16)
    vb = sb(nc, es, tag + "vb", [128, 2, T], BF16)
    khT = sb(nc, es, tag + "khT", [128, 4, 256], BF16)
    vT = sb(nc, es, tag + "vT", [128, 4, 256], BF16)
    Asb = [sb(nc, es, tag + f"Asb{i}", [128, 4, 128], BF16) for i in range(4)]
    qtm = [sb(nc, es, tag + f"qtm{i}", [128, 2, T], BF16) for i in range(2)]
    qem = [sb(nc, es, tag + f"qem{i}", [128, 2, T], BF16) for i in range(2)]
    vTm = [sb(nc, es, tag + f"vTm{i}", [128, 4, 256], BF16) for i in range(2)]
    S = sb(nc, es, tag + "S", [128, 128], F32)
    Sbf = sb(nc, es, tag + "Sbf", [128, 128], BF16)
    osq = sb(nc, es, tag + "osq", [128, 2, T], F32)
    rs = sb(nc, es, tag + "rs", [128, 2, T], F32)
    yb = [sb(nc, es, tag + f"yb{i}", [128, 2, T], BF16) for i in range(2)]
    pkT = [psb(P, tag + f"pkT{i}") for i in range(2)]
    pvT = [psb(P, tag + f"pvT{i}") for i in range(2)]
    pS = [psb(P, tag + f"pS{i}") for i in range(4)]
    pO = [psb(P, tag + f"pO{i}") for i in range(2)]
    pU = [psb(P, tag + f"pU{i}") for i in range(2)]

    P.pool(lambda e: e.memset(S[:], 0.0), writes=[tag + "S"])
    P.pool(lambda e: e.memset(Sbf[:], 0.0), writes=[tag + "Sbf"])
    pv = projT.rearrange("(c p) t -> p c t", p=128)
    yv = ycatT.rearrange("(c p) t -> p c t", p=128)
    nU = 0
    for it in range(nt):
        ts = slice(it * T, (it + 1) * T)
        b = it % 2
        P.dma(q[b][:], pv[:, 6:8, ts], reads=[("projT", it)], writes=[(tag + "q", b)])
        P.dma(f[b][:], pv[:, 8:10, ts], reads=[("projT", it)], writes=[(tag + "f", b)])
        P.dma(vi[b][:], pv[:, 10:12, ts], reads=[("projT", it)], writes=[(tag + "vi", b)])
        P.dma(g[b][:], pv[:, 12:14, ts], reads=[("projT", it)], writes=[(tag + "g", b)])
        fb, qb_, vib, gb_ = f[b], q[b], vi[b], g[b]
        kf, kq, kv, kg = (tag + "f", b), (tag + "q", b), (tag + "vi", b), (tag + "g", b)
        P.act(lambda e, fb=fb: e.activation(out=fb[:], in_=fb[:], func=AF.Sigmoid), reads=[kf], writes=[kf])
        for c in range(2):
            P.dve(lambda e, c=c, fb=fb: e.tensor_scalar(out=kk[:, c, :], in0=fb[:, c, :], scalar1=LB[:, 2, c:c + 1],
                                                        scalar2=LB[:, 1, c:c + 1], op0=ALU.mult, op1=ALU.add),
                  reads=[kf, "LB"], writes=[tag + "kk"])
            P.dve(lambda e, c=c, fb=fb: e.tensor_scalar(out=fb[:, c, :], in0=fb[:, c, :], scalar1=LB[:, 1, c:c + 1],
                                                        scalar2=LB[:, 0, c:c + 1], op0=ALU.mult, op1=ALU.add),
                  reads=[kf, "LB", tag + "kk"], writes=[kf])
        P.act(lambda e, fb=fb: e.activation(out=fb[:], in_=fb[:], func=AF.Ln), reads=[kf], writes=[kf])
        for c in range(2):
            P.dve(lambda e, c=c, fb=fb: e.tensor_tensor_scan(out=bb[:, c, :], data0=rmask[:], data1=fb[:, c, :],
                                                             initial=0.0, op0=ALU.mult, op1=ALU.add),
                  reads=[kf, "KC"], writes=[tag + "bb"])
        b4 = bb[:].rearrange("p c (n s) -> p c n s", s=64)
        mid = b4[:, :, :, 31:32].to_broadcast([128, 2, 8, 64])
        last = b4[:, :, :, 63:64].to_broadcast([128, 2, 8, 64])
        t14 = t1[:].rearrange("p c (n s) -> p c n s", s=64)
        t44 = t4[:].rearrange("p c (n s) -> p c n s", s=64)
        P.dve(lambda e: e.tensor_tensor(out=t14, in0=b4, in1=mid, op=ALU.subtract), reads=[tag + "bb"], writes=[tag + "t1"])
        P.dve(lambda e: e.tensor_tensor(out=t44, in0=last, in1=b4, op=ALU.subtract), reads=[tag + "bb"], writes=[tag + "t4"])
        P.act(lambda e: e.activation(out=e3[:], in_=bb[:], func=AF.Exp), reads=[tag + "bb"], writes=[tag + "e3"])
        P.dve(lambda e, qb_=qb_: e.tensor_tensor(out=qe[:], in0=qb_[:], in1=e3[:], op=ALU.mult),
              reads=[kq, tag + "e3"], writes=[tag + "qe"])
        P.act(lambda e: e.activation(out=ee[:], in_=t1[:], func=AF.Exp), reads=[tag + "t1"], writes=[tag + "ee"])
        P.dve(lambda e, qb_=qb_: e.tensor_tensor(out=qt[:], in0=qb_[:], in1=ee[:], op=ALU.mult),
              reads=[kq, tag + "ee"], writes=[tag + "qt"])
        P.act(lambda e: e.activation(out=ee[:], in_=t1[:], func=AF.Exp, scale=-1.0), reads=[tag + "t1", tag + "qt"],
              writes=[tag + "ee"])
        P.dve(lambda e: e.tensor_tensor(out=ktl[:], in0=kk[:], in1=ee[:], op=ALU.mult),
              reads=[tag + "kk", tag + "ee"], writes=[tag + "ktl"])
        P.act(lambda e: e.activation(out=t4[:], in_=t4[:], func=AF.Exp), reads=[tag + "t4"], writes=[tag + "t4"])
        P.dve(lambda e: e.tensor_tensor(out=kh[:], in0=kk[:], in1=t4[:], op=ALU.mult),
               reads=[tag + "kk", tag + "t4"], writes=[tag + "kh"])
        P.pool(lambda e, vib=vib: e.tensor_copy(out=vb[:], in_=vib[:]), reads=[kv], writes=[tag + "vb"])
        P.act(lambda e, gb_=gb_: e.activation(out=gb_[:], in_=gb_[:], func=AF.Silu), reads=[kg], writes=[kg])
        for (src, ksrc, pst, kps, dst, kdst) in ((kh, tag + "kh", pkT, tag + "pkT", khT, tag + "khT"),
                                                  (vb, tag + "vb", pvT, tag + "pvT", vT, tag + "vT")):
            for j in range(4):
                for c in range(2):
                    P.pe(lambda e, src=src, pst=pst, j=j, c=c: e.matmul(
                        pst[j // 2][:, (j % 2) * 256 + c * 128:(j % 2) * 256 + c * 128 + 128],
                        lhsT=src[:, c, j * 128:(j + 1) * 128], rhs=ident[:], start=True, stop=True),
                        reads=[ksrc, "KC"], writes=[(kps, j // 2)])
            for hf in range(2):
                eng = P.act if hf == 0 else P.dve
                if hf == 0:
                    P.act(lambda e, pst=pst, dst=dst: e.copy(out=dst[:, 0:2, :].rearrange("p j n -> p (j n)"), in_=pst[0][:]),
                          reads=[(kps, 0)], writes=[kdst])
                else:
                    P.dve(lambda e, pst=pst, dst=dst: e.tensor_copy(out=dst[:, 2:4, :].rearrange("p j n -> p (j n)"), in_=pst[1][:]),
                          reads=[(kps, 1)], writes=[kdst])
        for e_ in range(2):
            P.dve(lambda e, e_=e_: e.tensor_scalar(out=qtm[e_][:], in0=qt[:], scalar1=rm[:, e_:e_ + 1], scalar2=None,
                                                   op0=ALU.mult), reads=[tag + "qt", "KC"], writes=[(tag + "qtm", e_)])
            P.dve(lambda e, e_=e_: e.tensor_scalar(out=qem[e_][:], in0=qe[:], scalar1=rm[:, e_:e_ + 1], scalar2=None,
                                                   op0=ALU.mult), reads=[tag + "qe", "KC"], writes=[(tag + "qem", e_)])
            P.dve(lambda e, e_=e_: e.tensor_scalar(out=vTm[e_][:], in0=vT[:], scalar1=rm[:, e_:e_ + 1], scalar2=None,
                                                   op0=ALU.mult), reads=[tag + "vT", "KC"], writes=[(tag + "vTm", e_)])
        for j in range(4):
            blk = slice(j * 128, (j + 1) * 128)
            for h in range(4):
                c2, e_ = h // 2, h % 2
                P.pe(lambda e, j=j, h=h, c2=c2, e_=e_, blk=blk: e.matmul(
                    pS[j][:, h * 128:(h + 1) * 128], lhsT=ktl[:, c2, blk], rhs=qtm[e_][:, c2, blk], start=True, stop=True),
                    reads=[tag + "ktl", (tag + "qtm", e_)], writes=[(tag + "pS", j)])
            P.dve(lambda e, j=j: e.tensor_tensor(out=Asb[j][:], in0=pS[j][:].rearrange("p (h t) -> p h t", h=4),
                                                 in1=tri2.rearrange("p (o t) -> p o t", o=1).to_broadcast([128, 4, 128]),
                                                 op=ALU.mult),
                  reads=[(tag + "pS", j), "KC"], writes=[(tag + "Asb", j)])
        for j in range(4):
            blk = slice(j * 128, (j + 1) * 128)
            for h in range(4):
                c2, hp = h // 2, (h % 2) * 64
                P.pe(lambda e, j=j, h=h, c2=c2, hp=hp, blk=blk: e.matmul(
                    pO[c2][hp:hp + 64, blk], lhsT=vT[:, j, h * 64:(h + 1) * 64], rhs=Asb[j][:, h, :],
                    start=True, stop=False), reads=[tag + "vT", (tag + "Asb", j)], writes=[(tag + "pO", c2)])
            for par in range(2):
                c = 2 * j + par
                cols = slice(c * 64, c * 64 + 64)
                ub = nU % 2
                nU += 1
                for h in range(4):
                    c2, hp, e_ = h // 2, (h % 2) * 64, h % 2
                    P.pe(lambda e, c2=c2, hp=hp, e_=e_, cols=cols, par=par: e.matmul(
                        pO[c2][hp:hp + 64, cols], lhsT=Sbf[:, c2 * 64:(c2 + 1) * 64], rhs=qem[e_][:, c2, cols],
                        start=False, stop=(par == 1)), reads=[tag + "Sbf", (tag + "qem", e_)], writes=[(tag + "pO", c2)])
                for h in range(4):
                    c2, hp = h // 2, (h % 2) * 64
                    P.pe(lambda e, c2=c2, hp=hp, j=j, h=h, ub=ub, par=par: e.matmul(
                        pU[ub][hp:hp + 64, c2 * 64:(c2 + 1) * 64], lhsT=khT[:, j, h * 64:(h + 1) * 64],
                        rhs=vTm[par][:, j, h * 64:(h + 1) * 64], start=True, stop=True),
                        reads=[tag + "khT", (tag + "vTm", par)], writes=[(tag + "pU", ub)])
                for c2 in range(2):
                    P.dve(lambda e, c2=c2, ub=ub, c=c: e.scalar_tensor_tensor(
                        out=S[:, c2 * 64:(c2 + 1) * 64], in0=S[:, c2 * 64:(c2 + 1) * 64],
                        scalar=e3[:, c2, c * 64 + 63:c * 64 + 64], in1=pU[ub][:, c2 * 64:(c2 + 1) * 64],
                        op0=ALU.mult, op1=ALU.add), reads=[tag + "S", tag + "e3", (tag + "pU", ub)], writes=[tag + "S"])
                P.act(lambda e: e.copy(out=Sbf[:], in_=S[:]), reads=[tag + "S"], writes=[tag + "Sbf"])
        for c2 in range(2):
            P.act(lambda e, c2=c2: e.activation(out=osq[:, c2, :], in_=pO[c2][:], func=AF.Square),
                  reads=[(tag + "pO", c2)], writes=[(tag + "osq", c2)])
            P.pe(lambda e, c2=c2: e.matmul(pS[c2][:], lhsT=bd[:], rhs=osq[:, c2, :], start=True, stop=True),
                 reads=["KC", (tag + "osq", c2)], writes=[(tag + "pS", c2)])
            P.dve(lambda e, c2=c2: e.tensor_scalar(out=rs[:, c2, :], in0=pS[c2][:], scalar1=1.0 / 64, scalar2=1e-6,
                                                   op0=ALU.mult, op1=ALU.add), reads=[(tag + "pS", c2)], writes=[(tag + "rs", c2)])
            P.act(lambda e, c2=c2: e.activation(out=rs[:, c2, :], in_=rs[:, c2, :], func=AF.Sqrt),
                  reads=[(tag + "rs", c2)], writes=[(tag + "rs", c2)])
            P.dve(lambda e, c2=c2: e.reciprocal(out=rs[:, c2, :], in_=rs[:, c2, :]), reads=[(tag + "rs", c2)],
                  writes=[(tag + "rs", c2)])
            P.dve(lambda e, c2=c2: e.scalar_tensor_tensor(out=rs[:, c2, :], in0=rs[:, c2, :], scalar=vcol(V, "b_norm_g", c2),
                                                          in1=pO[c2][:], op0=ALU.mult, op1=ALU.mult),
                  reads=[(tag + "rs", c2), (tag + "pO", c2), "V"], writes=[(tag + "rs", c2)])
            P.dve(lambda e, c2=c2, b=b, gb_=gb_: e.tensor_tensor(out=yb[b][:, c2, :], in0=rs[:, c2, :], in1=gb_[:, c2, :],
                                                                 op=ALU.mult),
                  reads=[(tag + "rs", c2), kg], writes=[(tag + "yb", b)])
        P.dma(yv[:, 2:4, ts], yb[b][:], reads=[(tag + "yb", b)], writes=[("ycatB", it)])


def make_kc_host():
    ident = np.eye(128, dtype=np.float32)
    bd = np.kron(np.eye(2, dtype=np.float32), np.ones((64, 64), np.float32))
    s = np.arange(128) % 64
    tri = (s[:, None] <= np.arange(64)[None, :]).astype(np.float32)
    rmask = np.ones((128, T), np.float32)
    rmask[:, ::64] = 0.0
    rm = np.zeros((128, 2), np.float32)
    rm[0:64, 0] = 1.0
    rm[64:128, 1] = 1.0
    pp = np.arange(128)
    tri2 = ((pp[:, None] // 64 == pp[None, :] // 64) & (pp[:, None] % 64 <= pp[None, :] % 64)).astype(np.float32)
    return np.ascontiguousarray(np.concatenate([ident, bd, tri, rmask, rm, tri2], axis=1))


def load_kc(nc, P, es, d_kc):
    raw = sb(nc, es, "kc_raw", [128, 962], F32)
    ident = sb(nc, es, "kc_ident", [128, 128], BF16)
    P.dma(raw[:], d_kc[:, :], writes=["KC"])
    P.dve(lambda e: e.tensor_copy(out=ident[:], in_=raw[:, 0:128]), reads=["KC"], writes=["KC"])
    return {"ident": ident, "bd": raw[:, 128:256], "tri": raw[:, 256:320], "rmask": raw[:, 320:832], "rm": raw[:, 832:834],
            "tri2": raw[:, 834:962], "raw": raw}


def make_kc2_host():
    j = np.arange(128)
    su = (j[:, None] > j[None, :]).astype(np.float32)
    lt = (j[:, None] <= j[None, :]).astype(np.float32)
    negi = (-30000.0 * np.eye(128)).astype(np.float32)
    ones = np.ones((128, 128), np.float32)
    return np.ascontiguousarray(np.concatenate([su, lt, su, negi, ones, np.eye(128, dtype=np.float32)], axis=1))


class _Stop(Exception):
    pass


def _ck(k):
    import os
    if os.environ.get("CSTOP") == str(k):
        raise _Stop()


def emit_C(nc, P, es, projT, V, KC, KC2, ycatT, ntok, tag="C"):
    nt = ntok // T
    ident = KC["ident"]
    SU, LT, GT, NEGI, ONES, IDF = (KC2[:, i * 128:(i + 1) * 128] for i in range(6))
    xb = sb(nc, es, tag + "xb", [128, 6, 4 + T], F32)
    xc = sb(nc, es, tag + "xc", [128, 6, T], F32)
    bcb = sb(nc, es, tag + "bcb", [128, 6, T], BF16)
    z = [sb(nc, es, tag + f"z{i}", [128, 2, T], F32) for i in range(2)]
    dta = sb(nc, es, tag + "dta", [128, T], F32)
    negA = sb(nc, es, tag + "negA", [128, 1], F32)
    atm = sb(nc, es, tag + "atm", [128, 16], F32)
    d2a = sb(nc, es, tag + "d2a", [128, 4, 4], F32)
    datm = sb(nc, es, tag + "datm", [128, 4, 8], F32)
    Lh = sb(nc, es, tag + "Lh", [128, 4, 128], F32)
    AB = sb(nc, es, tag + "AB", [128, 4, 128], F32)
    Lm = sb(nc, es, tag + "Lm", [128, 4, 128], F32)
    Sc = sb(nc, es, tag + "Sc", [128, 4, 128], BF16)
    DEC = sb(nc, es, tag + "DEC", [128, 2, T], F32)
    dl = sb(nc, es, tag + "dl", [128, 4], F32)
    d2 = sb(nc, es, tag + "d2", [128, 4], F32)
    xdtf = sb(nc, es, tag + "xdtf", [128, 256], F32)
    xdtb = sb(nc, es, tag + "xdtb", [128, 256], BF16)
    wxb = sb(nc, es, tag + "wxb", [128, 256], BF16)
    Btm = sb(nc, es, tag + "Btm", [128, 256], BF16)
    st = sb(nc, es, tag + "st", [128, 4, 64], F32)
    stb = sb(nc, es, tag + "stb", [128, 4, 64], BF16)
    yt = sb(nc, es, tag + "yt", [128, 2, T], F32)
    ysq = sb(nc, es, tag + "ysq", [128, 2, T], F32)
    rs = sb(nc, es, tag + "rs", [128, 2, T], F32)
    yc = [sb(nc, es, tag + f"yc{i}", [128, 2, T], BF16) for i in range(2)]
    p_da = psb(P, tag + "p_da")
    p_seg = psb(P, tag + "p_seg")
    p_acs = psb(P, tag + "p_acs")
    p_m1 = psb(P, tag + "p_m1")
    p_m2 = psb(P, tag + "p_m2")
    p_y = [psb(P, tag + f"p_y{i}") for i in range(2)]
    p_ss = psb(P, tag + "p_ss")

    P.pool(lambda e: e.memset(xb[:, :, 0:4], 0.0), writes=[tag + "xb"])
    P.pool(lambda e: e.memset(st[:], 0.0), writes=[tag + "st"])
    P.pool(lambda e: e.memset(stb[:], 0.0), writes=[tag + "stb"])
    P.pool(lambda e: e.memset(dta[:], 0.0), writes=[tag + "dta"])
    P.act(lambda e: e.activation(out=negA[:], in_=vcol(V, "c_a_log", rows=128), func=AF.Exp), reads=["V"], writes=[tag + "negA"])
    P.dve(lambda e: e.tensor_scalar(out=negA[:], in0=negA[:], scalar1=-1.0, scalar2=None, op0=ALU.mult),
          reads=[tag + "negA"], writes=[tag + "negA"])
    pv = projT.rearrange("(c p) t -> p c t", p=128)
    yv = ycatT.rearrange("(c p) t -> p c t", p=128)
    ny = 0
    for it in range(nt):
        ts = slice(it * T, (it + 1) * T)
        b = it % 2
        P.dma(xb[:, :, 4:4 + T], pv[:, 14:20, ts], reads=[("projT", it)], writes=[tag + "xb"])
        P.dma(z[b][:], pv[:, 20:22, ts], reads=[("projT", it)], writes=[(tag + "z", b)])
        P.dma(dta[0:4, :], projT[R_DT:R_DT + 4, ts], reads=[("projT", it)], writes=[tag + "dta"])
        P.dma(dta[64:68, :], projT[R_DT:R_DT + 4, ts], reads=[("projT", it)], writes=[tag + "dta"])
        for c in range(6):
            eng = P.dve
            for j in range(4):
                src = xb[:, c, 1 + j:1 + j + T]
                wj = vcol(V, "c_conv_w", c * 4 + j)
                if j == 0:
                    eng(lambda e, c=c, src=src, wj=wj: e.tensor_scalar(out=xc[:, c, :], in0=src, scalar1=wj,
                                                                      scalar2=vcol(V, "c_conv_b", c), op0=ALU.mult, op1=ALU.add),
                        reads=[tag + "xb", "V"], writes=[(tag + "xc", c)])
                else:
                    eng(lambda e, c=c, src=src, wj=wj: e.scalar_tensor_tensor(out=xc[:, c, :], in0=src, scalar=wj,
                                                                             in1=xc[:, c, :], op0=ALU.mult, op1=ALU.add),
                        reads=[tag + "xb", "V", (tag + "xc", c)], writes=[(tag + "xc", c)])
        allxc = [(tag + "xc", c) for c in range(6)]
        P.act(lambda e: e.copy(out=xb[:, :, 0:4], in_=xb[:, :, T:T + 4]), reads=[tag + "xb"] + allxc, writes=[tag + "xb"])
        P.act(lambda e: e.activation(out=xc[:], in_=xc[:], func=AF.Silu), reads=allxc, writes=allxc)
        P.dve(lambda e: e.tensor_copy(out=bcb[:], in_=xc[:]), reads=allxc, writes=[tag + "bcb"])
        P.act(lambda e, b=b: e.activation(out=z[b][:], in_=z[b][:], func=AF.Silu), reads=[(tag + "z", b)], writes=[(tag + "z", b)])
        _ck(1)
        P.act(lambda e: e.activation(out=dta[:], in_=dta[:], func=AF.Exp, bias=vcol(V, "c_dt_bias", rows=128)),
              reads=[tag + "dta", "V"], writes=[tag + "dta"])
        P.dve(lambda e: e.tensor_scalar(out=dta[:], in0=dta[:], scalar1=1.0, scalar2=None, op0=ALU.add),
              reads=[tag + "dta"], writes=[tag + "dta"])
        P.act(lambda e: e.activation(out=dta[:], in_=dta[:], func=AF.Ln), reads=[tag + "dta"], writes=[tag + "dta"])
        P.dve(lambda e: e.tensor_scalar(out=dta[64:128, :], in0=dta[64:128, :], scalar1=negA[64:128, 0:1], scalar2=None,
                                        op0=ALU.mult), reads=[tag + "dta", tag + "negA"], writes=[tag + "dta"])
        for j in range(4):
            P.pe(lambda e, j=j: e.matmul(p_da[:, j * 128:(j + 1) * 128], lhsT=dta[:, j * 128:(j + 1) * 128], rhs=IDF,
                                         start=True, stop=True), reads=[tag + "dta", "KC2"], writes=[tag + "p_da"])
        for j in range(4):
            P.dve(lambda e, j=j: e.tensor_copy(out=datm[:, j, 0:4], in_=p_da[:, j * 128:j * 128 + 4]),
                  reads=[tag + "p_da"], writes=[tag + "datm"])
            P.dve(lambda e, j=j: e.tensor_copy(out=datm[:, j, 4:8], in_=p_da[:, j * 128 + 64:j * 128 + 68]),
                  reads=[tag + "p_da"], writes=[tag + "datm"])
            P.dve(lambda e, j=j: e.tensor_copy(out=atm[:, j * 4:j * 4 + 4], in_=p_da[:, j * 128 + 64:j * 128 + 68]),
                  reads=[tag + "p_da"], writes=[tag + "atm"])
        P.pe(lambda e: e.matmul(p_da[:, 0:16], lhsT=GT, rhs=atm[:], start=True, stop=True),
             reads=["KC2", tag + "atm"], writes=[tag + "p_da"])
        P.act(lambda e: e.activation(out=d2a[:].rearrange("p j h -> p (j h)"), in_=p_da[:, 0:16], func=AF.Exp),
              reads=[tag + "p_da"], writes=[tag + "d2a"])
        _ck(2)
        for j in range(4):
            blk = slice(j * 128, (j + 1) * 128)
            yb_ = ny % 2
            ny += 1
            py = p_y[yb_]
            kpy = (tag + "p_y", yb_)
            for h in range(4):
                asc = datm[:, j, 4 + h:5 + h]
                P.dve(lambda e, h=h, asc=asc: e.tensor_scalar(out=Lh[:, h, :], in0=SU, scalar1=asc, scalar2=None, op0=ALU.mult),
                      reads=[tag + "datm", "KC2"], writes=[(tag + "Lh", h)])
                P.dve(lambda e, h=h, asc=asc: e.tensor_scalar(out=AB[:, h, :], in0=ONES, scalar1=asc, scalar2=None, op0=ALU.mult),
                       reads=[tag + "datm", "KC2"], writes=[(tag + "AB", h)])
            for h in range(4):
                P.pe(lambda e, h=h: e.matmul(p_seg[:, h * 128:(h + 1) * 128], lhsT=Lh[:, h, :], rhs=LT, start=True, stop=False),
                     reads=[(tag + "Lh", h), "KC2"], writes=[tag + "p_seg"])
                P.pe(lambda e, h=h: e.matmul(p_seg[:, h * 128:(h + 1) * 128], lhsT=NEGI, rhs=GT, start=False, stop=True),
                     reads=["KC2"], writes=[tag + "p_seg"])
            for h in range(4):
                P.pe(lambda e, h=h: e.matmul(p_acs[:, h * 128:(h + 1) * 128], lhsT=AB[:, h, :], rhs=LT, start=True, stop=True),
                     reads=[(tag + "AB", h), "KC2"], writes=[tag + "p_acs"])
            P.act(lambda e: e.activation(out=Lm[:].rearrange("p h l -> p (h l)"), in_=p_seg[:], func=AF.Exp),
                  reads=[tag + "p_seg"], writes=[tag + "Lm"])
            for h in range(4):
                P.act(lambda e, h=h: e.activation(out=dl[:, h:h + 1], in_=p_acs[:, h * 128 + 127:h * 128 + 128], func=AF.Exp),
                      reads=[tag + "p_acs"], writes=[tag + "dl"])
            for h in range(4):
                g_, hp = h // 2, (h % 2) * 64
                P.act(lambda e, h=h, g_=g_, hp=hp, blk=blk: e.activation(out=DEC[hp:hp + 64, g_, blk],
                                                                        in_=p_acs[hp:hp + 64, h * 128:(h + 1) * 128], func=AF.Exp),
                      reads=[tag + "p_acs"], writes=[tag + "DEC"])
            _ck(3)
            for g_ in range(2):
                P.pe(lambda e, g_=g_, blk=blk: e.matmul(p_m1[:, g_ * 128:(g_ + 1) * 128], lhsT=bcb[:, g_, blk], rhs=ident[:],
                                                         start=True, stop=True), reads=[tag + "bcb", "KC"], writes=[tag + "p_m1"])
                P.pe(lambda e, g_=g_, blk=blk: e.matmul(p_m1[:, 256 + g_ * 128:256 + (g_ + 1) * 128], lhsT=bcb[:, 2 + g_, blk],
                                                         rhs=ident[:], start=True, stop=True), reads=[tag + "bcb", "KC"], writes=[tag + "p_m1"])
            for h in range(4):
                P.dve(lambda e, h=h, j=j: e.tensor_scalar(out=xdtf[:, h * 64:(h + 1) * 64], in0=p_m1[:, h * 64:(h + 1) * 64],
                                                          scalar1=datm[:, j, h:h + 1], scalar2=None, op0=ALU.mult),
                      reads=[tag + "p_m1", tag + "datm"], writes=[tag + "xdtf"])
            P.act(lambda e: e.copy(out=Btm[:], in_=p_m1[:, 256:512]), reads=[tag + "p_m1"], writes=[tag + "Btm"])
            P.act(lambda e: e.copy(out=xdtb[:], in_=xdtf[:]), reads=[tag + "xdtf"], writes=[tag + "xdtb"])
            for h in range(4):
                P.dve(lambda e, h=h, j=j: e.tensor_scalar(out=wxb[:, h * 64:(h + 1) * 64], in0=xdtf[:, h * 64:(h + 1) * 64],
                                                          scalar1=d2a[:, j, h:h + 1], scalar2=None, op0=ALU.mult),
                      reads=[tag + "xdtf", tag + "d2a"], writes=[tag + "wxb"])
            _ck(4)
            for g_ in range(2):
                P.pe(lambda e, g_=g_, blk=blk: e.matmul(p_m2[:, g_ * 128:(g_ + 1) * 128], lhsT=bcb[:, 2 + g_, blk],
                                                         rhs=bcb[:, 4 + g_, blk], start=True, stop=True),
                     reads=[tag + "bcb"], writes=[tag + "p_m2"])
            _ck(40)
            for h in range(4):
                g_ = h // 2
                P.dve(lambda e, h=h, g_=g_: e.tensor_tensor(out=Sc[:, h, :], in0=p_m2[:, g_ * 128:(g_ + 1) * 128], in1=Lm[:, h, :],
                                                            op=ALU.mult), reads=[tag + "p_m2", tag + "Lm"], writes=[(tag + "Sc", h)])
            _ck(41)
            for h in range(4):
                g_, hp = h // 2, (h % 2) * 64
                P.pe(lambda e, h=h, g_=g_, hp=hp, py=py: e.matmul(py[hp:hp + 64, g_ * 128:(g_ + 1) * 128],
                                                                  lhsT=xdtb[:, h * 64:(h + 1) * 64], rhs=Sc[:, h, :],
                                                                  start=True, stop=True),
                     reads=[tag + "xdtb", (tag + "Sc", h)], writes=[kpy])
                P.pe(lambda e, h=h, g_=g_, hp=hp, py=py, blk=blk: e.matmul(py[hp:hp + 64, 256 + g_ * 128:256 + (g_ + 1) * 128],
                                                                           lhsT=stb[:, h, :], rhs=bcb[:, 4 + g_, blk],
                                                                           start=True, stop=True),
                     reads=[tag + "stb", tag + "bcb"], writes=[kpy])
            _ck(42)
            for h in range(4):
                g_ = h // 2
                P.pe(lambda e, h=h, g_=g_: e.matmul(p_m2[:, 256 + h * 64:256 + (h + 1) * 64], lhsT=Btm[:, g_ * 128:(g_ + 1) * 128],
                                                    rhs=wxb[:, h * 64:(h + 1) * 64], start=True, stop=True),
                     reads=[tag + "Btm", tag + "wxb"], writes=[tag + "p_m2"])
            _ck(43)
            for h in range(4):
                P.dve(lambda e, h=h: e.scalar_tensor_tensor(out=st[:, h, :], in0=st[:, h, :], scalar=dl[:, h:h + 1],
                                                            in1=p_m2[:, 256 + h * 64:256 + (h + 1) * 64], op0=ALU.mult, op1=ALU.add),
                      reads=[tag + "st", tag + "dl", tag + "p_m2"], writes=[tag + "st"])
            P.act(lambda e: e.copy(out=stb[:], in_=st[:]), reads=[tag + "st"], writes=[tag + "stb"])
            _ck(5)
            yt3 = yt[:, :, blk]
            P.dve(lambda e, py=py, blk=blk, yt3=yt3: e.tensor_tensor(out=yt3, in0=py[:, 256:512].rearrange("p (g l) -> p g l", g=2),
                                                                     in1=DEC[:, :, blk], op=ALU.mult),
                  reads=[kpy, tag + "DEC"], writes=[tag + "yt"])
            P.dve(lambda e, py=py, yt3=yt3: e.tensor_tensor(out=yt3, in0=py[:, 0:256].rearrange("p (g l) -> p g l", g=2),
                                                            in1=yt3, op=ALU.add), reads=[kpy, tag + "yt"], writes=[tag + "yt"])
        _ck(6)
        for g_ in range(2):
            P.dve(lambda e, g_=g_: e.scalar_tensor_tensor(out=yt[:, g_, :], in0=xc[:, g_, :], scalar=vcol(V, "c_d", g_),
                                                          in1=yt[:, g_, :], op0=ALU.mult, op1=ALU.add),
                  reads=[(tag + "xc", g_), "V", tag + "yt"], writes=[tag + "yt"])
        P.dve(lambda e, b=b: e.tensor_tensor(out=yt[:], in0=yt[:], in1=z[b][:], op=ALU.mult),
              reads=[tag + "yt", (tag + "z", b)], writes=[tag + "yt"])
        P.act(lambda e: e.activation(out=ysq[:], in_=yt[:], func=AF.Square), reads=[tag + "yt"], writes=[tag + "ysq"])
        for g_ in range(2):
            P.pe(lambda e, g_=g_: e.matmul(p_ss[:], lhsT=ONES, rhs=ysq[:, g_, :], start=True, stop=True),
                 reads=["KC2", tag + "ysq"], writes=[tag + "p_ss"])
            P.dve(lambda e, g_=g_: e.tensor_scalar(out=rs[:, g_, :], in0=p_ss[:], scalar1=1.0 / 128, scalar2=1e-6,
                                                   op0=ALU.mult, op1=ALU.add), reads=[tag + "p_ss"], writes=[(tag + "rs", g_)])
            P.act(lambda e, g_=g_: e.activation(out=rs[:, g_, :], in_=rs[:, g_, :], func=AF.Sqrt), reads=[(tag + "rs", g_)],
                  writes=[(tag + "rs", g_)])
            P.dve(lambda e, g_=g_: e.reciprocal(out=rs[:, g_, :], in_=rs[:, g_, :]), reads=[(tag + "rs", g_)], writes=[(tag + "rs", g_)])
            P.dve(lambda e, g_=g_, b=b: e.scalar_tensor_tensor(out=yc[b][:, g_, :], in0=yt[:, g_, :], scalar=vcol(V, "c_norm_g", g_),
                                                               in1=rs[:, g_, :], op0=ALU.mult, op1=ALU.mult),
                  reads=[tag + "yt", (tag + "rs", g_), "V"], writes=[(tag + "yc", b)])
        P.dma(yv[:, 4:6, ts], yc[b][:], reads=[(tag + "yc", b)], writes=[("ycatC", it)])


def build_full(ntok=SEQ, depth=DEPTH):
    nc = bass.Bass("TRN2", target_bir_lowering=False)
    di = lambda name, shape, dt=F32: nc.dram_tensor(name, list(shape), dt, kind="ExternalInput").ap()
    dn = lambda name, shape, dt=F32: nc.dram_tensor(name, list(shape), dt, kind="Internal").ap()
    xT = di("xT", [D_MODEL, ntok])
    pos = di("pos", [1, ntok], I32)
    Vd = di("vecs", [depth, 128, NV])
    w_in = di("w_in", [depth, D_MODEL, NPROJ])
    w_out = di("w_out", [depth, D_MODEL, D_MODEL])
    pw = di("a_pw", [depth, 256, 256])
    qbw = di("qbw", [depth, 256, 768])
    kvbw = di("kvbw", [depth, 128, 512])
    cp_d = di("cp", [128, NCP])
    kc_d = di("kc", [128, 962])
    kc2_d = di("kc2", [128, 768])
    xTo = nc.dram_tensor("xTo", [D_MODEL, ntok], F32, kind="ExternalOutput").ap()
    projT = dn("projT", [NPROJ, ntok])
    ycatT = dn("ycatT", [D_MODEL, ntok], BF16)
    xbuf = [dn("xbuf0", [D_MODEL, ntok]), dn("xbuf1", [D_MODEL, ntok])]
    QT = dn("QT", [4, 96, ntok], BF16)
    KT = dn("KT", [4, 96, ntok], BF16)
    VD = dn("VD", [4, 128, ntok // 128, 65], BF16)
    OD = dn("OD", [4, 65, ntok])
    with ExitStack() as es:
        P = Prog(nc, es)
        Vs = [sb(nc, es, f"V_sb{l}", [128, NV], F32) for l in range(depth)]
        CP = sb(nc, es, "CP_sb", [128, NCP], F32)
        KC2 = sb(nc, es, "KC2_sb", [128, 768], F32)
        LB = sb(nc, es, "LB_sb", [128, 3, 2], F32)
        for l in range(depth):
            P.dma(Vs[l][:], Vd[l], writes=["V"])
        P.dma(CP[:], cp_d[:, :], writes=["CP"])
        P.dma(KC2[:], kc2_d[:, :], writes=["KC2"])
        KC = load_kc(nc, P, es, kc_d)
        P.phase_end()
        for l in range(depth):
            V = Vs[l]
            x_in = xT if l == 0 else xbuf[(l - 1) % 2]
            x_out = xTo if l == depth - 1 else xbuf[l % 2]
            with ExitStack() as pes:
                emit_lb(nc, P, pes, V, LB, l)
                emit_l1(nc, P, pes, x_in, w_in[l], V, projT, ntok=ntok)
                P.phase_end()
            with ExitStack() as pes:
                emit_A(nc, P, pes, projT, V, pw[l], ycatT, ntok)
                P.phase_end()
            with ExitStack() as pes:
                emit_B(nc, P, pes, projT, V, LB, KC, ycatT, ntok)
                P.phase_end()
            with ExitStack() as pes:
                emit_C(nc, P, pes, projT, V, KC, KC2, ycatT, ntok)
                P.phase_end()
            with ExitStack() as pes:
                emit_D(nc, P, pes, projT, pos, V, CP, qbw[l], kvbw[l], QT, KT, VD, OD, ycatT, ntok)
                P.phase_end()
            with ExitStack() as pes:
                emit_O(nc, P, pes, ycatT, x_in, V, w_out[l], x_out, ntok)
                P.phase_end()
    return nc


def make_in_map(x_b, pos_b, p, depth=DEPTH):
    return {
        "xT": np.ascontiguousarray(np.asarray(x_b, np.float32).T),
        "pos": np.ascontiguousarray(np.asarray(pos_b, np.int32).reshape(1, -1)),
        "vecs": np.stack([pack_vecs(p, l) for l in range(depth)]),
        "w_in": np.stack([pack_w_in(np.asarray(p["w_in"][l], np.float32)) for l in range(depth)]),
        "w_out": np.ascontiguousarray(np.asarray(p["w_out"], np.float32)[:depth]),
        "a_pw": np.ascontiguousarray(np.asarray(p["a_pw_w"], np.float32)[:depth]),
        "qbw": np.stack([pack_qb(p["d_qb_w"][l]).reshape(256, 768) for l in range(depth)]),
        "kvbw": np.stack([pack_kvb(p["d_kvb_w"][l]).reshape(128, 512) for l in range(depth)]),
        "cp": make_cp(), "kc": make_kc_host(), "kc2": make_kc2_host(),
    }


def kernel(**inputs):
    x = np.asarray(inputs["x"], np.float32)
    positions = np.asarray(inputs["positions"], np.int32)
    p = {k: np.asarray(v) for k, v in inputs.items() if k not in ("x", "positions")}
    nb, ntok, _ = x.shape
    nc = build_full(ntok, DEPTH)
    in_maps = [make_in_map(x[b], positions[b], p) for b in range(nb)]
    res = run_bass_kernel_spmd(nc, in_maps, core_ids=list(range(nb)))
    out = np.stack([np.ascontiguousarray(res.results[b]["xTo"].T) for b in range(nb)])
    return out.astype(np.float32)
```

```python
import numpy as np
from contextlib import ExitStack
import concourse.bass as bass
import concourse.mybir as mybir
from concourse.bass_utils import run_bass_kernel_spmd

F32 = mybir.dt.float32
BF16 = mybir.dt.bfloat16
I32 = mybir.dt.int32
AF = mybir.ActivationFunctionType
ALU = mybir.AluOpType
AX = mybir.AxisListType

D_MODEL = 1024
SEQ = 16384
DEPTH = 4
NCORE = 8
SEG = 2048
NSEG = 8
TOK = 4096
T = 512
NT = TOK // T
NPROJ = 3584
SEM_ROT = 24000


class Op:
    __slots__ = ("eng", "fn", "waits", "sem", "val", "dma", "idx")


class Prog:
    ENGS = ("pe", "act", "dve", "pool", "sp")

    def __init__(self, nc, es, ndma_sems=14):
        self.nc = nc
        self.es = es
        self.ops = {e: [] for e in self.ENGS}
        self.count = {e: 0 for e in self.ENGS}
        self.last_op = {e: None for e in self.ENGS}
        self.last_w = {}
        self.readers = {}
        self.waited = {e: {} for e in self.ENGS}
        self.eng_sems = {e: [] for e in self.ENGS}
        self.dsems = [es.enter_context(nc.semaphore(f"dma{i}")) for i in range(ndma_sems)]
        self.ndma = 0
        self.dma_ops = []
        self.banks = [es.enter_context(nc.psum_tensor(f"psbank{i}", [128, T], F32)) for i in range(8)]
        self.alias = {}
        self.nbank = 0

    def ps_reset(self):
        self.nbank = 0

    def bank(self, name):
        k = self.nbank % 8
        self.nbank += 1
        self.alias[name] = ("PS", k)
        return self.banks[k]

    def canon(self, k):
        if isinstance(k, str):
            return self.alias.get(k, k)
        if isinstance(k, tuple) and len(k) == 2 and isinstance(k[0], str):
            return self.alias.get(k[0] + str(k[1]), k)
        return k

    def _eng_sem(self, eng, k):
        lst = self.eng_sems[eng]
        while len(lst) <= k:
            lst.append(self.es.enter_context(self.nc.semaphore(f"s_{eng}{len(lst)}")))
        return lst[k]

    def _need(self, op, d, raw):
        if d is None or d is op:
            return
        if not d.dma and d.eng == op.eng:
            if op.eng == "pe" or op.eng == "sp":
                return
        key = id(d.sem)
        if self.waited[op.eng].get(key, 0) >= d.val:
            return
        self.waited[op.eng][key] = d.val
        op.waits.append((d.sem, d.val))

    def add(self, eng, fn, reads=(), writes=(), dma=False):
        op = Op()
        op.eng, op.fn, op.waits, op.dma = eng, fn, [], dma
        op.idx = self.count[eng]
        self.count[eng] += 1
        reads = [self.canon(k) for k in reads]
        writes = [self.canon(k) for k in writes]
        if dma:
            n = self.ndma
            R = len(self.dsems)
            op.sem = self.dsems[n % R]
            op.val = 16 * (n // R + 1)
            if n >= R:
                prev = self.dma_ops[n - R]
                self._need(op, prev, True)
            self.ndma += 1
            self.dma_ops.append(op)
        else:
            k = op.idx // SEM_ROT
            op.sem = self._eng_sem(eng, k)
            op.val = op.idx % SEM_ROT + 1
        for k in reads:
            self._need(op, self.last_w.get(k), True)
        for k in writes:
            self._need(op, self.last_w.get(k), False)
            for r in self.readers.get(k, ()):
                self._need(op, r, False)
        for k in reads:
            lst = self.readers.setdefault(k, [])
            if not dma:
                lst[:] = [r for r in lst if r.dma or r.eng != eng]
            lst.append(op)
        for k in writes:
            self.last_w[k] = op
            self.readers[k] = []
        self.ops[eng].append(op)
        self.last_op[eng] = op
        return op

    def phase_end(self):
        R = len(self.dsems)
        for eng in self.ENGS:
            op = Op()
            op.eng, op.fn, op.waits, op.dma, op.idx = eng, None, [], False, -1
            for x in self.ENGS:
                if x != eng and x != "sp" and self.last_op[x] is not None:
                    self._need(op, self.last_op[x], True)
            for d in self.dma_ops[-R:]:
                self._need(op, d, True)
            self.ops[eng].append(op)
        self.emit()
        self.ops = {e: [] for e in self.ENGS}
        self.last_w = {}
        self.readers = {}
        self.alias = {}
        self.nbank = 0

    def pe(self, fn, reads=(), writes=()):
        return self.add("pe", fn, reads, writes)

    def act(self, fn, reads=(), writes=()):
        return self.add("act", fn, reads, writes)

    def dve(self, fn, reads=(), writes=()):
        return self.add("dve", fn, reads, writes)

    def pool(self, fn, reads=(), writes=()):
        return self.add("pool", fn, reads, writes)

    def dma(self, out, in_, reads=(), writes=(), **kw):
        return self.add("sp", lambda e: e.dma_start(out=out, in_=in_, **kw), reads, writes, dma=True)

    def finish(self):
        pass

    def emit(self):
        nc = self.nc
        with nc.Block() as block:
            def replay(name, e):
                for op in self.ops[name]:
                    for (s, v) in op.waits:
                        e.wait_ge(s, v)
                    if op.fn is None:
                        continue
                    ins = op.fn(e)
                    ins.then_inc(op.sem, 16 if op.dma else 1)

            @block.tensor
            def _(e):
                replay("pe", e)

            @block.scalar
            def _(e):
                replay("act", e)

            @block.vector
            def _(e):
                replay("dve", e)

            @block.gpsimd
            def _(e):
                replay("pool", e)

            @block.sync
            def _(e):
                replay("sp", e)


_UNIQ = [0]


def sb(nc, es, name, shape, dt):
    _UNIQ[0] += 1
    return es.enter_context(nc.sbuf_tensor(f"{name}_{_UNIQ[0]}", list(shape), dt))


def psb(P, name):
    return P.bank(name)


def emit_l1(nc, P, es, xT, w_in, V, projT, tag="l1", ntok=TOK):
    NCC = NPROJ // 128
    Wb = sb(nc, es, tag + "Wb", [128, 8, NPROJ], BF16)
    Wst = [sb(nc, es, tag + f"Wst{i}", [128, NPROJ], F32) for i in range(2)]
    X = [sb(nc, es, tag + f"X{i}", [128, 8, T], F32) for i in range(2)]
    Xsq = sb(nc, es, tag + "Xsq", [128, 8, T], BF16)
    H = [sb(nc, es, tag + f"H{i}", [128, 8, T], BF16) for i in range(2)]
    ones = sb(nc, es, tag + "ones", [128, 128], BF16)
    rs = sb(nc, es, tag + "rs", [128, T], F32)
    O = [sb(nc, es, tag + f"O{i}", [128, T], F32) for i in range(4)]
    ps_ss = psb(P, tag + "ps_ss")
    ps = [psb(P, tag + f"ps{i}") for i in range(4)]

    P.pool(lambda e: e.memset(ones[:], 1.0), writes=["ones"])
    w_v = w_in.rearrange("(kc p) n -> kc p n", p=128)
    for kc in range(8):
        st = Wst[kc % 2]
        P.dma(st[:], w_v[kc], writes=[("Wst", kc % 2)])
        P.pool(lambda e, st=st, kc=kc: e.tensor_copy(out=Wb[:, kc, :], in_=st[:]),
               reads=[("Wst", kc % 2)], writes=[("Wb", kc)])
    x_v = xT.rearrange("(kc p) t -> p kc t", p=128)
    nout = 0
    for it in range(ntok // T):
        xs = X[it % 2]
        hs = H[it % 2]
        ts = slice(it * T, (it + 1) * T)
        P.dma(xs[:], x_v[:, :, ts], reads=[("xT", it)], writes=[("X", it % 2)])
        P.act(lambda e, xs=xs: e.activation(out=Xsq[:], in_=xs[:], func=AF.Square),
              reads=[("X", it % 2)], writes=["Xsq"])
        for kc in range(8):
            P.pe(lambda e, kc=kc: e.matmul(ps_ss[:], lhsT=ones[:], rhs=Xsq[:, kc, :],
                                           start=(kc == 0), stop=(kc == 7)),
                 reads=["ones", "Xsq"], writes=[tag + "ps_ss"])
        P.dve(lambda e: e.tensor_scalar(out=rs[:], in0=ps_ss[:], scalar1=1.0 / D_MODEL, scalar2=1e-6,
                                        op0=ALU.mult, op1=ALU.add),
              reads=[tag + "ps_ss"], writes=["rs"])
        P.act(lambda e: e.activation(out=rs[:], in_=rs[:], func=AF.Sqrt), reads=["rs"], writes=["rs"])
        P.dve(lambda e: e.reciprocal(out=rs[:], in_=rs[:]), reads=["rs"], writes=["rs"])
        for kc in range(8):
            P.dve(lambda e, kc=kc, xs=xs, hs=hs: e.scalar_tensor_tensor(
                out=hs[:, kc, :], in0=xs[:, kc, :], scalar=vcol(V, "pre_g", kc), in1=rs[:],
                op0=ALU.mult, op1=ALU.mult),
                reads=[("X", it % 2), "V", "rs"], writes=[("H", it % 2, kc)])
        for cc in range(NCC):
            pb = ps[cc % 4]
            for kc in range(8):
                P.pe(lambda e, kc=kc, cc=cc, pb=pb, hs=hs: e.matmul(
                    pb[:], lhsT=Wb[:, kc, cc * 128:(cc + 1) * 128], rhs=hs[:, kc, :],
                    start=(kc == 0), stop=(kc == 7)),
                    reads=[("Wb", kc), ("H", it % 2, kc)], writes=[(tag + "ps", cc % 4)])
            ob = O[nout % 4]
            if cc % 2 == 0:
                P.act(lambda e, ob=ob, pb=pb: e.copy(out=ob[:], in_=pb[:]),
                      reads=[(tag + "ps", cc % 4)], writes=[("O", nout % 4)])
            else:
                P.dve(lambda e, ob=ob, pb=pb: e.tensor_copy(out=ob[:], in_=pb[:]),
                      reads=[(tag + "ps", cc % 4)], writes=[("O", nout % 4)])
            P.dma(projT[cc * 128:(cc + 1) * 128, ts], ob[:], reads=[("O", nout % 4)],
                  writes=[("projT", it, cc)])
            nout += 1


VEC_SPEC = [("pre_g", 8), ("post_g", 8), ("a_dw_w", 62), ("a_dw_b", 2), ("a_ln_g", 2), ("a_ln_b", 2),
            ("a_pw_b", 2), ("b_lb", 8), ("b_norm_g", 2), ("c_conv_w", 24), ("c_conv_b", 6),
            ("c_norm_g", 2), ("d_qa_g", 2), ("d_kva_g", 1), ("c_dt_bias", 1), ("c_a_log", 1), ("c_d", 2)]
VEC_OFF = {}
_o = 0
for _n, _w in VEC_SPEC:
    VEC_OFF[_n] = (_o, _w)
    _o += _w
NV = _o


def vcol(V, name, j=0, n=1, rows=128):
    o, w = VEC_OFF[name]
    return V[0:rows, o + j:o + j + n]


def emit_A(nc, P, es, projT, V, pw_w, ycatT, ntok, tag="A"):
    HAL = 32
    val = [sb(nc, es, tag + f"val{i}", [128, 2, T], F32) for i in range(2)]
    glu = [sb(nc, es, tag + f"glu{i}", [128, 2, T], F32) for i in range(2)]
    gat = [sb(nc, es, tag + f"gat{i}", [128, 2, T], F32) for i in range(2)]
    gb = sb(nc, es, tag + "gb", [128, 2, HAL + T], F32)
    acc = sb(nc, es, tag + "acc", [128, 2, T], F32)
    sq = sb(nc, es, tag + "sq", [128, 2, T], F32)
    mean = sb(nc, es, tag + "mean", [128, T], F32)
    rstd = sb(nc, es, tag + "rstd", [128, T], F32)
    hs = sb(nc, es, tag + "hs", [128, 2, T], BF16)
    ya = [sb(nc, es, tag + f"ya{i}", [128, 2, T], BF16) for i in range(2)]
    onesf = sb(nc, es, tag + "onesf", [128, 128], F32)
    pwst = sb(nc, es, tag + "pwst", [128, 2, 256], F32)
    pwb = sb(nc, es, tag + "pwb", [128, 2, 256], BF16)
    ps1 = psb(P, tag + "ps1")
    ps2 = psb(P, tag + "ps2")
    pso = [psb(P, tag + f"pso{i}") for i in range(2)]

    P.pool(lambda e: e.memset(onesf[:], 1.0), writes=[tag + "onesf"])
    P.dma(pwst[:], pw_w.rearrange("(c p) n -> p c n", p=128), writes=[tag + "pwst"])
    P.pool(lambda e: e.tensor_copy(out=pwb[:], in_=pwst[:]), reads=[tag + "pwst"], writes=[tag + "pwb"])
    P.pool(lambda e: e.memset(gb[:, :, 0:HAL], 0.0), writes=[tag + "gb"])
    pv = projT.rearrange("(c p) t -> p c t", p=128)
    for it in range(ntok // T):
        ts = slice(it * T, (it + 1) * T)
        b = it % 2
        P.dma(val[b][:], pv[:, 0:2, ts], reads=[("projT", it)], writes=[(tag + "val", b)])
        P.dma(glu[b][:], pv[:, 2:4, ts], reads=[("projT", it)], writes=[(tag + "glu", b)])
        P.dma(gat[b][:], pv[:, 4:6, ts], reads=[("projT", it)], writes=[(tag + "gat", b)])
        P.act(lambda e, b=b: e.activation(out=glu[b][:], in_=glu[b][:], func=AF.Sigmoid),
              reads=[(tag + "glu", b)], writes=[(tag + "glu", b)])
        P.dve(lambda e, b=b: e.tensor_tensor(out=gb[:, :, HAL:HAL + T], in0=val[b][:], in1=glu[b][:], op=ALU.mult),
              reads=[(tag + "glu", b), (tag + "val", b)], writes=[tag + "gb"])
        P.act(lambda e, b=b: e.activation(out=gat[b][:], in_=gat[b][:], func=AF.Silu),
              reads=[(tag + "gat", b)], writes=[(tag + "gat", b)])
        for c in range(2):
            eng = P.dve
            for j in range(31):
                src = gb[:, c, HAL - 30 + j:HAL - 30 + j + T]
                wj = vcol(V, "a_dw_w", c * 31 + j)
                if j == 0:
                    eng(lambda e, c=c, src=src, wj=wj: e.tensor_scalar(
                        out=acc[:, c, :], in0=src, scalar1=wj, scalar2=vcol(V, "a_dw_b", c),
                        op0=ALU.mult, op1=ALU.add),
                        reads=[tag + "gb", "V"], writes=[(tag + "acc", c)])
                else:
                    eng(lambda e, c=c, src=src, wj=wj: e.scalar_tensor_tensor(
                        out=acc[:, c, :], in0=src, scalar=wj, in1=acc[:, c, :], op0=ALU.mult, op1=ALU.add),
                        reads=[tag + "gb", "V", (tag + "acc", c)], writes=[(tag + "acc", c)])
        P.act(lambda e: e.copy(out=gb[:, :, 0:HAL], in_=gb[:, :, T:T + HAL]),
              reads=[tag + "gb"], writes=[tag + "gb"])
        P.act(lambda e: e.activation(out=sq[:], in_=acc[:], func=AF.Square),
              reads=[(tag + "acc", 0), (tag + "acc", 1)], writes=[tag + "sq"])
        for c in range(2):
            P.pe(lambda e, c=c: e.matmul(ps1[:], lhsT=onesf[:], rhs=acc[:, c, :], start=(c == 0), stop=(c == 1)),
                 reads=[tag + "onesf", (tag + "acc", c)], writes=[tag + "ps1"])
        for c in range(2):
            P.pe(lambda e, c=c: e.matmul(ps2[:], lhsT=onesf[:], rhs=sq[:, c, :], start=(c == 0), stop=(c == 1)),
                 reads=[tag + "onesf", tag + "sq"], writes=[tag + "ps2"])
        P.dve(lambda e: e.tensor_scalar(out=mean[:], in0=ps1[:], scalar1=1.0 / 256, scalar2=None, op0=ALU.mult),
              reads=[tag + "ps1"], writes=[tag + "mean"])
        P.dve(lambda e: e.tensor_tensor(out=rstd[:], in0=mean[:], in1=mean[:], op=ALU.mult),
              reads=[tag + "mean"], writes=[tag + "rstd"])
        P.dve(lambda e: e.scalar_tensor_tensor(out=rstd[:], in0=ps2[:], scalar=1.0 / 256, in1=rstd[:],
                                               op0=ALU.mult, op1=ALU.subtract),
              reads=[tag + "ps2", tag + "rstd"], writes=[tag + "rstd"])
        P.dve(lambda e: e.tensor_scalar(out=rstd[:], in0=rstd[:], scalar1=1e-5, scalar2=None, op0=ALU.add),
              reads=[tag + "rstd"], writes=[tag + "rstd"])
        P.act(lambda e: e.activation(out=rstd[:], in_=rstd[:], func=AF.Sqrt), reads=[tag + "rstd"], writes=[tag + "rstd"])
        P.dve(lambda e: e.reciprocal(out=rstd[:], in_=rstd[:]), reads=[tag + "rstd"], writes=[tag + "rstd"])
        for c in range(2):
            P.dve(lambda e, c=c: e.tensor_tensor(out=acc[:, c, :], in0=acc[:, c, :], in1=mean[:], op=ALU.subtract),
                  reads=[(tag + "acc", c), tag + "mean", tag + "ps1"], writes=[(tag + "acc", c)])
            P.dve(lambda e, c=c: e.tensor_tensor(out=acc[:, c, :], in0=acc[:, c, :], in1=rstd[:], op=ALU.mult),
                  reads=[(tag + "acc", c), tag + "rstd"], writes=[(tag + "acc", c)])
            P.act(lambda e, c=c: e.activation(out=hs[:, c, :], in_=acc[:, c, :], func=AF.Silu,
                                              scale=vcol(V, "a_ln_g", c), bias=vcol(V, "a_ln_b", c)),
                  reads=[(tag + "acc", c), "V"], writes=[(tag + "hs", c)])
        for co in range(2):
            for ci in range(2):
                P.pe(lambda e, co=co, ci=ci: e.matmul(pso[co][:], lhsT=pwb[:, ci, co * 128:(co + 1) * 128],
                                                      rhs=hs[:, ci, :], start=(ci == 0), stop=(ci == 1)),
                     reads=[tag + "pwb", (tag + "hs", ci)], writes=[(tag + "pso", co)])
            P.dve(lambda e, co=co, b=b: e.scalar_tensor_tensor(
                out=ya[b][:, co, :], in0=pso[co][:], scalar=vcol(V, "a_pw_b", co), in1=gat[b][:, co, :],
                op0=ALU.add, op1=ALU.mult),
                reads=[(tag + "pso", co), (tag + "gat", b), "V"], writes=[(tag + "ya", b)])
        P.dma(ycatT.rearrange("(c p) t -> p c t", p=128)[:, 0:2, ts], ya[b][:],
              reads=[(tag + "ya", b)], writes=[("ycatA", it)])


COLMAP = [(0, 768), (768, 1792), (1792, 2560), (2564, 2820), (2820, 3076), (3076, 3204),
          (3236, 3492), (3204, 3236), (2560, 2564)]
R_VAL, R_GLU, R_AG = 0, 256, 512
R_BQ, R_BF, R_BI, R_BG = 768, 1024, 1280, 1536
R_XBC, R_Z, R_CQ, R_CKV, R_DG, R_KR, R_DT = 1792, 2560, 2816, 3072, 3200, 3456, 3488


def pack_w_in(w):
    out = np.zeros((D_MODEL, NPROJ), np.float32)
    o = 0
    for a, b in COLMAP:
        out[:, o:o + b - a] = w[:, a:b]
        o += b - a
    return out


def _pc(v):
    return np.ascontiguousarray(np.asarray(v, np.float32).reshape(-1, 128).T)


def pack_vecs(p, l):
    V = np.zeros((128, NV), np.float32)

    def put(name, arr):
        o, w = VEC_OFF[name]
        V[:arr.shape[0], o:o + arr.shape[1]] = arr

    put("pre_g", _pc(p["pre_norm_g"][l]))
    put("post_g", _pc(p["post_norm_g"][l]))
    w = np.asarray(p["a_dw_w"][l], np.float32)
    put("a_dw_w", np.concatenate([w[:, c * 128:(c + 1) * 128].T for c in range(2)], axis=1))
    put("a_dw_b", _pc(p["a_dw_b"][l]))
    put("a_ln_g", _pc(p["a_ln_g"][l]))
    put("a_ln_b", _pc(p["a_ln_b"][l]))
    put("a_pw_b", _pc(p["a_pw_b"][l]))
    lg = np.asarray(p["b_lb_logits"], np.float32)
    put("b_lb", np.concatenate([lg[:, c * 128:(c + 1) * 128].T for c in range(2)], axis=1))
    put("b_norm_g", _pc(p["b_norm_g"][l]))
    cw = np.asarray(p["c_conv_w"][l], np.float32)
    put("c_conv_w", np.concatenate([cw[:, c * 128:(c + 1) * 128].T for c in range(6)], axis=1))
    put("c_conv_b", _pc(p["c_conv_b"][l]))
    put("c_norm_g", _pc(p["c_norm_g"][l]))
    put("d_qa_g", _pc(p["d_qa_g"][l]))
    put("d_kva_g", _pc(p["d_kva_g"][l]))
    for nm in ("c_dt_bias", "c_a_log"):
        col = np.zeros((128, 1), np.float32)
        col[0:4, 0] = np.asarray(p[nm][l], np.float32)
        col[64:68, 0] = np.asarray(p[nm][l], np.float32)
        put(nm, col)
    cd = np.asarray(p["c_d"][l], np.float32)
    put("c_d", np.stack([np.repeat(cd[2 * g:2 * g + 2], 64) for g in range(2)], axis=1))
    return V


def emit_O(nc, P, es, ycatT, xT_in, V, w_out, xT_out, ntok, tag="O"):
    Wo = sb(nc, es, tag + "Wo", [128, 8, D_MODEL], BF16)
    Wst = [sb(nc, es, tag + f"Wst{i}", [128, D_MODEL], F32) for i in range(2)]
    Y = [sb(nc, es, tag + f"Y{i}", [128, 8, T], BF16) for i in range(2)]
    X = [sb(nc, es, tag + f"X{i}", [128, 8, T], F32) for i in range(2)]
    Yo = sb(nc, es, tag + "Yo", [128, 8, T], F32)
    Ysq = sb(nc, es, tag + "Ysq", [128, 8, T], BF16)
    ones = sb(nc, es, tag + "ones", [128, 128], BF16)
    rs = sb(nc, es, tag + "rs", [128, T], F32)
    ps = [psb(P, tag + f"ps{i}") for i in range(4)]
    ps_ss = psb(P, tag + "ps_ss")
    P.pool(lambda e: e.memset(ones[:], 1.0), writes=[tag + "ones"])
    w_v = w_out.rearrange("(kc p) n -> kc p n", p=128)
    for kc in range(8):
        st = Wst[kc % 2]
        P.dma(st[:], w_v[kc], writes=[(tag + "Wst", kc % 2)])
        P.pool(lambda e, st=st, kc=kc: e.tensor_copy(out=Wo[:, kc, :], in_=st[:]),
               reads=[(tag + "Wst", kc % 2)], writes=[(tag + "Wo", kc)])
    yv = ycatT.rearrange("(c p) t -> p c t", p=128)
    xv = xT_in.rearrange("(c p) t -> p c t", p=128)
    xo = xT_out.rearrange("(c p) t -> p c t", p=128)
    for it in range(ntok // T):
        ts = slice(it * T, (it + 1) * T)
        b = it % 2
        P.dma(Y[b][:], yv[:, :, ts], reads=[("ycatA", it), ("ycatB", it), ("ycatC", it), ("ycatD", it)],
              writes=[(tag + "Y", b)])
        P.dma(X[b][:], xv[:, :, ts], reads=[("xT", it)], writes=[(tag + "X", b)])
        for do in range(8):
            pb = ps[do % 4]
            for kc in range(8):
                P.pe(lambda e, kc=kc, do=do, pb=pb, b=b: e.matmul(
                    pb[:], lhsT=Wo[:, kc, do * 128:(do + 1) * 128], rhs=Y[b][:, kc, :],
                    start=(kc == 0), stop=(kc == 7)),
                    reads=[(tag + "Wo", kc), (tag + "Y", b)], writes=[(tag + "ps", do % 4)])
            P.act(lambda e, do=do, pb=pb: e.copy(out=Yo[:, do, :], in_=pb[:]),
                  reads=[(tag + "ps", do % 4)], writes=[(tag + "Yo", do)])
            P.act(lambda e, do=do, pb=pb: e.activation(out=Ysq[:, do, :], in_=pb[:], func=AF.Square),
                  reads=[(tag + "ps", do % 4)], writes=[(tag + "Ysq", do)])
        for do in range(8):
            P.pe(lambda e, do=do: e.matmul(ps_ss[:], lhsT=ones[:], rhs=Ysq[:, do, :],
                                           start=(do == 0), stop=(do == 7)),
                 reads=[tag + "ones", (tag + "Ysq", do)], writes=[tag + "ps_ss"])
        P.dve(lambda e: e.tensor_scalar(out=rs[:], in0=ps_ss[:], scalar1=1.0 / D_MODEL, scalar2=1e-6,
                                        op0=ALU.mult, op1=ALU.add), reads=[tag + "ps_ss"], writes=[tag + "rs"])
        P.act(lambda e: e.activation(out=rs[:], in_=rs[:], func=AF.Sqrt), reads=[tag + "rs"], writes=[tag + "rs"])
        P.dve(lambda e: e.reciprocal(out=rs[:], in_=rs[:]), reads=[tag + "rs"], writes=[tag + "rs"])
        for do in range(8):
            P.dve(lambda e, do=do: e.scalar_tensor_tensor(
                out=Yo[:, do, :], in0=Yo[:, do, :], scalar=vcol(V, "post_g", do), in1=rs[:],
                op0=ALU.mult, op1=ALU.mult), reads=[(tag + "Yo", do), tag + "rs", "V"], writes=[(tag + "Yo", do)])
            P.dve(lambda e, do=do, b=b: e.tensor_tensor(out=Yo[:, do, :], in0=Yo[:, do, :], in1=X[b][:, do, :],
                                                        op=ALU.add),
                   reads=[(tag + "Yo", do), (tag + "X", b)], writes=[(tag + "Yo", do)])
        P.dma(xo[:, :, ts], Yo[:], reads=[(tag + "Yo", do) for do in range(8)], writes=[("xTo", it)])


NCP = 8
TWO_PI = float(2 * np.pi)


def make_cp():
    cp = np.zeros((128, NCP), np.float32)
    inv = (10000.0 ** (-np.arange(0, 32, 2, dtype=np.float32) / 32)).astype(np.float32)
    cp[64:96, 0] = np.concatenate([inv, inv])
    cp[64:96, 1] = np.concatenate([-np.ones(16), np.ones(16)])
    cp[64:96, 2] = np.concatenate([-np.pi * np.ones(16), np.pi * np.ones(16)])
    cp[:, 3] = np.pi
    cp[:, 4] = np.pi / 2
    return cp


def pack_qb(w):
    w = np.asarray(w, np.float32).reshape(256, 4, 96)
    b = w.copy()
    b[:, :, 64:80] = w[:, :, 80:96]
    b[:, :, 80:96] = w[:, :, 64:80]
    return np.ascontiguousarray(np.stack([w, b], axis=2))


def pack_kvb(w):
    w = np.asarray(w, np.float32).reshape(128, 4, 128)
    return np.ascontiguousarray(np.stack([w[:, :, 0:64].reshape(128, 256), w[:, :, 64:128].reshape(128, 256)], axis=1))


def emit_D(nc, P, es, projT, pos, V, CP, qbw, kvbw, QT, KT, VD, OD, ycatT, ntok, tag="D"):
    nt = ntok // T
    SC = float(96 ** -0.5)
    qst = sb(nc, es, tag + "qst", [128, 2, 768], F32)
    qb = sb(nc, es, tag + "qb", [128, 2, 768], BF16)
    kst = sb(nc, es, tag + "kst", [128, 512], F32)
    kb_ = sb(nc, es, tag + "kb", [128, 512], BF16)
    ones = sb(nc, es, tag + "ones", [128, 128], BF16)
    cq = [sb(nc, es, tag + f"cq{i}", [128, 2, T], F32) for i in range(2)]
    ckv = [sb(nc, es, tag + f"ckv{i}", [128, T], F32) for i in range(2)]
    krA = [sb(nc, es, tag + f"krA{i}", [128, T], F32) for i in range(2)]
    krB = [sb(nc, es, tag + f"krB{i}", [128, T], F32) for i in range(2)]
    posi = [sb(nc, es, tag + f"posi{i}", [128, T], I32) for i in range(2)]
    sqb = sb(nc, es, tag + "sqb", [128, 3, T], BF16)
    rs = sb(nc, es, tag + "rs", [128, T], F32)
    rs2 = sb(nc, es, tag + "rs2", [128, T], F32)
    cqn = sb(nc, es, tag + "cqn", [128, 2, T], BF16)
    ckvn = sb(nc, es, tag + "ckvn", [128, T], BF16)
    ang = sb(nc, es, tag + "ang", [128, T], F32)
    cosT = sb(nc, es, tag + "cos", [128, T], F32)
    sinT = sb(nc, es, tag + "sin", [128, T], F32)
    tmp = sb(nc, es, tag + "tmp", [128, T], F32)
    tmp2 = sb(nc, es, tag + "tmp2", [128, T], F32)
    kf = sb(nc, es, tag + "kf", [128, T], F32)
    ki = sb(nc, es, tag + "ki", [128, T], I32)
    qt = [sb(nc, es, tag + f"qt{i}", [96, 4, T], BF16) for i in range(2)]
    kt = [sb(nc, es, tag + f"kt{i}", [96, 4, T], BF16) for i in range(2)]
    vt = [sb(nc, es, tag + f"vt{i}", [128, 4, 4, 65], BF16) for i in range(2)]
    ps_a = psb(P, tag + "ps_a")
    ps_b = psb(P, tag + "ps_b")
    ps_q = [psb(P, tag + f"ps_q{i}") for i in range(2)]
    ps_q2 = [psb(P, tag + f"ps_q2{i}") for i in range(2)]
    ps_v = psb(P, tag + "ps_v")

    P.pool(lambda e: e.memset(ones[:], 1.0), writes=[tag + "ones"])
    P.dma(qst[:], qbw.rearrange("(c p) n -> p c n", p=128), writes=[tag + "qst"])
    P.pool(lambda e: e.tensor_copy(out=qb[:], in_=qst[:]), reads=[tag + "qst"], writes=[tag + "qb"])
    P.dma(kst[:], kvbw[:, :], writes=[tag + "kst"])
    P.pool(lambda e: e.tensor_copy(out=kb_[:], in_=kst[:]), reads=[tag + "kst"], writes=[tag + "kb"])
    for i in range(2):
        P.pool(lambda e, i=i: e.memset(vt[i][:, :, :, 64:65], 1.0), writes=[(tag + "vt", i)])
    pv = projT.rearrange("(c p) t -> p c t", p=128)
    QTv = QT.rearrange("h d t -> d h t")
    KTv = KT.rearrange("h d t -> d h t")
    for it in range(nt):
        ts = slice(it * T, (it + 1) * T)
        b = it % 2
        P.dma(cq[b][:], pv[:, 22:24, ts], reads=[("projT", it)], writes=[(tag + "cq", b)])
        P.dma(ckv[b][:], projT[R_CKV:R_CKV + 128, ts], reads=[("projT", it)], writes=[(tag + "ckv", b)])
        P.dma(krA[b][64:96, :], projT[R_KR:R_KR + 32, ts], reads=[("projT", it)], writes=[(tag + "krA", b)])
        P.dma(krB[b][64:80, :], projT[R_KR + 16:R_KR + 32, ts], reads=[("projT", it)], writes=[(tag + "krB", b)])
        P.dma(krB[b][80:96, :], projT[R_KR:R_KR + 16, ts], reads=[("projT", it)], writes=[(tag + "krB", b)])
        P.dma(posi[b][64:96, :], pos[0:1, ts].partition_broadcast(32), writes=[(tag + "posi", b)])
        P.act(lambda e, b=b: e.activation(out=sqb[:, 0:2, :], in_=cq[b][:], func=AF.Square),
              reads=[(tag + "cq", b)], writes=[tag + "sqq"])
        P.act(lambda e, b=b: e.activation(out=sqb[:, 2, :], in_=ckv[b][:], func=AF.Square),
              reads=[(tag + "ckv", b)], writes=[tag + "sqk"])
        for c in range(2):
            P.pe(lambda e, c=c: e.matmul(ps_a[:], lhsT=ones[:], rhs=sqb[:, c, :], start=(c == 0), stop=(c == 1)),
                 reads=[tag + "ones", tag + "sqq"], writes=[tag + "ps_a"])
        P.pe(lambda e: e.matmul(ps_b[:], lhsT=ones[:], rhs=sqb[:, 2, :], start=True, stop=True),
             reads=[tag + "ones", tag + "sqk"], writes=[tag + "ps_b"])
        for (pss, r, n) in ((ps_a, rs, 256.0), (ps_b, rs2, 128.0)):
            k1, k2 = (tag + "ps_a", tag + "rs") if pss is ps_a else (tag + "ps_b", tag + "rs2")
            P.dve(lambda e, pss=pss, r=r, n=n: e.tensor_scalar(out=r[:], in0=pss[:], scalar1=1.0 / n, scalar2=1e-6,
                                                             op0=ALU.mult, op1=ALU.add), reads=[k1], writes=[k2])
            P.act(lambda e, r=r: e.activation(out=r[:], in_=r[:], func=AF.Sqrt), reads=[k2], writes=[k2])
            P.dve(lambda e, r=r: e.reciprocal(out=r[:], in_=r[:]), reads=[k2], writes=[k2])
        for c in range(2):
            P.dve(lambda e, c=c, b=b: e.scalar_tensor_tensor(out=cqn[:, c, :], in0=cq[b][:, c, :],
                                                            scalar=vcol(V, "d_qa_g", c), in1=rs[:],
                                                            op0=ALU.mult, op1=ALU.mult),
                  reads=[(tag + "cq", b), tag + "rs", "V"], writes=[tag + "cqn"])
        P.dve(lambda e, b=b: e.scalar_tensor_tensor(out=ckvn[:], in0=ckv[b][:], scalar=vcol(V, "d_kva_g"),
                                                     in1=rs2[:], op0=ALU.mult, op1=ALU.mult),
              reads=[(tag + "ckv", b), tag + "rs2", "V"], writes=[tag + "ckvn"])
        R_ = slice(64, 96)
        P.dve(lambda e, b=b: e.tensor_copy(out=ang[R_, :], in_=posi[b][R_, :]), reads=[(tag + "posi", b)],
              writes=[tag + "ang"])
        P.dve(lambda e: e.tensor_scalar(out=ang[R_, :], in0=ang[R_, :], scalar1=CP[R_, 0:1], scalar2=None,
                                        op0=ALU.mult), reads=[tag + "ang", "CP"], writes=[tag + "ang"])
        def reduce_angle(dst, kd, add_half_pi):
            if add_half_pi:
                P.dve(lambda e: e.tensor_scalar(out=dst[R_, :], in0=ang[R_, :], scalar1=CP[R_, 4:5], scalar2=None,
                                                op0=ALU.add), reads=[tag + "ang", "CP"], writes=[kd])
            else:
                P.dve(lambda e: e.tensor_copy(out=dst[R_, :], in_=ang[R_, :]), reads=[tag + "ang"], writes=[kd])
            P.dve(lambda e: e.tensor_scalar(out=kf[R_, :], in0=dst[R_, :], scalar1=1.0 / TWO_PI, scalar2=None,
                                            op0=ALU.mult), reads=[kd], writes=[tag + "kf"])
            P.dve(lambda e: e.tensor_copy(out=ki[R_, :], in_=kf[R_, :]), reads=[tag + "kf"], writes=[tag + "ki"])
            P.dve(lambda e: e.tensor_copy(out=kf[R_, :], in_=ki[R_, :]), reads=[tag + "ki"], writes=[tag + "kf"])
            P.dve(lambda e: e.scalar_tensor_tensor(out=dst[R_, :], in0=kf[R_, :], scalar=-6.28125, in1=dst[R_, :],
                                                   op0=ALU.mult, op1=ALU.add), reads=[tag + "kf", kd], writes=[kd])
            P.dve(lambda e: e.scalar_tensor_tensor(out=dst[R_, :], in0=kf[R_, :], scalar=-(TWO_PI - 6.28125),
                                                   in1=dst[R_, :], op0=ALU.mult, op1=ALU.add),
                  reads=[tag + "kf", kd], writes=[kd])
            P.dve(lambda e: e.tensor_scalar(out=kf[R_, :], in0=dst[R_, :], scalar1=float(np.pi), scalar2=None,
                                            op0=ALU.is_gt), reads=[kd], writes=[tag + "kf"])
            P.dve(lambda e: e.scalar_tensor_tensor(out=dst[R_, :], in0=kf[R_, :], scalar=-TWO_PI, in1=dst[R_, :],
                                                   op0=ALU.mult, op1=ALU.add), reads=[tag + "kf", kd], writes=[kd])

        reduce_angle(tmp, tag + "tmp", False)
        P.act(lambda e: e.activation(out=sinT[R_, :], in_=tmp[R_, :], func=AF.Sin, scale=CP[R_, 1:2]),
              reads=[tag + "tmp", "CP"], writes=[tag + "sin"])
        reduce_angle(tmp2, tag + "tmp2", True)
        P.act(lambda e: e.activation(out=cosT[R_, :], in_=tmp2[R_, :], func=AF.Sin),
              reads=[tag + "tmp2"], writes=[tag + "cos"])
        P.dve(lambda e, b=b: e.tensor_tensor(out=krA[b][R_, :], in0=krA[b][R_, :], in1=cosT[R_, :], op=ALU.mult),
              reads=[(tag + "krA", b), tag + "cos"], writes=[(tag + "krA", b)])
        P.dve(lambda e, b=b: e.tensor_tensor(out=krB[b][R_, :], in0=krB[b][R_, :], in1=sinT[R_, :], op=ALU.mult),
              reads=[(tag + "krB", b), tag + "sin"], writes=[(tag + "krB", b)])
        for h in range(4):
            P.dve(lambda e, b=b, h=h: e.tensor_tensor(out=kt[b][R_, h, :], in0=krA[b][R_, :], in1=krB[b][R_, :],
                                                       op=ALU.add),
                   reads=[(tag + "krA", b), (tag + "krB", b)], writes=[(tag + "kt", b)])
        for h in range(4):
            P.pe(lambda e, h=h: e.matmul(ps_q[h % 2][0:64, :], lhsT=kb_[:, h * 64:(h + 1) * 64], rhs=ckvn[:],
                                         start=True, stop=True),
                 reads=[tag + "kb", tag + "ckvn"], writes=[(tag + "ps_q", h % 2)])
            P.act(lambda e, h=h, b=b: e.copy(out=kt[b][0:64, h, :], in_=ps_q[h % 2][0:64, :]),
                  reads=[(tag + "ps_q", h % 2)], writes=[(tag + "kt", b)])
        for h in range(4):
            pA, pB = ps_q[h % 2], ps_q2[h % 2]
            for c in range(2):
                P.pe(lambda e, h=h, c=c, pA=pA: e.matmul(pA[0:96, :], lhsT=qb[:, c, (h * 2) * 96:(h * 2 + 1) * 96],
                                                         rhs=cqn[:, c, :], start=(c == 0), stop=(c == 1)),
                     reads=[tag + "qb", tag + "cqn"], writes=[(tag + "ps_q", h % 2)])
            for c in range(2):
                P.pe(lambda e, h=h, c=c, pB=pB: e.matmul(pB[0:96, :], lhsT=qb[:, c, (h * 2 + 1) * 96:(h * 2 + 2) * 96],
                                                         rhs=cqn[:, c, :], start=(c == 0), stop=(c == 1)),
                     reads=[tag + "qb", tag + "cqn"], writes=[(tag + "ps_q2", h % 2)])
            P.act(lambda e, h=h, b=b, pA=pA: e.copy(out=qt[b][0:64, h, :], in_=pA[0:64, :]),
                  reads=[(tag + "ps_q", h % 2)], writes=[(tag + "qt", b)])
            P.dve(lambda e, pA=pA: e.tensor_tensor(out=tmp[R_, :], in0=pA[R_, :], in1=cosT[R_, :], op=ALU.mult),
                  reads=[(tag + "ps_q", h % 2), tag + "cos"], writes=[tag + "tmp"])
            P.dve(lambda e, pB=pB: e.tensor_tensor(out=tmp2[R_, :], in0=pB[R_, :], in1=sinT[R_, :], op=ALU.mult),
                  reads=[(tag + "ps_q2", h % 2), tag + "sin"], writes=[tag + "tmp2"])
            P.dve(lambda e, h=h, b=b: e.tensor_tensor(out=qt[b][R_, h, :], in0=tmp[R_, :], in1=tmp2[R_, :], op=ALU.add),
                  reads=[tag + "tmp", tag + "tmp2"], writes=[(tag + "qt", b)])
        for j in range(4):
            P.pe(lambda e, j=j: e.matmul(ps_v[:, 0:256],
                                         lhsT=ckvn[:, j * 128:(j + 1) * 128], rhs=kb_[:, 256:512],
                                         start=True, stop=True),
                 reads=[tag + "ckvn", tag + "kb"], writes=[tag + "ps_v"])
            P.act(lambda e, j=j, b=b: e.copy(out=vt[b][:, j, :, 0:64],
                                             in_=ps_v[:, 0:256].rearrange("p (h v) -> p h v", h=4)),
                  reads=[tag + "ps_v"], writes=[(tag + "vt", b)])
        P.dma(QTv[:, :, ts], qt[b][:], reads=[(tag + "qt", b)], writes=[("QT", it)])
        P.dma(KTv[:, :, ts], kt[b][:], reads=[(tag + "kt", b)], writes=[("KT", it)])
        for h in range(4):
            P.dma(VD[h, :, it * 4:(it + 1) * 4, :], vt[b][:, :, h, :], reads=[(tag + "vt", b)], writes=[("VD", it)])

    P.ps_reset()
    Kall = sb(nc, es, tag + "Kall", [96, ntok], BF16)
    Vall = sb(nc, es, tag + "Vall", [128, ntok // 128, 65], BF16)
    Qg = [sb(nc, es, tag + f"Qg{i}", [96, T], BF16) for i in range(2)]
    Pt = [sb(nc, es, tag + f"Pt{i}", [128, T], BF16) for i in range(3)]
    Ost = [sb(nc, es, tag + f"Ost{i}", [65, T], F32) for i in range(2)]
    S_ps = [psb(P, tag + f"S_ps{i}") for i in range(3)]
    O_ps = [psb(P, tag + f"O_ps{i}") for i in range(2)]
    allk = [("KT", i) for i in range(nt)]
    allv = [("VD", i) for i in range(nt)]
    n = 0
    ng = 0
    for h in range(4):
        P.dma(Kall[:], KT[h], reads=allk, writes=[tag + "Kall"])
        P.dma(Vall[:], VD[h], reads=allv, writes=[tag + "Vall"])
        for qg in range(nt):
            gb = ng % 2
            ng += 1
            P.dma(Qg[gb][:], QT[h, :, qg * T:(qg + 1) * T], reads=[("QT", qg)], writes=[(tag + "Qg", gb)])
            nkb = 4 * qg + 4
            for kb in range(nkb):
                j = kb - 4 * qg
                c0 = max(0, j) * 128
                sp, pt = S_ps[n % 3], Pt[n % 3]
                kS, kP = (tag + "S_ps", n % 3), (tag + "Pt", n % 3)
                n += 1
                P.pe(lambda e, sp=sp, kb=kb, gb=gb, c0=c0: e.matmul(
                    sp[:, c0:T], lhsT=Kall[:, kb * 128:(kb + 1) * 128], rhs=Qg[gb][:, c0:T], start=True, stop=True),
                    reads=[tag + "Kall", (tag + "Qg", gb)], writes=[kS])
                P.act(lambda e, sp=sp, pt=pt, c0=c0: e.activation(out=pt[:, c0:T], in_=sp[:, c0:T], func=AF.Exp,
                                                                  scale=SC), reads=[kS], writes=[kP])
                if j >= 0:
                    P.pool(lambda e, pt=pt, c0=c0: e.memset(pt[64:128, c0:c0 + 64], 0.0), reads=[kP], writes=[kP])
                P.pe(lambda e, pt=pt, kb=kb, gb=gb, c0=c0, nkb=nkb: e.matmul(
                    O_ps[gb][0:65, c0:T], lhsT=Vall[:, kb, :], rhs=pt[:, c0:T], start=(kb == 0), stop=(kb == nkb - 1)),
                    reads=[tag + "Vall", kP], writes=[(tag + "O_ps", gb)])
            P.dve(lambda e, gb=gb: e.tensor_copy(out=Ost[gb][:], in_=O_ps[gb][0:65, :]),
                  reads=[(tag + "O_ps", gb)], writes=[(tag + "Ost", gb)])
            P.dma(OD[h, :, qg * T:(qg + 1) * T], Ost[gb][:], reads=[(tag + "Ost", gb)], writes=[("OD", qg, h)])

    Oa = [sb(nc, es, tag + f"Oa{i}", [128, 2, T], F32) for i in range(2)]
    La = [sb(nc, es, tag + f"La{i}", [128, 2, T], F32) for i in range(2)]
    Ga = [sb(nc, es, tag + f"Ga{i}", [128, 2, T], F32) for i in range(2)]
    Yd = [sb(nc, es, tag + f"Yd{i}", [128, 2, T], BF16) for i in range(2)]
    yv = ycatT.rearrange("(c p) t -> p c t", p=128)
    for it in range(nt):
        ts = slice(it * T, (it + 1) * T)
        b = it % 2
        odk = [("OD", it, h) for h in range(4)]
        for h in range(4):
            P.dma(Oa[b][(h % 2) * 64:(h % 2) * 64 + 64, h // 2, :], OD[h, 0:64, ts], reads=odk, writes=[(tag + "Oa", b)])
            P.dma(La[b][(h % 2) * 64:(h % 2) * 64 + 64, h // 2, :], OD[h, 64:65, ts].partition_broadcast(64),
                  reads=odk, writes=[(tag + "La", b)])
        P.dma(Ga[b][:], pv[:, 25:27, ts], reads=[("projT", it)], writes=[(tag + "Ga", b)])
        P.act(lambda e, b=b: e.activation(out=Ga[b][:], in_=Ga[b][:], func=AF.Silu), reads=[(tag + "Ga", b)],
              writes=[(tag + "Ga", b)])
        P.dve(lambda e, b=b: e.reciprocal(out=La[b][:], in_=La[b][:]), reads=[(tag + "La", b)], writes=[(tag + "La", b)])
        P.dve(lambda e, b=b: e.tensor_tensor(out=Oa[b][:], in0=Oa[b][:], in1=La[b][:], op=ALU.mult),
              reads=[(tag + "Oa", b), (tag + "La", b)], writes=[(tag + "Oa", b)])
        P.dve(lambda e, b=b: e.tensor_tensor(out=Yd[b][:], in0=Oa[b][:], in1=Ga[b][:], op=ALU.mult),
              reads=[(tag + "Oa", b), (tag + "Ga", b)], writes=[(tag + "Yd", b)])
        P.dma(yv[:, 6:8, ts], Yd[b][:], reads=[(tag + "Yd", b)], writes=[("ycatD", it)])


def emit_lb(nc, P, es, V, LB, l):
    ex = sb(nc, es, "lb_ex", [128, 2, 4], F32)
    sm = sb(nc, es, "lb_sm", [128, 2], F32)
    o, w = VEC_OFF["b_lb"]
    lg = V[:, o:o + 8].rearrange("p (c l) -> p c l", c=2)
    P.act(lambda e: e.activation(out=ex[:], in_=lg, func=AF.Exp), reads=["V"], writes=["lb_ex"])
    P.dve(lambda e: e.tensor_reduce(out=sm[:], in_=ex[:], axis=AX.X, op=ALU.add), reads=["lb_ex"], writes=["lb_sm"])
    P.dve(lambda e: e.reciprocal(out=sm[:], in_=sm[:]), reads=["lb_sm"], writes=["lb_sm"])
    P.pool(lambda e: e.memset(LB[:, 0, :], 0.0), writes=["LB"])
    for j in range(1, l + 1):
        P.dve(lambda e, j=j: e.tensor_tensor(out=LB[:, 0, :], in0=LB[:, 0, :], in1=ex[:, :, j], op=ALU.add),
              reads=["LB", "lb_ex"], writes=["LB"])
    P.dve(lambda e: e.tensor_tensor(out=LB[:, 0, :], in0=LB[:, 0, :], in1=sm[:], op=ALU.mult),
          reads=["LB", "lb_sm"], writes=["LB"])
    P.dve(lambda e: e.tensor_scalar(out=LB[:, 1, :], in0=LB[:, 0, :], scalar1=-1.0, scalar2=1.0, op0=ALU.mult,
                                    op1=ALU.add), reads=["LB"], writes=["LB"])
    P.dve(lambda e: e.tensor_scalar(out=LB[:, 2, :], in0=LB[:, 1, :], scalar1=-1.0, scalar2=None, op0=ALU.mult),
          reads=["LB"], writes=["LB"])


def emit_B(nc, P, es, projT, V, LB, KC, ycatT, ntok, tag="B"):
    nt = ntok // T
    ident, bd, rmask, rm, tri2 = KC["ident"], KC["bd"], KC["rmask"], KC["rm"], KC["tri2"]
    q = [sb(nc, es, tag + f"q{i}", [128, 2, T], F32) for i in range(2)]
    f = [sb(nc, es, tag + f"f{i}", [128, 2, T], F32) for i in range(2)]
    vi = [sb(nc, es, tag + f"vi{i}", [128, 2, T], F32) for i in range(2)]
    g = [sb(nc, es, tag + f"g{i}", [128, 2, T], F32) for i in range(2)]
    kk = sb(nc, es, tag + "kk", [128, 2, T], F32)
    bb = sb(nc, es, tag + "bb", [128, 2, T], F32)
    t1 = sb(nc, es, tag + "t1", [128, 2, T], F32)
    t4 = sb(nc, es, tag + "t4", [128, 2, T], F32)
    e3 = sb(nc, es, tag + "e3", [128, 2, T], F32)
    ee = sb(nc, es, tag + "ee", [128, 2, T], F32)
    qt = sb(nc, es, tag + "qt", [128, 2, T], BF16)
    ktl = sb(nc, es, tag + "ktl", [128, 2, T], BF16)
    qe = sb(nc, es, tag + "qe", [128, 2, T], BF16)
    kh = sb(nc, es, tag + "kh", [128, 2, T], BF16)
    vb = sb(nc, es, tag + "vb", [128, 2, T], BF16)
    khT = sb(nc, es, tag + "khT", [128, 4, 256], BF16)
    vT = sb(nc, es, tag + "vT", [128, 4, 256], BF16)
    Asb = [sb(nc, es, tag + f"Asb{i}", [128, 4, 128], BF16) for i in range(4)]
    qtm = [sb(nc, es, tag + f"qtm{i}", [128, 2, T], BF16) for i in range(2)]
    qem = [sb(nc, es, tag + f"qem{i}", [128, 2, T], BF16) for i in range(2)]
    vTm = [sb(nc, es, tag + f"vTm{i}", [128, 4, 256], BF16) for i in range(2)]
    S = sb(nc, es, tag + "S", [128, 128], F32)
    Sbf = sb(nc, es, tag + "Sbf", [128, 128], BF16)
    osq = sb(nc, es, tag + "osq", [128, 2, T], F32)
    rs = sb(nc, es, tag + "rs", [128, 2, T], F32)
    yb = [sb(nc, es, tag + f"yb{i}", [128, 2, T], BF16) for i in range(2)]
    pkT = [psb(P, tag + f"pkT{i}") for i in range(2)]
    pvT = [psb(P, tag + f"pvT{i}") for i in range(2)]
    pS = [psb(P, tag + f"pS{i}") for i in range(4)]
    pO = [psb(P, tag + f"pO{i}") for i in range(2)]
    pU = [psb(P, tag + f"pU{i}") for i in range(2)]

    P.pool(lambda e: e.memset(S[:], 0.0), writes=[tag + "S"])
    P.pool(lambda e: e.memset(Sbf[:], 0.0), writes=[tag + "Sbf"])
    pv = projT.rearrange("(c p) t -> p c t", p=128)
    yv = ycatT.rearrange("(c p) t -> p c t", p=128)
    nU = 0
    for it in range(nt):
        ts = slice(it * T, (it + 1) * T)
        b = it % 2
        P.dma(q[b][:], pv[:, 6:8, ts], reads=[("projT", it)], writes=[(tag + "q", b)])
        P.dma(f[b][:], pv[:, 8:10, ts], reads=[("projT", it)], writes=[(tag + "f", b)])
        P.dma(vi[b][:], pv[:, 10:12, ts], reads=[("projT", it)], writes=[(tag + "vi", b)])
        P.dma(g[b][:], pv[:, 12:14, ts], reads=[("projT", it)], writes=[(tag + "g", b)])
        fb, qb_, vib, gb_ = f[b], q[b], vi[b], g[b]
        kf, kq, kv, kg = (tag + "f", b), (tag + "q", b), (tag + "vi", b), (tag + "g", b)
        P.act(lambda e, fb=fb: e.activation(out=fb[:], in_=fb[:], func=AF.Sigmoid), reads=[kf], writes=[kf])
        for c in range(2):
            P.dve(lambda e, c=c, fb=fb: e.tensor_scalar(out=kk[:, c, :], in0=fb[:, c, :], scalar1=LB[:, 2, c:c + 1],
                                                        scalar2=LB[:, 1, c:c + 1], op0=ALU.mult, op1=ALU.add),
                  reads=[kf, "LB"], writes=[tag + "kk"])
            P.dve(lambda e, c=c, fb=fb: e.tensor_scalar(out=fb[:, c, :], in0=fb[:, c, :], scalar1=LB[:, 1, c:c + 1],
                                                        scalar2=LB[:, 0, c:c + 1], op0=ALU.mult, op1=ALU.add),
                  reads=[kf, "LB", tag + "kk"], writes=[kf])
        P.act(lambda e, fb=fb: e.activation(out=fb[:], in_=fb[:], func=AF.Ln), reads=[kf], writes=[kf])
        for c in range(2):
            P.dve(lambda e, c=c, fb=fb: e.tensor_tensor_scan(out=bb[:, c, :], data0=rmask[:], data1=fb[:, c, :],
                                                             initial=0.0, op0=ALU.mult, op1=ALU.add),
                  reads=[kf, "KC"], writes=[tag + "bb"])
        b4 = bb[:].rearrange("p c (n s) -> p c n s", s=64)
        mid = b4[:, :, :, 31:32].to_broadcast([128, 2, 8, 64])
        last = b4[:, :, :, 63:64].to_broadcast([128, 2, 8, 64])
        t14 = t1[:].rearrange("p c (n s) -> p c n s", s=64)
        t44 = t4[:].rearrange("p c (n s) -> p c n s", s=64)
        P.dve(lambda e: e.tensor_tensor(out=t14, in0=b4, in1=mid, op=ALU.subtract), reads=[tag + "bb"], writes=[tag + "t1"])
        P.dve(lambda e: e.tensor_tensor(out=t44, in0=last, in1=b4, op=ALU.subtract), reads=[tag + "bb"], writes=[tag + "t4"])
        P.act(lambda e: e.activation(out=e3[:], in_=bb[:], func=AF.Exp), reads=[tag + "bb"], writes=[tag + "e3"])
        P.dve(lambda e, qb_=qb_: e.tensor_tensor(out=qe[:], in0=qb_[:], in1=e3[:], op=ALU.mult),
              reads=[kq, tag + "e3"], writes=[tag + "qe"])
        P.act(lambda e: e.activation(out=ee[:], in_=t1[:], func=AF.Exp), reads=[tag + "t1"], writes=[tag + "ee"])
        P.dve(lambda e, qb_=qb_: e.tensor_tensor(out=qt[:], in0=qb_[:], in1=ee[:], op=ALU.mult),
              reads=[kq, tag + "ee"], writes=[tag + "qt"])
        P.act(lambda e: e.activation(out=ee[:], in_=t1[:], func=AF.Exp, scale=-1.0), reads=[tag + "t1", tag + "qt"],
              writes=[tag + "ee"])
        P.dve(lambda e: e.tensor_tensor(out=ktl[:], in0=kk[:], in1=ee[:], op=ALU.mult),
              reads=[tag + "kk", tag + "ee"], writes=[tag + "ktl"])
        P.act(lambda e: e.activation(out=t4[:], in_=t4[:], func=AF.Exp), reads=[tag + "t4"], writes=[tag + "t4"])
        P.dve(lambda e: e.tensor_tensor(out=kh[:], in0=kk[:], in1=t4[:], op=ALU.mult),
               reads=[tag + "kk", tag + "t4"], writes=[tag + "kh"])
        P.pool(lambda e, vib=vib: e.tensor_copy(out=vb[:], in_=vib[:]), reads=[kv], writes=[tag + "vb"])
        P.act(lambda e, gb_=gb_: e.activation(out=gb_[:], in_=gb_[:], func=AF.Silu), reads=[kg], writes=[kg])
        for (src, ksrc, pst, kps, dst, kdst) in ((kh, tag + "kh", pkT, tag + "pkT", khT, tag + "khT"),
                                                  (vb, tag + "vb", pvT, tag + "pvT", vT, tag + "vT")):
            for j in range(4):
                for c in range(2):
                    P.pe(lambda e, src=src, pst=pst, j=j, c=c: e.matmul(
                        pst[j // 2][:, (j % 2) * 256 + c * 128:(j % 2) * 256 + c * 128 + 128],
                        lhsT=src[:, c, j * 128:(j + 1) * 128], rhs=ident[:], start=True, stop=True),
                        reads=[ksrc, "KC"], writes=[(kps, j // 2)])
            for hf in range(2):
                eng = P.act if hf == 0 else P.dve
                if hf == 0:
                    P.act(lambda e, pst=pst, dst=dst: e.copy(out=dst[:, 0:2, :].rearrange("p j n -> p (j n)"), in_=pst[0][:]),
                          reads=[(kps, 0)], writes=[kdst])
                else:
                    P.dve(lambda e, pst=pst, dst=dst: e.tensor_copy(out=dst[:, 2:4, :].rearrange("p j n -> p (j n)"), in_=pst[1][:]),
                          reads=[(kps, 1)], writes=[kdst])
        for e_ in range(2):
            P.dve(lambda e, e_=e_: e.tensor_scalar(out=qtm[e_][:], in0=qt[:], scalar1=rm[:, e_:e_ + 1], scalar2=None,
                                                   op0=ALU.mult), reads=[tag + "qt", "KC"], writes=[(tag + "qtm", e_)])
            P.dve(lambda e, e_=e_: e.tensor_scalar(out=qem[e_][:], in0=qe[:], scalar1=rm[:, e_:e_ + 1], scalar2=None,
                                                   op0=ALU.mult), reads=[tag + "qe", "KC"], writes=[(tag + "qem", e_)])
            P.dve(lambda e, e_=e_: e.tensor_scalar(out=vTm[e_][:], in0=vT[:], scalar1=rm[:, e_:e_ + 1], scalar2=None,
                                                   op0=ALU.mult), reads=[tag + "vT", "KC"], writes=[(tag + "vTm", e_)])
        for j in range(4):
            blk = slice(j * 128, (j + 1) * 128)
            for h in range(4):
                c2, e_ = h // 2, h % 2
                P.pe(lambda e, j=j, h=h, c2=c2, e_=e_, blk=blk: e.matmul(
                    pS[j][:, h * 128:(h + 1) * 128], lhsT=ktl[:, c2, blk], rhs=qtm[e_][:, c2, blk], start=True, stop=True),
                    reads=[tag + "ktl", (tag + "qtm", e_)], writes=[(tag + "pS", j)])
            P.dve(lambda e, j=j: e.tensor_tensor(out=Asb[j][:], in0=pS[j][:].rearrange("p (h t) -> p h t", h=4),
                                                 in1=tri2.rearrange("p (o t) -> p o t", o=1).to_broadcast([128, 4, 128]),
                                                 op=ALU.mult),
                  reads=[(tag + "pS", j), "KC"], writes=[(tag + "Asb", j)])
        for j in range(4):
            blk = slice(j * 128, (j + 1) * 128)
            for h in range(4):
                c2, hp = h // 2, (h % 2) * 64
                P.pe(lambda e, j=j, h=h, c2=c2, hp=hp, blk=blk: e.matmul(
                    pO[c2][hp:hp + 64, blk], lhsT=vT[:, j, h * 64:(h + 1) * 64], rhs=Asb[j][:, h, :],
                    start=True, stop=False), reads=[tag + "vT", (tag + "Asb", j)], writes=[(tag + "pO", c2)])
            for par in range(2):
                c = 2 * j + par
                cols = slice(c * 64, c * 64 + 64)
                ub = nU % 2
                nU += 1
                for h in range(4):
                    c2, hp, e_ = h // 2, (h % 2) * 64, h % 2
                    P.pe(lambda e, c2=c2, hp=hp, e_=e_, cols=cols, par=par: e.matmul(
                        pO[c2][hp:hp + 64, cols], lhsT=Sbf[:, c2 * 64:(c2 + 1) * 64], rhs=qem[e_][:, c2, cols],
                        start=False, stop=(par == 1)), reads=[tag + "Sbf", (tag + "qem", e_)], writes=[(tag + "pO", c2)])
                for h in range(4):
                    c2, hp = h // 2, (h % 2) * 64
                    P.pe(lambda e, c2=c2, hp=hp, j=j, h=h, ub=ub, par=par: e.matmul(
                        pU[ub][hp:hp + 64, c2 * 64:(c2 + 1) * 64], lhsT=khT[:, j, h * 64:(h + 1) * 64],
                        rhs=vTm[par][:, j, h * 64:(h + 1) * 64], start=True, stop=True),
                        reads=[tag + "khT", (tag + "vTm", par)], writes=[(tag + "pU", ub)])
                for c2 in range(2):
                    P.dve(lambda e, c2=c2, ub=ub, c=c: e.scalar_tensor_tensor(
                        out=S[:, c2 * 64:(c2 + 1) * 64], in0=S[:, c2 * 64:(c2 + 1) * 64],
                        scalar=e3[:, c2, c * 64 + 63:c * 64 + 64], in1=pU[ub][:, c2 * 64:(c2 + 1) * 64],
                        op0=ALU.mult, op1=ALU.add), reads=[tag + "S", tag + "e3", (tag + "pU", ub)], writes=[tag + "S"])
                P.act(lambda e: e.copy(out=Sbf[:], in_=S[:]), reads=[tag + "S"], writes=[tag + "Sbf"])
        for c2 in range(2):
            P.act(lambda e, c2=c2: e.activation(out=osq[:, c2, :], in_=pO[c2][:], func=AF.Square),
                  reads=[(tag + "pO", c2)], writes=[(tag + "osq", c2)])
            P.pe(lambda e, c2=c2: e.matmul(pS[c2][:], lhsT=bd[:], rhs=osq[:, c2, :], start=True, stop=True),
                 reads=["KC", (tag + "osq", c2)], writes=[(tag + "pS", c2)])
            P.dve(lambda e, c2=c2: e.tensor_scalar(out=rs[:, c2, :], in0=pS[c2][:], scalar1=1.0 / 64, scalar2=1e-6,
                                                   op0=ALU.mult, op1=ALU.add), reads=[(tag + "pS", c2)], writes=[(tag + "rs", c2)])
            P.act(lambda e, c2=c2: e.activation(out=rs[:, c2, :], in_=rs[:, c2, :], func=AF.Sqrt),
                  reads=[(tag + "rs", c2)], writes=[(tag + "rs", c2)])
            P.dve(lambda e, c2=c2: e.reciprocal(out=rs[:, c2, :], in_=rs[:, c2, :]), reads=[(tag + "rs", c2)],
                  writes=[(tag + "rs", c2)])
            P.dve(lambda e, c2=c2: e.scalar_tensor_tensor(out=rs[:, c2, :], in0=rs[:, c2, :], scalar=vcol(V, "b_norm_g", c2),
                                                          in1=pO[c2][:], op0=ALU.mult, op1=ALU.mult),
                  reads=[(tag + "rs", c2), (tag + "pO", c2), "V"], writes=[(tag + "rs", c2)])
            P.dve(lambda e, c2=c2, b=b, gb_=gb_: e.tensor_tensor(out=yb[b][:, c2, :], in0=rs[:, c2, :], in1=gb_[:, c2, :],
                                                                 op=ALU.mult),
                  reads=[(tag + "rs", c2), kg], writes=[(tag + "yb", b)])
        P.dma(yv[:, 2:4, ts], yb[b][:], reads=[(tag + "yb", b)], writes=[("ycatB", it)])
        yield


def make_kc_host():
    ident = np.eye(128, dtype=np.float32)
    bd = np.kron(np.eye(2, dtype=np.float32), np.ones((64, 64), np.float32))
    s = np.arange(128) % 64
    tri = (s[:, None] <= np.arange(64)[None, :]).astype(np.float32)
    rmask = np.ones((128, T), np.float32)
    rmask[:, ::64] = 0.0
    rm = np.zeros((128, 2), np.float32)
    rm[0:64, 0] = 1.0
    rm[64:128, 1] = 1.0
    pp = np.arange(128)
    tri2 = ((pp[:, None] // 64 == pp[None, :] // 64) & (pp[:, None] % 64 <= pp[None, :] % 64)).astype(np.float32)
    return np.ascontiguousarray(np.concatenate([ident, bd, tri, rmask, rm, tri2], axis=1))


def load_kc(nc, P, es, d_kc):
    raw = sb(nc, es, "kc_raw", [128, 962], F32)
    ident = sb(nc, es, "kc_ident", [128, 128], BF16)
    P.dma(raw[:], d_kc[:, :], writes=["KC"])
    P.dve(lambda e: e.tensor_copy(out=ident[:], in_=raw[:, 0:128]), reads=["KC"], writes=["KC"])
    return {"ident": ident, "bd": raw[:, 128:256], "tri": raw[:, 256:320], "rmask": raw[:, 320:832], "rm": raw[:, 832:834],
            "tri2": raw[:, 834:962], "raw": raw}


def make_kc2_host():
    j = np.arange(128)
    su = (j[:, None] > j[None, :]).astype(np.float32)
    lt = (j[:, None] <= j[None, :]).astype(np.float32)
    negi = (-30000.0 * np.eye(128)).astype(np.float32)
    ones = np.ones((128, 128), np.float32)
    return np.ascontiguousarray(np.concatenate([su, lt, su, negi, ones, np.eye(128, dtype=np.float32)], axis=1))


class _Stop(Exception):
    pass


def _ck(k):
    import os
    if os.environ.get("CSTOP") == str(k):
        raise _Stop()


def emit_C(nc, P, es, projT, V, KC, KC2, ycatT, ntok, tag="C"):
    nt = ntok // T
    ident = KC["ident"]
    SU, LT, GT, NEGI, ONES, IDF = (KC2[:, i * 128:(i + 1) * 128] for i in range(6))
    xb = sb(nc, es, tag + "xb", [128, 6, 4 + T], F32)
    xc = sb(nc, es, tag + "xc", [128, 6, T], F32)
    bcb = sb(nc, es, tag + "bcb", [128, 6, T], BF16)
    z = [sb(nc, es, tag + f"z{i}", [128, 2, T], F32) for i in range(2)]
    dta = sb(nc, es, tag + "dta", [128, T], F32)
    negA = sb(nc, es, tag + "negA", [128, 1], F32)
    atm = sb(nc, es, tag + "atm", [128, 16], F32)
    d2a = sb(nc, es, tag + "d2a", [128, 4, 4], F32)
    datm = sb(nc, es, tag + "datm", [128, 4, 8], F32)
    Lh = sb(nc, es, tag + "Lh", [128, 4, 128], F32)
    AB = sb(nc, es, tag + "AB", [128, 4, 128], F32)
    Lm = sb(nc, es, tag + "Lm", [128, 4, 128], F32)
    Sc = sb(nc, es, tag + "Sc", [128, 4, 128], BF16)
    DEC = sb(nc, es, tag + "DEC", [128, 2, T], F32)
    dl = sb(nc, es, tag + "dl", [128, 4], F32)
    d2 = sb(nc, es, tag + "d2", [128, 4], F32)
    xdtf = sb(nc, es, tag + "xdtf", [128, 256], F32)
    xdtb = sb(nc, es, tag + "xdtb", [128, 256], BF16)
    wxb = sb(nc, es, tag + "wxb", [128, 256], BF16)
    Btm = sb(nc, es, tag + "Btm", [128, 256], BF16)
    st = sb(nc, es, tag + "st", [128, 4, 64], F32)
    stb = sb(nc, es, tag + "stb", [128, 4, 64], BF16)
    yt = sb(nc, es, tag + "yt", [128, 2, T], F32)
    ysq = sb(nc, es, tag + "ysq", [128, 2, T], F32)
    rs = sb(nc, es, tag + "rs", [128, 2, T], F32)
    yc = [sb(nc, es, tag + f"yc{i}", [128, 2, T], BF16) for i in range(2)]
    p_da = psb(P, tag + "p_da")
    p_seg = psb(P, tag + "p_seg")
    p_acs = psb(P, tag + "p_acs")
    p_m1 = psb(P, tag + "p_m1")
    p_m2 = psb(P, tag + "p_m2")
    p_y = [psb(P, tag + f"p_y{i}") for i in range(2)]
    p_ss = psb(P, tag + "p_ss")

    P.pool(lambda e: e.memset(xb[:, :, 0:4], 0.0), writes=[tag + "xb"])
    P.pool(lambda e: e.memset(st[:], 0.0), writes=[tag + "st"])
    P.pool(lambda e: e.memset(stb[:], 0.0), writes=[tag + "stb"])
    P.pool(lambda e: e.memset(dta[:], 0.0), writes=[tag + "dta"])
    P.act(lambda e: e.activation(out=negA[:], in_=vcol(V, "c_a_log", rows=128), func=AF.Exp), reads=["V"], writes=[tag + "negA"])
    P.dve(lambda e: e.tensor_scalar(out=negA[:], in0=negA[:], scalar1=-1.0, scalar2=None, op0=ALU.mult),
          reads=[tag + "negA"], writes=[tag + "negA"])
    pv = projT.rearrange("(c p) t -> p c t", p=128)
    yv = ycatT.rearrange("(c p) t -> p c t", p=128)
    ny = 0
    for it in range(nt):
        ts = slice(it * T, (it + 1) * T)
        b = it % 2
        P.dma(xb[:, :, 4:4 + T], pv[:, 14:20, ts], reads=[("projT", it)], writes=[tag + "xb"])
        P.dma(z[b][:], pv[:, 20:22, ts], reads=[("projT", it)], writes=[(tag + "z", b)])
        P.dma(dta[0:4, :], projT[R_DT:R_DT + 4, ts], reads=[("projT", it)], writes=[tag + "dta"])
        P.dma(dta[64:68, :], projT[R_DT:R_DT + 4, ts], reads=[("projT", it)], writes=[tag + "dta"])
        for c in range(6):
            eng = P.dve
            for j in range(4):
                src = xb[:, c, 1 + j:1 + j + T]
                wj = vcol(V, "c_conv_w", c * 4 + j)
                if j == 0:
                    eng(lambda e, c=c, src=src, wj=wj: e.tensor_scalar(out=xc[:, c, :], in0=src, scalar1=wj,
                                                                      scalar2=vcol(V, "c_conv_b", c), op0=ALU.mult, op1=ALU.add),
                        reads=[tag + "xb", "V"], writes=[(tag + "xc", c)])
                else:
                    eng(lambda e, c=c, src=src, wj=wj: e.scalar_tensor_tensor(out=xc[:, c, :], in0=src, scalar=wj,
                                                                             in1=xc[:, c, :], op0=ALU.mult, op1=ALU.add),
                        reads=[tag + "xb", "V", (tag + "xc", c)], writes=[(tag + "xc", c)])
        allxc = [(tag + "xc", c) for c in range(6)]
        P.act(lambda e: e.copy(out=xb[:, :, 0:4], in_=xb[:, :, T:T + 4]), reads=[tag + "xb"] + allxc, writes=[tag + "xb"])
        P.act(lambda e: e.activation(out=xc[:], in_=xc[:], func=AF.Silu), reads=allxc, writes=allxc)
        P.dve(lambda e: e.tensor_copy(out=bcb[:], in_=xc[:]), reads=allxc, writes=[tag + "bcb"])
        P.act(lambda e, b=b: e.activation(out=z[b][:], in_=z[b][:], func=AF.Silu), reads=[(tag + "z", b)], writes=[(tag + "z", b)])
        _ck(1)
        P.act(lambda e: e.activation(out=dta[:], in_=dta[:], func=AF.Exp, bias=vcol(V, "c_dt_bias", rows=128)),
              reads=[tag + "dta", "V"], writes=[tag + "dta"])
        P.dve(lambda e: e.tensor_scalar(out=dta[:], in0=dta[:], scalar1=1.0, scalar2=None, op0=ALU.add),
              reads=[tag + "dta"], writes=[tag + "dta"])
        P.act(lambda e: e.activation(out=dta[:], in_=dta[:], func=AF.Ln), reads=[tag + "dta"], writes=[tag + "dta"])
        P.dve(lambda e: e.tensor_scalar(out=dta[64:128, :], in0=dta[64:128, :], scalar1=negA[64:128, 0:1], scalar2=None,
                                        op0=ALU.mult), reads=[tag + "dta", tag + "negA"], writes=[tag + "dta"])
        for j in range(4):
            P.pe(lambda e, j=j: e.matmul(p_da[:, j * 128:(j + 1) * 128], lhsT=dta[:, j * 128:(j + 1) * 128], rhs=IDF,
                                         start=True, stop=True), reads=[tag + "dta", "KC2"], writes=[tag + "p_da"])
        for j in range(4):
            P.dve(lambda e, j=j: e.tensor_copy(out=datm[:, j, 0:4], in_=p_da[:, j * 128:j * 128 + 4]),
                  reads=[tag + "p_da"], writes=[tag + "datm"])
            P.dve(lambda e, j=j: e.tensor_copy(out=datm[:, j, 4:8], in_=p_da[:, j * 128 + 64:j * 128 + 68]),
                  reads=[tag + "p_da"], writes=[tag + "datm"])
            P.dve(lambda e, j=j: e.tensor_copy(out=atm[:, j * 4:j * 4 + 4], in_=p_da[:, j * 128 + 64:j * 128 + 68]),
                  reads=[tag + "p_da"], writes=[tag + "atm"])
        P.pe(lambda e: e.matmul(p_da[:, 0:16], lhsT=GT, rhs=atm[:], start=True, stop=True),
             reads=["KC2", tag + "atm"], writes=[tag + "p_da"])
        P.act(lambda e: e.activation(out=d2a[:].rearrange("p j h -> p (j h)"), in_=p_da[:, 0:16], func=AF.Exp),
              reads=[tag + "p_da"], writes=[tag + "d2a"])
        _ck(2)
        for j in range(4):
            blk = slice(j * 128, (j + 1) * 128)
            yb_ = ny % 2
            ny += 1
            py = p_y[yb_]
            kpy = (tag + "p_y", yb_)
            for h in range(4):
                asc = datm[:, j, 4 + h:5 + h]
                P.dve(lambda e, h=h, asc=asc: e.tensor_scalar(out=Lh[:, h, :], in0=SU, scalar1=asc, scalar2=None, op0=ALU.mult),
                      reads=[tag + "datm", "KC2"], writes=[(tag + "Lh", h)])
                P.dve(lambda e, h=h, asc=asc: e.tensor_scalar(out=AB[:, h, :], in0=ONES, scalar1=asc, scalar2=None, op0=ALU.mult),
                       reads=[tag + "datm", "KC2"], writes=[(tag + "AB", h)])
            for h in range(4):
                P.pe(lambda e, h=h: e.matmul(p_seg[:, h * 128:(h + 1) * 128], lhsT=Lh[:, h, :], rhs=LT, start=True, stop=False),
                     reads=[(tag + "Lh", h), "KC2"], writes=[tag + "p_seg"])
                P.pe(lambda e, h=h: e.matmul(p_seg[:, h * 128:(h + 1) * 128], lhsT=NEGI, rhs=GT, start=False, stop=True),
                     reads=["KC2"], writes=[tag + "p_seg"])
            for h in range(4):
                P.pe(lambda e, h=h: e.matmul(p_acs[:, h * 128:(h + 1) * 128], lhsT=AB[:, h, :], rhs=LT, start=True, stop=True),
                     reads=[(tag + "AB", h), "KC2"], writes=[tag + "p_acs"])
            P.act(lambda e: e.activation(out=Lm[:].rearrange("p h l -> p (h l)"), in_=p_seg[:], func=AF.Exp),
                  reads=[tag + "p_seg"], writes=[tag + "Lm"])
            for h in range(4):
                P.act(lambda e, h=h: e.activation(out=dl[:, h:h + 1], in_=p_acs[:, h * 128 + 127:h * 128 + 128], func=AF.Exp),
                      reads=[tag + "p_acs"], writes=[tag + "dl"])
            for h in range(4):
                g_, hp = h // 2, (h % 2) * 64
                P.act(lambda e, h=h, g_=g_, hp=hp, blk=blk: e.activation(out=DEC[hp:hp + 64, g_, blk],
                                                                        in_=p_acs[hp:hp + 64, h * 128:(h + 1) * 128], func=AF.Exp),
                      reads=[tag + "p_acs"], writes=[tag + "DEC"])
            _ck(3)
            for g_ in range(2):
                P.pe(lambda e, g_=g_, blk=blk: e.matmul(p_m1[:, g_ * 128:(g_ + 1) * 128], lhsT=bcb[:, g_, blk], rhs=ident[:],
                                                         start=True, stop=True), reads=[tag + "bcb", "KC"], writes=[tag + "p_m1"])
                P.pe(lambda e, g_=g_, blk=blk: e.matmul(p_m1[:, 256 + g_ * 128:256 + (g_ + 1) * 128], lhsT=bcb[:, 2 + g_, blk],
                                                         rhs=ident[:], start=True, stop=True), reads=[tag + "bcb", "KC"], writes=[tag + "p_m1"])
            for h in range(4):
                P.dve(lambda e, h=h, j=j: e.tensor_scalar(out=xdtf[:, h * 64:(h + 1) * 64], in0=p_m1[:, h * 64:(h + 1) * 64],
                                                          scalar1=datm[:, j, h:h + 1], scalar2=None, op0=ALU.mult),
                      reads=[tag + "p_m1", tag + "datm"], writes=[tag + "xdtf"])
            P.act(lambda e: e.copy(out=Btm[:], in_=p_m1[:, 256:512]), reads=[tag + "p_m1"], writes=[tag + "Btm"])
            P.act(lambda e: e.copy(out=xdtb[:], in_=xdtf[:]), reads=[tag + "xdtf"], writes=[tag + "xdtb"])
            for h in range(4):
                P.dve(lambda e, h=h, j=j: e.tensor_scalar(out=wxb[:, h * 64:(h + 1) * 64], in0=xdtf[:, h * 64:(h + 1) * 64],
                                                          scalar1=d2a[:, j, h:h + 1], scalar2=None, op0=ALU.mult),
                      reads=[tag + "xdtf", tag + "d2a"], writes=[tag + "wxb"])
            _ck(4)
            for g_ in range(2):
                P.pe(lambda e, g_=g_, blk=blk: e.matmul(p_m2[:, g_ * 128:(g_ + 1) * 128], lhsT=bcb[:, 2 + g_, blk],
                                                         rhs=bcb[:, 4 + g_, blk], start=True, stop=True),
                     reads=[tag + "bcb"], writes=[tag + "p_m2"])
            _ck(40)
            for h in range(4):
                g_ = h // 2
                P.dve(lambda e, h=h, g_=g_: e.tensor_tensor(out=Sc[:, h, :], in0=p_m2[:, g_ * 128:(g_ + 1) * 128], in1=Lm[:, h, :],
                                                            op=ALU.mult), reads=[tag + "p_m2", tag + "Lm"], writes=[(tag + "Sc", h)])
            _ck(41)
            for h in range(4):
                g_, hp = h // 2, (h % 2) * 64
                P.pe(lambda e, h=h, g_=g_, hp=hp, py=py: e.matmul(py[hp:hp + 64, g_ * 128:(g_ + 1) * 128],
                                                                  lhsT=xdtb[:, h * 64:(h + 1) * 64], rhs=Sc[:, h, :],
                                                                  start=True, stop=True),
                     reads=[tag + "xdtb", (tag + "Sc", h)], writes=[kpy])
                P.pe(lambda e, h=h, g_=g_, hp=hp, py=py, blk=blk: e.matmul(py[hp:hp + 64, 256 + g_ * 128:256 + (g_ + 1) * 128],
                                                                           lhsT=stb[:, h, :], rhs=bcb[:, 4 + g_, blk],
                                                                           start=True, stop=True),
                     reads=[tag + "stb", tag + "bcb"], writes=[kpy])
            _ck(42)
            for h in range(4):
                g_ = h // 2
                P.pe(lambda e, h=h, g_=g_: e.matmul(p_m2[:, 256 + h * 64:256 + (h + 1) * 64], lhsT=Btm[:, g_ * 128:(g_ + 1) * 128],
                                                    rhs=wxb[:, h * 64:(h + 1) * 64], start=True, stop=True),
                     reads=[tag + "Btm", tag + "wxb"], writes=[tag + "p_m2"])
            _ck(43)
            for h in range(4):
                P.dve(lambda e, h=h: e.scalar_tensor_tensor(out=st[:, h, :], in0=st[:, h, :], scalar=dl[:, h:h + 1],
                                                            in1=p_m2[:, 256 + h * 64:256 + (h + 1) * 64], op0=ALU.mult, op1=ALU.add),
                      reads=[tag + "st", tag + "dl", tag + "p_m2"], writes=[tag + "st"])
            P.act(lambda e: e.copy(out=stb[:], in_=st[:]), reads=[tag + "st"], writes=[tag + "stb"])
            _ck(5)
            yt3 = yt[:, :, blk]
            P.dve(lambda e, py=py, blk=blk, yt3=yt3: e.tensor_tensor(out=yt3, in0=py[:, 256:512].rearrange("p (g l) -> p g l", g=2),
                                                                     in1=DEC[:, :, blk], op=ALU.mult),
                  reads=[kpy, tag + "DEC"], writes=[tag + "yt"])
            P.dve(lambda e, py=py, yt3=yt3: e.tensor_tensor(out=yt3, in0=py[:, 0:256].rearrange("p (g l) -> p g l", g=2),
                                                            in1=yt3, op=ALU.add), reads=[kpy, tag + "yt"], writes=[tag + "yt"])
        _ck(6)
        for g_ in range(2):
            P.dve(lambda e, g_=g_: e.scalar_tensor_tensor(out=yt[:, g_, :], in0=xc[:, g_, :], scalar=vcol(V, "c_d", g_),
                                                          in1=yt[:, g_, :], op0=ALU.mult, op1=ALU.add),
                  reads=[(tag + "xc", g_), "V", tag + "yt"], writes=[tag + "yt"])
        P.dve(lambda e, b=b: e.tensor_tensor(out=yt[:], in0=yt[:], in1=z[b][:], op=ALU.mult),
              reads=[tag + "yt", (tag + "z", b)], writes=[tag + "yt"])
        P.act(lambda e: e.activation(out=ysq[:], in_=yt[:], func=AF.Square), reads=[tag + "yt"], writes=[tag + "ysq"])
        for g_ in range(2):
            P.pe(lambda e, g_=g_: e.matmul(p_ss[:], lhsT=ONES, rhs=ysq[:, g_, :], start=True, stop=True),
                 reads=["KC2", tag + "ysq"], writes=[tag + "p_ss"])
            P.dve(lambda e, g_=g_: e.tensor_scalar(out=rs[:, g_, :], in0=p_ss[:], scalar1=1.0 / 128, scalar2=1e-6,
                                                   op0=ALU.mult, op1=ALU.add), reads=[tag + "p_ss"], writes=[(tag + "rs", g_)])
            P.act(lambda e, g_=g_: e.activation(out=rs[:, g_, :], in_=rs[:, g_, :], func=AF.Sqrt), reads=[(tag + "rs", g_)],
                  writes=[(tag + "rs", g_)])
            P.dve(lambda e, g_=g_: e.reciprocal(out=rs[:, g_, :], in_=rs[:, g_, :]), reads=[(tag + "rs", g_)], writes=[(tag + "rs", g_)])
            P.dve(lambda e, g_=g_, b=b: e.scalar_tensor_tensor(out=yc[b][:, g_, :], in0=yt[:, g_, :], scalar=vcol(V, "c_norm_g", g_),
                                                               in1=rs[:, g_, :], op0=ALU.mult, op1=ALU.mult),
                  reads=[tag + "yt", (tag + "rs", g_), "V"], writes=[(tag + "yc", b)])
        P.dma(yv[:, 4:6, ts], yc[b][:], reads=[(tag + "yc", b)], writes=[("ycatC", it)])
        yield


def build_full(ntok=SEQ, depth=DEPTH):
    nc = bass.Bass("TRN2", target_bir_lowering=False)
    di = lambda name, shape, dt=F32: nc.dram_tensor(name, list(shape), dt, kind="ExternalInput").ap()
    dn = lambda name, shape, dt=F32: nc.dram_tensor(name, list(shape), dt, kind="Internal").ap()
    xT = di("xT", [D_MODEL, ntok])
    pos = di("pos", [1, ntok], I32)
    Vd = di("vecs", [depth, 128, NV])
    w_in = di("w_in", [depth, D_MODEL, NPROJ])
    w_out = di("w_out", [depth, D_MODEL, D_MODEL])
    pw = di("a_pw", [depth, 256, 256])
    qbw = di("qbw", [depth, 256, 768])
    kvbw = di("kvbw", [depth, 128, 512])
    cp_d = di("cp", [128, NCP])
    kc_d = di("kc", [128, 962])
    kc2_d = di("kc2", [128, 768])
    xTo = nc.dram_tensor("xTo", [D_MODEL, ntok], F32, kind="ExternalOutput").ap()
    projT = dn("projT", [NPROJ, ntok])
    ycatT = dn("ycatT", [D_MODEL, ntok], BF16)
    xbuf = [dn("xbuf0", [D_MODEL, ntok]), dn("xbuf1", [D_MODEL, ntok])]
    QT = dn("QT", [4, 96, ntok], BF16)
    KT = dn("KT", [4, 96, ntok], BF16)
    VD = dn("VD", [4, 128, ntok // 128, 65], BF16)
    OD = dn("OD", [4, 65, ntok])
    with ExitStack() as es:
        P = Prog(nc, es)
        Vs = [sb(nc, es, f"V_sb{l}", [128, NV], F32) for l in range(depth)]
        CP = sb(nc, es, "CP_sb", [128, NCP], F32)
        KC2 = sb(nc, es, "KC2_sb", [128, 768], F32)
        LB = sb(nc, es, "LB_sb", [128, 3, 2], F32)
        for l in range(depth):
            P.dma(Vs[l][:], Vd[l], writes=["V"])
        P.dma(CP[:], cp_d[:, :], writes=["CP"])
        P.dma(KC2[:], kc2_d[:, :], writes=["KC2"])
        KC = load_kc(nc, P, es, kc_d)
        P.phase_end()
        for l in range(depth):
            V = Vs[l]
            x_in = xT if l == 0 else xbuf[(l - 1) % 2]
            x_out = xTo if l == depth - 1 else xbuf[l % 2]
            with ExitStack() as pes:
                emit_lb(nc, P, pes, V, LB, l)
                emit_l1(nc, P, pes, x_in, w_in[l], V, projT, ntok=ntok)
                P.phase_end()
            with ExitStack() as pes:
                emit_A(nc, P, pes, projT, V, pw[l], ycatT, ntok)
                P.phase_end()
            with ExitStack() as pes:
                gB = emit_B(nc, P, pes, projT, V, LB, KC, ycatT, ntok)
                gC = emit_C(nc, P, pes, projT, V, KC, KC2, ycatT, ntok)
                doneB = doneC = False
                while not (doneB and doneC):
                    if not doneB:
                        doneB = next(gB, "end") == "end"
                    if not doneC:
                        doneC = next(gC, "end") == "end"
                P.phase_end()
            with ExitStack() as pes:
                emit_D(nc, P, pes, projT, pos, V, CP, qbw[l], kvbw[l], QT, KT, VD, OD, ycatT, ntok)
                P.phase_end()
            with ExitStack() as pes:
                emit_O(nc, P, pes, ycatT, x_in, V, w_out[l], x_out, ntok)
                P.phase_end()
    return nc


def make_in_map(x_b, pos_b, p, depth=DEPTH):
    return {
        "xT": np.ascontiguousarray(np.asarray(x_b, np.float32).T),
        "pos": np.ascontiguousarray(np.asarray(pos_b, np.int32).reshape(1, -1)),
        "vecs": np.stack([pack_vecs(p, l) for l in range(depth)]),
        "w_in": np.stack([pack_w_in(np.asarray(p["w_in"][l], np.float32)) for l in range(depth)]),
        "w_out": np.ascontiguousarray(np.asarray(p["w_out"], np.float32)[:depth]),
        "a_pw": np.ascontiguousarray(np.asarray(p["a_pw_w"], np.float32)[:depth]),
        "qbw": np.stack([pack_qb(p["d_qb_w"][l]).reshape(256, 768) for l in range(depth)]),
        "kvbw": np.stack([pack_kvb(p["d_kvb_w"][l]).reshape(128, 512) for l in range(depth)]),
        "cp": make_cp(), "kc": make_kc_host(), "kc2": make_kc2_host(),
    }


def kernel(**inputs):
    x = np.asarray(inputs["x"], np.float32)
    positions = np.asarray(inputs["positions"], np.int32)
    p = {k: np.asarray(v) for k, v in inputs.items() if k not in ("x", "positions")}
    nb, ntok, _ = x.shape
    nc = build_full(ntok, DEPTH)
    in_maps = [make_in_map(x[b], positions[b], p) for b in range(nb)]
    res = run_bass_kernel_spmd(nc, in_maps, core_ids=list(range(nb)))
    out = np.stack([np.ascontiguousarray(res.results[b]["xTo"].T) for b in range(nb)])
    return out.astype(np.float32)
```

```python
import numpy as np
from contextlib import ExitStack
import concourse.bass as bass
import concourse.mybir as mybir
from concourse.bass_utils import run_bass_kernel_spmd

F32 = mybir.dt.float32
BF16 = mybir.dt.bfloat16
I32 = mybir.dt.int32
AF = mybir.ActivationFunctionType
ALU = mybir.AluOpType
AX = mybir.AxisListType

D_MODEL = 1024
SEQ = 16384
DEPTH = 4
NCORE = 8
SEG = 2048
NSEG = 8
TOK = 4096
T = 512
NT = TOK // T
NPROJ = 3584
SEM_ROT = 24000


class Op:
    __slots__ = ("eng", "fn", "waits", "sem", "val", "dma", "idx")


class Prog:
    ENGS = ("pe", "act", "dve", "pool", "sp")

    def __init__(self, nc, es, ndma_sems=14):
        self.nc = nc
        self.es = es
        self.ops = {e: [] for e in self.ENGS}
        self.count = {e: 0 for e in self.ENGS}
        self.last_op = {e: None for e in self.ENGS}
        self.last_w = {}
        self.readers = {}
        self.waited = {e: {} for e in self.ENGS}
        self.eng_sems = {e: [] for e in self.ENGS}
        self.dsems = [es.enter_context(nc.semaphore(f"dma{i}")) for i in range(ndma_sems)]
        self.ndma = 0
        self.dma_ops = []
        self.banks = [es.enter_context(nc.psum_tensor(f"psbank{i}", [128, T], F32)) for i in range(8)]
        self.alias = {}
        self.nbank = 0

    def ps_reset(self):
        self.nbank = 0

    def bank(self, name):
        k = self.nbank % 8
        self.nbank += 1
        self.alias[name] = ("PS", k)
        return self.banks[k]

    def canon(self, k):
        if isinstance(k, str):
            return self.alias.get(k, k)
        if isinstance(k, tuple) and len(k) == 2 and isinstance(k[0], str):
            return self.alias.get(k[0] + str(k[1]), k)
        return k

    def _eng_sem(self, eng, k):
        lst = self.eng_sems[eng]
        while len(lst) <= k:
            lst.append(self.es.enter_context(self.nc.semaphore(f"s_{eng}{len(lst)}")))
        return lst[k]

    def _need(self, op, d, raw):
        if d is None or d is op:
            return
        if not d.dma and d.eng == op.eng:
            if op.eng == "pe" or op.eng == "sp":
                return
        key = id(d.sem)
        if self.waited[op.eng].get(key, 0) >= d.val:
            return
        self.waited[op.eng][key] = d.val
        op.waits.append((d.sem, d.val))

    def add(self, eng, fn, reads=(), writes=(), dma=False):
        op = Op()
        op.eng, op.fn, op.waits, op.dma = eng, fn, [], dma
        op.idx = self.count[eng]
        self.count[eng] += 1
        reads = [self.canon(k) for k in reads]
        writes = [self.canon(k) for k in writes]
        if dma:
            n = self.ndma
            R = len(self.dsems)
            op.sem = self.dsems[n % R]
            op.val = 16 * (n // R + 1)
            if n >= R:
                prev = self.dma_ops[n - R]
                self._need(op, prev, True)
            self.ndma += 1
            self.dma_ops.append(op)
        else:
            k = op.idx // SEM_ROT
            op.sem = self._eng_sem(eng, k)
            op.val = op.idx % SEM_ROT + 1
        for k in reads:
            self._need(op, self.last_w.get(k), True)
        for k in writes:
            self._need(op, self.last_w.get(k), False)
            for r in self.readers.get(k, ()):
                self._need(op, r, False)
        for k in reads:
            lst = self.readers.setdefault(k, [])
            if not dma:
                lst[:] = [r for r in lst if r.dma or r.eng != eng]
            lst.append(op)
        for k in writes:
            self.last_w[k] = op
            self.readers[k] = []
        self.ops[eng].append(op)
        self.last_op[eng] = op
        return op

    def phase_end(self):
        R = len(self.dsems)
        for eng in self.ENGS:
            op = Op()
            op.eng, op.fn, op.waits, op.dma, op.idx = eng, None, [], False, -1
            for x in self.ENGS:
                if x != eng and x != "sp" and self.last_op[x] is not None:
                    self._need(op, self.last_op[x], True)
            for d in self.dma_ops[-R:]:
                self._need(op, d, True)
            self.ops[eng].append(op)
        self.emit()
        self.ops = {e: [] for e in self.ENGS}
        self.last_w = {}
        self.readers = {}
        self.alias = {}
        self.nbank = 0

    def pe(self, fn, reads=(), writes=()):
        return self.add("pe", fn, reads, writes)

    def act(self, fn, reads=(), writes=()):
        return self.add("act", fn, reads, writes)

    def dve(self, fn, reads=(), writes=()):
        return self.add("dve", fn, reads, writes)

    def pool(self, fn, reads=(), writes=()):
        return self.add("pool", fn, reads, writes)

    def dma(self, out, in_, reads=(), writes=(), **kw):
        return self.add("sp", lambda e: e.dma_start(out=out, in_=in_, **kw), reads, writes, dma=True)

    def finish(self):
        pass

    def emit(self):
        nc = self.nc
        with nc.Block() as block:
            def replay(name, e):
                for op in self.ops[name]:
                    for (s, v) in op.waits:
                        e.wait_ge(s, v)
                    if op.fn is None:
                        continue
                    ins = op.fn(e)
                    ins.then_inc(op.sem, 16 if op.dma else 1)

            @block.tensor
            def _(e):
                replay("pe", e)

            @block.scalar
            def _(e):
                replay("act", e)

            @block.vector
            def _(e):
                replay("dve", e)

            @block.gpsimd
            def _(e):
                replay("pool", e)

            @block.sync
            def _(e):
                replay("sp", e)


_UNIQ = [0]


def sb(nc, es, name, shape, dt):
    _UNIQ[0] += 1
    return es.enter_context(nc.sbuf_tensor(f"{name}_{_UNIQ[0]}", list(shape), dt))


def psb(P, name):
    return P.bank(name)


def emit_l1(nc, P, es, xT, w_in, V, projT, tag="l1", ntok=TOK):
    NCC = NPROJ // 128
    Wb = sb(nc, es, tag + "Wb", [128, 8, NPROJ], BF16)
    Wst = [sb(nc, es, tag + f"Wst{i}", [128, NPROJ], F32) for i in range(2)]
    X = [sb(nc, es, tag + f"X{i}", [128, 8, T], F32) for i in range(2)]
    Xsq = sb(nc, es, tag + "Xsq", [128, 8, T], BF16)
    H = [sb(nc, es, tag + f"H{i}", [128, 8, T], BF16) for i in range(2)]
    ones = sb(nc, es, tag + "ones", [128, 128], BF16)
    rs = sb(nc, es, tag + "rs", [128, T], F32)
    O = [sb(nc, es, tag + f"O{i}", [128, T], F32) for i in range(4)]
    ps_ss = psb(P, tag + "ps_ss")
    ps = [psb(P, tag + f"ps{i}") for i in range(4)]

    P.pool(lambda e: e.memset(ones[:], 1.0), writes=["ones"])
    w_v = w_in.rearrange("(kc p) n -> kc p n", p=128)
    for kc in range(8):
        st = Wst[kc % 2]
        P.dma(st[:], w_v[kc], writes=[("Wst", kc % 2)])
        P.pool(lambda e, st=st, kc=kc: e.tensor_copy(out=Wb[:, kc, :], in_=st[:]),
               reads=[("Wst", kc % 2)], writes=[("Wb", kc)])
    x_v = xT.rearrange("(kc p) t -> p kc t", p=128)
    nout = 0
    for it in range(ntok // T):
        xs = X[it % 2]
        hs = H[it % 2]
        ts = slice(it * T, (it + 1) * T)
        P.dma(xs[:], x_v[:, :, ts], reads=[("xT", it)], writes=[("X", it % 2)])
        P.act(lambda e, xs=xs: e.activation(out=Xsq[:], in_=xs[:], func=AF.Square),
              reads=[("X", it % 2)], writes=["Xsq"])
        for kc in range(8):
            P.pe(lambda e, kc=kc: e.matmul(ps_ss[:], lhsT=ones[:], rhs=Xsq[:, kc, :],
                                           start=(kc == 0), stop=(kc == 7)),
                 reads=["ones", "Xsq"], writes=[tag + "ps_ss"])
        P.dve(lambda e: e.tensor_scalar(out=rs[:], in0=ps_ss[:], scalar1=1.0 / D_MODEL, scalar2=1e-6,
                                        op0=ALU.mult, op1=ALU.add),
              reads=[tag + "ps_ss"], writes=["rs"])
        P.act(lambda e: e.activation(out=rs[:], in_=rs[:], func=AF.Sqrt), reads=["rs"], writes=["rs"])
        P.dve(lambda e: e.reciprocal(out=rs[:], in_=rs[:]), reads=["rs"], writes=["rs"])
        for kc in range(8):
            P.dve(lambda e, kc=kc, xs=xs, hs=hs: e.scalar_tensor_tensor(
                out=hs[:, kc, :], in0=xs[:, kc, :], scalar=vcol(V, "pre_g", kc), in1=rs[:],
                op0=ALU.mult, op1=ALU.mult),
                reads=[("X", it % 2), "V", "rs"], writes=[("H", it % 2, kc)])
        for cc in range(NCC):
            pb = ps[cc % 4]
            for kc in range(8):
                P.pe(lambda e, kc=kc, cc=cc, pb=pb, hs=hs: e.matmul(
                    pb[:], lhsT=Wb[:, kc, cc * 128:(cc + 1) * 128], rhs=hs[:, kc, :],
                    start=(kc == 0), stop=(kc == 7)),
                    reads=[("Wb", kc), ("H", it % 2, kc)], writes=[(tag + "ps", cc % 4)])
            ob = O[nout % 4]
            if cc % 2 == 0:
                P.act(lambda e, ob=ob, pb=pb: e.copy(out=ob[:], in_=pb[:]),
                      reads=[(tag + "ps", cc % 4)], writes=[("O", nout % 4)])
            else:
                P.dve(lambda e, ob=ob, pb=pb: e.tensor_copy(out=ob[:], in_=pb[:]),
                      reads=[(tag + "ps", cc % 4)], writes=[("O", nout % 4)])
            P.dma(projT[cc * 128:(cc + 1) * 128, ts], ob[:], reads=[("O", nout % 4)],
                  writes=[("projT", it, cc)])
            nout += 1


VEC_SPEC = [("pre_g", 8), ("post_g", 8), ("a_dw_w", 62), ("a_dw_b", 2), ("a_ln_g", 2), ("a_ln_b", 2),
            ("a_pw_b", 2), ("b_lb", 8), ("b_norm_g", 2), ("c_conv_w", 24), ("c_conv_b", 6),
            ("c_norm_g", 2), ("d_qa_g", 2), ("d_kva_g", 1), ("c_dt_bias", 1), ("c_a_log", 1), ("c_d", 2)]
VEC_OFF = {}
_o = 0
for _n, _w in VEC_SPEC:
    VEC_OFF[_n] = (_o, _w)
    _o += _w
NV = _o


def vcol(V, name, j=0, n=1, rows=128):
    o, w = VEC_OFF[name]
    return V[0:rows, o + j:o + j + n]


def emit_A(nc, P, es, projT, V, pw_w, ycatT, ntok, tag="A"):
    HAL = 32
    val = [sb(nc, es, tag + f"val{i}", [128, 2, T], F32) for i in range(2)]
    glu = [sb(nc, es, tag + f"glu{i}", [128, 2, T], F32) for i in range(2)]
    gat = [sb(nc, es, tag + f"gat{i}", [128, 2, T], F32) for i in range(2)]
    gb = sb(nc, es, tag + "gb", [128, 2, HAL + T], F32)
    acc = sb(nc, es, tag + "acc", [128, 2, T], F32)
    sq = sb(nc, es, tag + "sq", [128, 2, T], F32)
    mean = sb(nc, es, tag + "mean", [128, T], F32)
    rstd = sb(nc, es, tag + "rstd", [128, T], F32)
    hs = sb(nc, es, tag + "hs", [128, 2, T], BF16)
    ya = [sb(nc, es, tag + f"ya{i}", [128, 2, T], BF16) for i in range(2)]
    onesf = sb(nc, es, tag + "onesf", [128, 128], F32)
    pwst = sb(nc, es, tag + "pwst", [128, 2, 256], F32)
    pwb = sb(nc, es, tag + "pwb", [128, 2, 256], BF16)
    ps1 = psb(P, tag + "ps1")
    ps2 = psb(P, tag + "ps2")
    pso = [psb(P, tag + f"pso{i}") for i in range(2)]

    P.pool(lambda e: e.memset(onesf[:], 1.0), writes=[tag + "onesf"])
    P.dma(pwst[:], pw_w.rearrange("(c p) n -> p c n", p=128), writes=[tag + "pwst"])
    P.pool(lambda e: e.tensor_copy(out=pwb[:], in_=pwst[:]), reads=[tag + "pwst"], writes=[tag + "pwb"])
    P.pool(lambda e: e.memset(gb[:, :, 0:HAL], 0.0), writes=[tag + "gb"])
    pv = projT.rearrange("(c p) t -> p c t", p=128)
    for it in range(ntok // T):
        ts = slice(it * T, (it + 1) * T)
        b = it % 2
        P.dma(val[b][:], pv[:, 0:2, ts], reads=[("projT", it)], writes=[(tag + "val", b)])
        P.dma(glu[b][:], pv[:, 2:4, ts], reads=[("projT", it)], writes=[(tag + "glu", b)])
        P.dma(gat[b][:], pv[:, 4:6, ts], reads=[("projT", it)], writes=[(tag + "gat", b)])
        P.act(lambda e, b=b: e.activation(out=glu[b][:], in_=glu[b][:], func=AF.Sigmoid),
              reads=[(tag + "glu", b)], writes=[(tag + "glu", b)])
        P.dve(lambda e, b=b: e.tensor_tensor(out=gb[:, :, HAL:HAL + T], in0=val[b][:], in1=glu[b][:], op=ALU.mult),
              reads=[(tag + "glu", b), (tag + "val", b)], writes=[tag + "gb"])
        P.act(lambda e, b=b: e.activation(out=gat[b][:], in_=gat[b][:], func=AF.Silu),
              reads=[(tag + "gat", b)], writes=[(tag + "gat", b)])
        for c in range(2):
            eng = P.dve
            for j in range(31):
                src = gb[:, c, HAL - 30 + j:HAL - 30 + j + T]
                wj = vcol(V, "a_dw_w", c * 31 + j)
                if j == 0:
                    eng(lambda e, c=c, src=src, wj=wj: e.tensor_scalar(
                        out=acc[:, c, :], in0=src, scalar1=wj, scalar2=vcol(V, "a_dw_b", c),
                        op0=ALU.mult, op1=ALU.add),
                        reads=[tag + "gb", "V"], writes=[(tag + "acc", c)])
                else:
                    eng(lambda e, c=c, src=src, wj=wj: e.scalar_tensor_tensor(
                        out=acc[:, c, :], in0=src, scalar=wj, in1=acc[:, c, :], op0=ALU.mult, op1=ALU.add),
                        reads=[tag + "gb", "V", (tag + "acc", c)], writes=[(tag + "acc", c)])
        P.act(lambda e: e.copy(out=gb[:, :, 0:HAL], in_=gb[:, :, T:T + HAL]),
              reads=[tag + "gb"], writes=[tag + "gb"])
        P.act(lambda e: e.activation(out=sq[:], in_=acc[:], func=AF.Square),
              reads=[(tag + "acc", 0), (tag + "acc", 1)], writes=[tag + "sq"])
        for c in range(2):
            P.pe(lambda e, c=c: e.matmul(ps1[:], lhsT=onesf[:], rhs=acc[:, c, :], start=(c == 0), stop=(c == 1)),
                 reads=[tag + "onesf", (tag + "acc", c)], writes=[tag + "ps1"])
        for c in range(2):
            P.pe(lambda e, c=c: e.matmul(ps2[:], lhsT=onesf[:], rhs=sq[:, c, :], start=(c == 0), stop=(c == 1)),
                 reads=[tag + "onesf", tag + "sq"], writes=[tag + "ps2"])
        P.dve(lambda e: e.tensor_scalar(out=mean[:], in0=ps1[:], scalar1=1.0 / 256, scalar2=None, op0=ALU.mult),
              reads=[tag + "ps1"], writes=[tag + "mean"])
        P.dve(lambda e: e.tensor_tensor(out=rstd[:], in0=mean[:], in1=mean[:], op=ALU.mult),
              reads=[tag + "mean"], writes=[tag + "rstd"])
        P.dve(lambda e: e.scalar_tensor_tensor(out=rstd[:], in0=ps2[:], scalar=1.0 / 256, in1=rstd[:],
                                               op0=ALU.mult, op1=ALU.subtract),
              reads=[tag + "ps2", tag + "rstd"], writes=[tag + "rstd"])
        P.dve(lambda e: e.tensor_scalar(out=rstd[:], in0=rstd[:], scalar1=1e-5, scalar2=None, op0=ALU.add),
              reads=[tag + "rstd"], writes=[tag + "rstd"])
        P.act(lambda e: e.activation(out=rstd[:], in_=rstd[:], func=AF.Sqrt), reads=[tag + "rstd"], writes=[tag + "rstd"])
        P.dve(lambda e: e.reciprocal(out=rstd[:], in_=rstd[:]), reads=[tag + "rstd"], writes=[tag + "rstd"])
        for c in range(2):
            P.dve(lambda e, c=c: e.tensor_tensor(out=acc[:, c, :], in0=acc[:, c, :], in1=mean[:], op=ALU.subtract),
                  reads=[(tag + "acc", c), tag + "mean", tag + "ps1"], writes=[(tag + "acc", c)])
            P.dve(lambda e, c=c: e.tensor_tensor(out=acc[:, c, :], in0=acc[:, c, :], in1=rstd[:], op=ALU.mult),
                  reads=[(tag + "acc", c), tag + "rstd"], writes=[(tag + "acc", c)])
            P.act(lambda e, c=c: e.activation(out=hs[:, c, :], in_=acc[:, c, :], func=AF.Silu,
                                              scale=vcol(V, "a_ln_g", c), bias=vcol(V, "a_ln_b", c)),
                  reads=[(tag + "acc", c), "V"], writes=[(tag + "hs", c)])
        for co in range(2):
            for ci in range(2):
                P.pe(lambda e, co=co, ci=ci: e.matmul(pso[co][:], lhsT=pwb[:, ci, co * 128:(co + 1) * 128],
                                                      rhs=hs[:, ci, :], start=(ci == 0), stop=(ci == 1)),
                     reads=[tag + "pwb", (tag + "hs", ci)], writes=[(tag + "pso", co)])
            P.dve(lambda e, co=co, b=b: e.scalar_tensor_tensor(
                out=ya[b][:, co, :], in0=pso[co][:], scalar=vcol(V, "a_pw_b", co), in1=gat[b][:, co, :],
                op0=ALU.add, op1=ALU.mult),
                reads=[(tag + "pso", co), (tag + "gat", b), "V"], writes=[(tag + "ya", b)])
        P.dma(ycatT.rearrange("(c p) t -> p c t", p=128)[:, 0:2, ts], ya[b][:],
              reads=[(tag + "ya", b)], writes=[("ycatA", it)])


COLMAP = [(0, 768), (768, 1792), (1792, 2560), (2564, 2820), (2820, 3076), (3076, 3204),
          (3236, 3492), (3204, 3236), (2560, 2564)]
R_VAL, R_GLU, R_AG = 0, 256, 512
R_BQ, R_BF, R_BI, R_BG = 768, 1024, 1280, 1536
R_XBC, R_Z, R_CQ, R_CKV, R_DG, R_KR, R_DT = 1792, 2560, 2816, 3072, 3200, 3456, 3488


def pack_w_in(w):
    out = np.zeros((D_MODEL, NPROJ), np.float32)
    o = 0
    for a, b in COLMAP:
        out[:, o:o + b - a] = w[:, a:b]
        o += b - a
    return out


def _pc(v):
    return np.ascontiguousarray(np.asarray(v, np.float32).reshape(-1, 128).T)


def pack_vecs(p, l):
    V = np.zeros((128, NV), np.float32)

    def put(name, arr):
        o, w = VEC_OFF[name]
        V[:arr.shape[0], o:o + arr.shape[1]] = arr

    put("pre_g", _pc(p["pre_norm_g"][l]))
    put("post_g", _pc(p["post_norm_g"][l]))
    w = np.asarray(p["a_dw_w"][l], np.float32)
    put("a_dw_w", np.concatenate([w[:, c * 128:(c + 1) * 128].T for c in range(2)], axis=1))
    put("a_dw_b", _pc(p["a_dw_b"][l]))
    put("a_ln_g", _pc(p["a_ln_g"][l]))
    put("a_ln_b", _pc(p["a_ln_b"][l]))
    put("a_pw_b", _pc(p["a_pw_b"][l]))
    lg = np.asarray(p["b_lb_logits"], np.float32)
    put("b_lb", np.concatenate([lg[:, c * 128:(c + 1) * 128].T for c in range(2)], axis=1))
    put("b_norm_g", _pc(p["b_norm_g"][l]))
    cw = np.asarray(p["c_conv_w"][l], np.float32)
    put("c_conv_w", np.concatenate([cw[:, c * 128:(c + 1) * 128].T for c in range(6)], axis=1))
    put("c_conv_b", _pc(p["c_conv_b"][l]))
    put("c_norm_g", _pc(p["c_norm_g"][l]))
    put("d_qa_g", _pc(p["d_qa_g"][l]))
    put("d_kva_g", _pc(p["d_kva_g"][l]))
    for nm in ("c_dt_bias", "c_a_log"):
        col = np.zeros((128, 1), np.float32)
        col[0:4, 0] = np.asarray(p[nm][l], np.float32)
        col[64:68, 0] = np.asarray(p[nm][l], np.float32)
        put(nm, col)
    cd = np.asarray(p["c_d"][l], np.float32)
    put("c_d", np.stack([np.repeat(cd[2 * g:2 * g + 2], 64) for g in range(2)], axis=1))
    return V


def emit_O(nc, P, es, ycatT, xT_in, V, w_out, xT_out, ntok, tag="O"):
    Wo = sb(nc, es, tag + "Wo", [128, 8, D_MODEL], BF16)
    Wst = [sb(nc, es, tag + f"Wst{i}", [128, D_MODEL], F32) for i in range(2)]
    Y = [sb(nc, es, tag + f"Y{i}", [128, 8, T], BF16) for i in range(2)]
    X = [sb(nc, es, tag + f"X{i}", [128, 8, T], F32) for i in range(2)]
    Yo = sb(nc, es, tag + "Yo", [128, 8, T], F32)
    Ysq = sb(nc, es, tag + "Ysq", [128, 8, T], BF16)
    ones = sb(nc, es, tag + "ones", [128, 128], BF16)
    rs = sb(nc, es, tag + "rs", [128, T], F32)
    ps = [psb(P, tag + f"ps{i}") for i in range(4)]
    ps_ss = psb(P, tag + "ps_ss")
    P.pool(lambda e: e.memset(ones[:], 1.0), writes=[tag + "ones"])
    w_v = w_out.rearrange("(kc p) n -> kc p n", p=128)
    for kc in range(8):
        st = Wst[kc % 2]
        P.dma(st[:], w_v[kc], writes=[(tag + "Wst", kc % 2)])
        P.pool(lambda e, st=st, kc=kc: e.tensor_copy(out=Wo[:, kc, :], in_=st[:]),
               reads=[(tag + "Wst", kc % 2)], writes=[(tag + "Wo", kc)])
    yv = ycatT.rearrange("(c p) t -> p c t", p=128)
    xv = xT_in.rearrange("(c p) t -> p c t", p=128)
    xo = xT_out.rearrange("(c p) t -> p c t", p=128)
    for it in range(ntok // T):
        ts = slice(it * T, (it + 1) * T)
        b = it % 2
        P.dma(Y[b][:], yv[:, :, ts], reads=[("ycatA", it), ("ycatB", it), ("ycatC", it), ("ycatD", it)],
              writes=[(tag + "Y", b)])
        P.dma(X[b][:], xv[:, :, ts], reads=[("xT", it)], writes=[(tag + "X", b)])
        for do in range(8):
            pb = ps[do % 4]
            for kc in range(8):
                P.pe(lambda e, kc=kc, do=do, pb=pb, b=b: e.matmul(
                    pb[:], lhsT=Wo[:, kc, do * 128:(do + 1) * 128], rhs=Y[b][:, kc, :],
                    start=(kc == 0), stop=(kc == 7)),
                    reads=[(tag + "Wo", kc), (tag + "Y", b)], writes=[(tag + "ps", do % 4)])
            P.act(lambda e, do=do, pb=pb: e.copy(out=Yo[:, do, :], in_=pb[:]),
                  reads=[(tag + "ps", do % 4)], writes=[(tag + "Yo", do)])
            P.act(lambda e, do=do, pb=pb: e.activation(out=Ysq[:, do, :], in_=pb[:], func=AF.Square),
                  reads=[(tag + "ps", do % 4)], writes=[(tag + "Ysq", do)])
        for do in range(8):
            P.pe(lambda e, do=do: e.matmul(ps_ss[:], lhsT=ones[:], rhs=Ysq[:, do, :],
                                           start=(do == 0), stop=(do == 7)),
                 reads=[tag + "ones", (tag + "Ysq", do)], writes=[tag + "ps_ss"])
        P.dve(lambda e: e.tensor_scalar(out=rs[:], in0=ps_ss[:], scalar1=1.0 / D_MODEL, scalar2=1e-6,
                                        op0=ALU.mult, op1=ALU.add), reads=[tag + "ps_ss"], writes=[tag + "rs"])
        P.act(lambda e: e.activation(out=rs[:], in_=rs[:], func=AF.Sqrt), reads=[tag + "rs"], writes=[tag + "rs"])
        P.dve(lambda e: e.reciprocal(out=rs[:], in_=rs[:]), reads=[tag + "rs"], writes=[tag + "rs"])
        for do in range(8):
            P.dve(lambda e, do=do: e.scalar_tensor_tensor(
                out=Yo[:, do, :], in0=Yo[:, do, :], scalar=vcol(V, "post_g", do), in1=rs[:],
                op0=ALU.mult, op1=ALU.mult), reads=[(tag + "Yo", do), tag + "rs", "V"], writes=[(tag + "Yo", do)])
            P.dve(lambda e, do=do, b=b: e.tensor_tensor(out=Yo[:, do, :], in0=Yo[:, do, :], in1=X[b][:, do, :],
                                                        op=ALU.add),
                   reads=[(tag + "Yo", do), (tag + "X", b)], writes=[(tag + "Yo", do)])
        P.dma(xo[:, :, ts], Yo[:], reads=[(tag + "Yo", do) for do in range(8)], writes=[("xTo", it)])


NCP = 8
TWO_PI = float(2 * np.pi)


def make_cp():
    cp = np.zeros((128, NCP), np.float32)
    inv = (10000.0 ** (-np.arange(0, 32, 2, dtype=np.float32) / 32)).astype(np.float32)
    cp[64:96, 0] = np.concatenate([inv, inv])
    cp[64:96, 1] = np.concatenate([-np.ones(16), np.ones(16)])
    cp[64:96, 2] = np.concatenate([-np.pi * np.ones(16), np.pi * np.ones(16)])
    cp[:, 3] = np.pi
    cp[:, 4] = np.pi / 2
    return cp


def pack_qb(w):
    w = np.asarray(w, np.float32).reshape(256, 4, 96)
    b = w.copy()
    b[:, :, 64:80] = w[:, :, 80:96]
    b[:, :, 80:96] = w[:, :, 64:80]
    return np.ascontiguousarray(np.stack([w, b], axis=2))


def pack_kvb(w):
    w = np.asarray(w, np.float32).reshape(128, 4, 128)
    return np.ascontiguousarray(np.stack([w[:, :, 0:64].reshape(128, 256), w[:, :, 64:128].reshape(128, 256)], axis=1))


def emit_D(nc, P, es, projT, pos, V, CP, qbw, kvbw, QT, KT, VD, OD, ycatT, ntok, tag="D"):
    nt = ntok // T
    SC = float(96 ** -0.5)
    qst = sb(nc, es, tag + "qst", [128, 2, 768], F32)
    qb = sb(nc, es, tag + "qb", [128, 2, 768], BF16)
    kst = sb(nc, es, tag + "kst", [128, 512], F32)
    kb_ = sb(nc, es, tag + "kb", [128, 512], BF16)
    ones = sb(nc, es, tag + "ones", [128, 128], BF16)
    cq = [sb(nc, es, tag + f"cq{i}", [128, 2, T], F32) for i in range(2)]
    ckv = [sb(nc, es, tag + f"ckv{i}", [128, T], F32) for i in range(2)]
    krA = [sb(nc, es, tag + f"krA{i}", [128, T], F32) for i in range(2)]
    krB = [sb(nc, es, tag + f"krB{i}", [128, T], F32) for i in range(2)]
    posi = [sb(nc, es, tag + f"posi{i}", [128, T], I32) for i in range(2)]
    sqb = sb(nc, es, tag + "sqb", [128, 3, T], BF16)
    rs = sb(nc, es, tag + "rs", [128, T], F32)
    rs2 = sb(nc, es, tag + "rs2", [128, T], F32)
    cqn = sb(nc, es, tag + "cqn", [128, 2, T], BF16)
    ckvn = sb(nc, es, tag + "ckvn", [128, T], BF16)
    ang = sb(nc, es, tag + "ang", [128, T], F32)
    cosT = sb(nc, es, tag + "cos", [128, T], F32)
    sinT = sb(nc, es, tag + "sin", [128, T], F32)
    tmp = sb(nc, es, tag + "tmp", [128, T], F32)
    tmp2 = sb(nc, es, tag + "tmp2", [128, T], F32)
    kf = sb(nc, es, tag + "kf", [128, T], F32)
    ki = sb(nc, es, tag + "ki", [128, T], I32)
    qt = [sb(nc, es, tag + f"qt{i}", [96, 4, T], BF16) for i in range(2)]
    kt = [sb(nc, es, tag + f"kt{i}", [96, 4, T], BF16) for i in range(2)]
    vt = [sb(nc, es, tag + f"vt{i}", [128, 4, 4, 65], BF16) for i in range(2)]
    ps_a = psb(P, tag + "ps_a")
    ps_b = psb(P, tag + "ps_b")
    ps_q = [psb(P, tag + f"ps_q{i}") for i in range(2)]
    ps_q2 = [psb(P, tag + f"ps_q2{i}") for i in range(2)]
    ps_v = psb(P, tag + "ps_v")

    P.pool(lambda e: e.memset(ones[:], 1.0), writes=[tag + "ones"])
    P.dma(qst[:], qbw.rearrange("(c p) n -> p c n", p=128), writes=[tag + "qst"])
    P.pool(lambda e: e.tensor_copy(out=qb[:], in_=qst[:]), reads=[tag + "qst"], writes=[tag + "qb"])
    P.dma(kst[:], kvbw[:, :], writes=[tag + "kst"])
    P.pool(lambda e: e.tensor_copy(out=kb_[:], in_=kst[:]), reads=[tag + "kst"], writes=[tag + "kb"])
    for i in range(2):
        P.pool(lambda e, i=i: e.memset(vt[i][:, :, :, 64:65], 1.0), writes=[(tag + "vt", i)])
    pv = projT.rearrange("(c p) t -> p c t", p=128)
    QTv = QT.rearrange("h d t -> d h t")
    KTv = KT.rearrange("h d t -> d h t")
    for it in range(nt):
        ts = slice(it * T, (it + 1) * T)
        b = it % 2
        P.dma(cq[b][:], pv[:, 22:24, ts], reads=[("projT", it)], writes=[(tag + "cq", b)])
        P.dma(ckv[b][:], projT[R_CKV:R_CKV + 128, ts], reads=[("projT", it)], writes=[(tag + "ckv", b)])
        P.dma(krA[b][64:96, :], projT[R_KR:R_KR + 32, ts], reads=[("projT", it)], writes=[(tag + "krA", b)])
        P.dma(krB[b][64:80, :], projT[R_KR + 16:R_KR + 32, ts], reads=[("projT", it)], writes=[(tag + "krB", b)])
        P.dma(krB[b][80:96, :], projT[R_KR:R_KR + 16, ts], reads=[("projT", it)], writes=[(tag + "krB", b)])
        P.dma(posi[b][64:96, :], pos[0:1, ts].partition_broadcast(32), writes=[(tag + "posi", b)])
        P.act(lambda e, b=b: e.activation(out=sqb[:, 0:2, :], in_=cq[b][:], func=AF.Square),
              reads=[(tag + "cq", b)], writes=[tag + "sqq"])
        P.act(lambda e, b=b: e.activation(out=sqb[:, 2, :], in_=ckv[b][:], func=AF.Square),
              reads=[(tag + "ckv", b)], writes=[tag + "sqk"])
        for c in range(2):
            P.pe(lambda e, c=c: e.matmul(ps_a[:], lhsT=ones[:], rhs=sqb[:, c, :], start=(c == 0), stop=(c == 1)),
                 reads=[tag + "ones", tag + "sqq"], writes=[tag + "ps_a"])
        P.pe(lambda e: e.matmul(ps_b[:], lhsT=ones[:], rhs=sqb[:, 2, :], start=True, stop=True),
             reads=[tag + "ones", tag + "sqk"], writes=[tag + "ps_b"])
        for (pss, r, n) in ((ps_a, rs, 256.0), (ps_b, rs2, 128.0)):
            k1, k2 = (tag + "ps_a", tag + "rs") if pss is ps_a else (tag + "ps_b", tag + "rs2")
            P.dve(lambda e, pss=pss, r=r, n=n: e.tensor_scalar(out=r[:], in0=pss[:], scalar1=1.0 / n, scalar2=1e-6,
                                                             op0=ALU.mult, op1=ALU.add), reads=[k1], writes=[k2])
            P.act(lambda e, r=r: e.activation(out=r[:], in_=r[:], func=AF.Sqrt), reads=[k2], writes=[k2])
            P.dve(lambda e, r=r: e.reciprocal(out=r[:], in_=r[:]), reads=[k2], writes=[k2])
        for c in range(2):
            P.dve(lambda e, c=c, b=b: e.scalar_tensor_tensor(out=cqn[:, c, :], in0=cq[b][:, c, :],
                                                            scalar=vcol(V, "d_qa_g", c), in1=rs[:],
                                                            op0=ALU.mult, op1=ALU.mult),
                  reads=[(tag + "cq", b), tag + "rs", "V"], writes=[tag + "cqn"])
        P.dve(lambda e, b=b: e.scalar_tensor_tensor(out=ckvn[:], in0=ckv[b][:], scalar=vcol(V, "d_kva_g"),
                                                     in1=rs2[:], op0=ALU.mult, op1=ALU.mult),
              reads=[(tag + "ckv", b), tag + "rs2", "V"], writes=[tag + "ckvn"])
        R_ = slice(64, 96)
        P.dve(lambda e, b=b: e.tensor_copy(out=ang[R_, :], in_=posi[b][R_, :]), reads=[(tag + "posi", b)],
              writes=[tag + "ang"])
        P.dve(lambda e: e.tensor_scalar(out=ang[R_, :], in0=ang[R_, :], scalar1=CP[R_, 0:1], scalar2=None,
                                        op0=ALU.mult), reads=[tag + "ang", "CP"], writes=[tag + "ang"])
        def reduce_angle(dst, kd, add_half_pi):
            if add_half_pi:
                P.dve(lambda e: e.tensor_scalar(out=dst[R_, :], in0=ang[R_, :], scalar1=CP[R_, 4:5], scalar2=None,
                                                op0=ALU.add), reads=[tag + "ang", "CP"], writes=[kd])
            else:
                P.dve(lambda e: e.tensor_copy(out=dst[R_, :], in_=ang[R_, :]), reads=[tag + "ang"], writes=[kd])
            P.dve(lambda e: e.tensor_scalar(out=kf[R_, :], in0=dst[R_, :], scalar1=1.0 / TWO_PI, scalar2=None,
                                            op0=ALU.mult), reads=[kd], writes=[tag + "kf"])
            P.dve(lambda e: e.tensor_copy(out=ki[R_, :], in_=kf[R_, :]), reads=[tag + "kf"], writes=[tag + "ki"])
            P.dve(lambda e: e.tensor_copy(out=kf[R_, :], in_=ki[R_, :]), reads=[tag + "ki"], writes=[tag + "kf"])
            P.dve(lambda e: e.scalar_tensor_tensor(out=dst[R_, :], in0=kf[R_, :], scalar=-6.28125, in1=dst[R_, :],
                                                   op0=ALU.mult, op1=ALU.add), reads=[tag + "kf", kd], writes=[kd])
            P.dve(lambda e: e.scalar_tensor_tensor(out=dst[R_, :], in0=kf[R_, :], scalar=-(TWO_PI - 6.28125),
                                                   in1=dst[R_, :], op0=ALU.mult, op1=ALU.add),
                  reads=[tag + "kf", kd], writes=[kd])
            P.dve(lambda e: e.tensor_scalar(out=kf[R_, :], in0=dst[R_, :], scalar1=float(np.pi), scalar2=None,
                                            op0=ALU.is_gt), reads=[kd], writes=[tag + "kf"])
            P.dve(lambda e: e.scalar_tensor_tensor(out=dst[R_, :], in0=kf[R_, :], scalar=-TWO_PI, in1=dst[R_, :],
                                                   op0=ALU.mult, op1=ALU.add), reads=[tag + "kf", kd], writes=[kd])

        reduce_angle(tmp, tag + "tmp", False)
        P.act(lambda e: e.activation(out=sinT[R_, :], in_=tmp[R_, :], func=AF.Sin, scale=CP[R_, 1:2]),
              reads=[tag + "tmp", "CP"], writes=[tag + "sin"])
        reduce_angle(tmp2, tag + "tmp2", True)
        P.act(lambda e: e.activation(out=cosT[R_, :], in_=tmp2[R_, :], func=AF.Sin),
              reads=[tag + "tmp2"], writes=[tag + "cos"])
        P.dve(lambda e, b=b: e.tensor_tensor(out=krA[b][R_, :], in0=krA[b][R_, :], in1=cosT[R_, :], op=ALU.mult),
              reads=[(tag + "krA", b), tag + "cos"], writes=[(tag + "krA", b)])
        P.dve(lambda e, b=b: e.tensor_tensor(out=krB[b][R_, :], in0=krB[b][R_, :], in1=sinT[R_, :], op=ALU.mult),
              reads=[(tag + "krB", b), tag + "sin"], writes=[(tag + "krB", b)])
        for h in range(4):
            P.dve(lambda e, b=b, h=h: e.tensor_tensor(out=kt[b][R_, h, :], in0=krA[b][R_, :], in1=krB[b][R_, :],
                                                       op=ALU.add),
                   reads=[(tag + "krA", b), (tag + "krB", b)], writes=[(tag + "kt", b)])
        for h in range(4):
            P.pe(lambda e, h=h: e.matmul(ps_q[h % 2][0:64, :], lhsT=kb_[:, h * 64:(h + 1) * 64], rhs=ckvn[:],
                                         start=True, stop=True),
                 reads=[tag + "kb", tag + "ckvn"], writes=[(tag + "ps_q", h % 2)])
            P.act(lambda e, h=h, b=b: e.copy(out=kt[b][0:64, h, :], in_=ps_q[h % 2][0:64, :]),
                  reads=[(tag + "ps_q", h % 2)], writes=[(tag + "kt", b)])
        for h in range(4):
            pA, pB = ps_q[h % 2], ps_q2[h % 2]
            for c in range(2):
                P.pe(lambda e, h=h, c=c, pA=pA: e.matmul(pA[0:96, :], lhsT=qb[:, c, (h * 2) * 96:(h * 2 + 1) * 96],
                                                         rhs=cqn[:, c, :], start=(c == 0), stop=(c == 1)),
                     reads=[tag + "qb", tag + "cqn"], writes=[(tag + "ps_q", h % 2)])
            for c in range(2):
                P.pe(lambda e, h=h, c=c, pB=pB: e.matmul(pB[0:96, :], lhsT=qb[:, c, (h * 2 + 1) * 96:(h * 2 + 2) * 96],
                                                         rhs=cqn[:, c, :], start=(c == 0), stop=(c == 1)),
                     reads=[tag + "qb", tag + "cqn"], writes=[(tag + "ps_q2", h % 2)])
            P.act(lambda e, h=h, b=b, pA=pA: e.copy(out=qt[b][0:64, h, :], in_=pA[0:64, :]),
                  reads=[(tag + "ps_q", h % 2)], writes=[(tag + "qt", b)])
            P.dve(lambda e, pA=pA: e.tensor_tensor(out=tmp[R_, :], in0=pA[R_, :], in1=cosT[R_, :], op=ALU.mult),
                  reads=[(tag + "ps_q", h % 2), tag + "cos"], writes=[tag + "tmp"])
            P.dve(lambda e, pB=pB: e.tensor_tensor(out=tmp2[R_, :], in0=pB[R_, :], in1=sinT[R_, :], op=ALU.mult),
                  reads=[(tag + "ps_q2", h % 2), tag + "sin"], writes=[tag + "tmp2"])
            P.dve(lambda e, h=h, b=b: e.tensor_tensor(out=qt[b][R_, h, :], in0=tmp[R_, :], in1=tmp2[R_, :], op=ALU.add),
                  reads=[tag + "tmp", tag + "tmp2"], writes=[(tag + "qt", b)])
        for j in range(4):
            P.pe(lambda e, j=j: e.matmul(ps_v[:, 0:256],
                                         lhsT=ckvn[:, j * 128:(j + 1) * 128], rhs=kb_[:, 256:512],
                                         start=True, stop=True),
                 reads=[tag + "ckvn", tag + "kb"], writes=[tag + "ps_v"])
            P.act(lambda e, j=j, b=b: e.copy(out=vt[b][:, j, :, 0:64],
                                             in_=ps_v[:, 0:256].rearrange("p (h v) -> p h v", h=4)),
                  reads=[tag + "ps_v"], writes=[(tag + "vt", b)])
        P.dma(QTv[:, :, ts], qt[b][:], reads=[(tag + "qt", b)], writes=[("QT", it)])
        P.dma(KTv[:, :, ts], kt[b][:], reads=[(tag + "kt", b)], writes=[("KT", it)])
        for h in range(4):
            P.dma(VD[h, :, it * 4:(it + 1) * 4, :], vt[b][:, :, h, :], reads=[(tag + "vt", b)], writes=[("VD", it)])

    P.ps_reset()
    Kall = sb(nc, es, tag + "Kall", [96, ntok], BF16)
    Vall = sb(nc, es, tag + "Vall", [128, ntok // 128, 65], BF16)
    Qg = [sb(nc, es, tag + f"Qg{i}", [96, T], BF16) for i in range(2)]
    Pt = [sb(nc, es, tag + f"Pt{i}", [128, T], BF16) for i in range(3)]
    Ost = [sb(nc, es, tag + f"Ost{i}", [65, T], F32) for i in range(2)]
    S_ps = [psb(P, tag + f"S_ps{i}") for i in range(3)]
    O_ps = [psb(P, tag + f"O_ps{i}") for i in range(2)]
    allk = [("KT", i) for i in range(nt)]
    allv = [("VD", i) for i in range(nt)]
    n = 0
    ng = 0
    for h in range(4):
        P.dma(Kall[:], KT[h], reads=allk, writes=[tag + "Kall"])
        P.dma(Vall[:], VD[h], reads=allv, writes=[tag + "Vall"])
        for qg in range(nt):
            gb = ng % 2
            ng += 1
            P.dma(Qg[gb][:], QT[h, :, qg * T:(qg + 1) * T], reads=[("QT", qg)], writes=[(tag + "Qg", gb)])
            nkb = 4 * qg + 4
            SKEW = 2
            tiles = []
            for step in range(nkb + SKEW):
                if step < nkb:
                    kb = step
                    j = kb - 4 * qg
                    c0 = max(0, j) * 128
                    sp, pt = S_ps[n % 3], Pt[n % 3]
                    kS, kP = (tag + "S_ps", n % 3), (tag + "Pt", n % 3)
                    n += 1
                    tiles.append((kb, c0, pt, kP))
                    P.pe(lambda e, sp=sp, kb=kb, gb=gb, c0=c0: e.matmul(
                        sp[:, c0:T], lhsT=Kall[:, kb * 128:(kb + 1) * 128], rhs=Qg[gb][:, c0:T], start=True, stop=True),
                        reads=[tag + "Kall", (tag + "Qg", gb)], writes=[kS])
                    P.act(lambda e, sp=sp, pt=pt, c0=c0: e.activation(out=pt[:, c0:T], in_=sp[:, c0:T], func=AF.Exp,
                                                                      scale=SC), reads=[kS], writes=[kP])
                    if j >= 0:
                        P.pool(lambda e, pt=pt, c0=c0: e.memset(pt[64:128, c0:c0 + 64], 0.0), reads=[kP], writes=[kP])
                if step >= SKEW:
                    kb, c0, pt, kP = tiles[step - SKEW]
                    P.pe(lambda e, pt=pt, kb=kb, gb=gb, c0=c0, nkb=nkb: e.matmul(
                        O_ps[gb][0:65, c0:T], lhsT=Vall[:, kb, :], rhs=pt[:, c0:T], start=(kb == 0), stop=(kb == nkb - 1)),
                        reads=[tag + "Vall", kP], writes=[(tag + "O_ps", gb)])
            P.dve(lambda e, gb=gb: e.tensor_copy(out=Ost[gb][:], in_=O_ps[gb][0:65, :]),
                  reads=[(tag + "O_ps", gb)], writes=[(tag + "Ost", gb)])
            P.dma(OD[h, :, qg * T:(qg + 1) * T], Ost[gb][:], reads=[(tag + "Ost", gb)], writes=[("OD", qg, h)])

    Oa = [sb(nc, es, tag + f"Oa{i}", [128, 2, T], F32) for i in range(2)]
    La = [sb(nc, es, tag + f"La{i}", [128, 2, T], F32) for i in range(2)]
    Ga = [sb(nc, es, tag + f"Ga{i}", [128, 2, T], F32) for i in range(2)]
    Yd = [sb(nc, es, tag + f"Yd{i}", [128, 2, T], BF16) for i in range(2)]
    yv = ycatT.rearrange("(c p) t -> p c t", p=128)
    for it in range(nt):
        ts = slice(it * T, (it + 1) * T)
        b = it % 2
        odk = [("OD", it, h) for h in range(4)]
        for h in range(4):
            P.dma(Oa[b][(h % 2) * 64:(h % 2) * 64 + 64, h // 2, :], OD[h, 0:64, ts], reads=odk, writes=[(tag + "Oa", b)])
            P.dma(La[b][(h % 2) * 64:(h % 2) * 64 + 64, h // 2, :], OD[h, 64:65, ts].partition_broadcast(64),
                  reads=odk, writes=[(tag + "La", b)])
        P.dma(Ga[b][:], pv[:, 25:27, ts], reads=[("projT", it)], writes=[(tag + "Ga", b)])
        P.act(lambda e, b=b: e.activation(out=Ga[b][:], in_=Ga[b][:], func=AF.Silu), reads=[(tag + "Ga", b)],
              writes=[(tag + "Ga", b)])
        P.dve(lambda e, b=b: e.reciprocal(out=La[b][:], in_=La[b][:]), reads=[(tag + "La", b)], writes=[(tag + "La", b)])
        P.dve(lambda e, b=b: e.tensor_tensor(out=Oa[b][:], in0=Oa[b][:], in1=La[b][:], op=ALU.mult),
              reads=[(tag + "Oa", b), (tag + "La", b)], writes=[(tag + "Oa", b)])
        P.dve(lambda e, b=b: e.tensor_tensor(out=Yd[b][:], in0=Oa[b][:], in1=Ga[b][:], op=ALU.mult),
              reads=[(tag + "Oa", b), (tag + "Ga", b)], writes=[(tag + "Yd", b)])
        P.dma(yv[:, 6:8, ts], Yd[b][:], reads=[(tag + "Yd", b)], writes=[("ycatD", it)])


def emit_lb(nc, P, es, V, LB, l):
    ex = sb(nc, es, "lb_ex", [128, 2, 4], F32)
    sm = sb(nc, es, "lb_sm", [128, 2], F32)
    o, w = VEC_OFF["b_lb"]
    lg = V[:, o:o + 8].rearrange("p (c l) -> p c l", c=2)
    P.act(lambda e: e.activation(out=ex[:], in_=lg, func=AF.Exp), reads=["V"], writes=["lb_ex"])
    P.dve(lambda e: e.tensor_reduce(out=sm[:], in_=ex[:], axis=AX.X, op=ALU.add), reads=["lb_ex"], writes=["lb_sm"])
    P.dve(lambda e: e.reciprocal(out=sm[:], in_=sm[:]), reads=["lb_sm"], writes=["lb_sm"])
    P.pool(lambda e: e.memset(LB[:, 0, :], 0.0), writes=["LB"])
    for j in range(1, l + 1):
        P.dve(lambda e, j=j: e.tensor_tensor(out=LB[:, 0, :], in0=LB[:, 0, :], in1=ex[:, :, j], op=ALU.add),
              reads=["LB", "lb_ex"], writes=["LB"])
    P.dve(lambda e: e.tensor_tensor(out=LB[:, 0, :], in0=LB[:, 0, :], in1=sm[:], op=ALU.mult),
          reads=["LB", "lb_sm"], writes=["LB"])
    P.dve(lambda e: e.tensor_scalar(out=LB[:, 1, :], in0=LB[:, 0, :], scalar1=-1.0, scalar2=1.0, op0=ALU.mult,
                                    op1=ALU.add), reads=["LB"], writes=["LB"])
    P.dve(lambda e: e.tensor_scalar(out=LB[:, 2, :], in0=LB[:, 1, :], scalar1=-1.0, scalar2=None, op0=ALU.mult),
          reads=["LB"], writes=["LB"])


def emit_B(nc, P, es, projT, V, LB, KC, ycatT, ntok, tag="B"):
    nt = ntok // T
    ident, bd, rmask, rm, tri2 = KC["ident"], KC["bd"], KC["rmask"], KC["rm"], KC["tri2"]
    q = [sb(nc, es, tag + f"q{i}", [128, 2, T], F32) for i in range(2)]
    f = [sb(nc, es, tag + f"f{i}", [128, 2, T], F32) for i in range(2)]
    vi = [sb(nc, es, tag + f"vi{i}", [128, 2, T], F32) for i in range(2)]
    g = [sb(nc, es, tag + f"g{i}", [128, 2, T], F32) for i in range(2)]
    kk = sb(nc, es, tag + "kk", [128, 2, T], F32)
    bb = sb(nc, es, tag + "bb", [128, 2, T], F32)
    t1 = sb(nc, es, tag + "t1", [128, 2, T], F32)
    t4 = sb(nc, es, tag + "t4", [128, 2, T], F32)
    e3 = sb(nc, es, tag + "e3", [128, 2, T], F32)
    ee = sb(nc, es, tag + "ee", [128, 2, T], F32)
    qt = sb(nc, es, tag + "qt", [128, 2, T], BF16)
    ktl = sb(nc, es, tag + "ktl", [128, 2, T], BF16)
    qe = sb(nc, es, tag + "qe", [128, 2, T], BF16)
    kh = sb(nc, es, tag + "kh", [128, 2, T], BF16)
    vb = sb(nc, es, tag + "vb", [128, 2, T], BF16)
    khT = sb(nc, es, tag + "khT", [128, 4, 256], BF16)
    vT = sb(nc, es, tag + "vT", [128, 4, 256], BF16)
    Asb = [sb(nc, es, tag + f"Asb{i}", [128, 4, 128], BF16) for i in range(4)]
    qtm = [sb(nc, es, tag + f"qtm{i}", [128, 2, T], BF16) for i in range(2)]
    qem = [sb(nc, es, tag + f"qem{i}", [128, 2, T], BF16) for i in range(2)]
    vTm = [sb(nc, es, tag + f"vTm{i}", [128, 4, 256], BF16) for i in range(2)]
    S = sb(nc, es, tag + "S", [128, 128], F32)
    Sbf = sb(nc, es, tag + "Sbf", [128, 128], BF16)
    osq = sb(nc, es, tag + "osq", [128, 2, T], F32)
    rs = sb(nc, es, tag + "rs", [128, 2, T], F32)
    yb = [sb(nc, es, tag + f"yb{i}", [128, 2, T], BF16) for i in range(2)]
    pkT = [psb(P, tag + f"pkT{i}") for i in range(2)]
    pvT = [psb(P, tag + f"pvT{i}") for i in range(2)]
    pS = [psb(P, tag + f"pS{i}") for i in range(4)]
    pO = [psb(P, tag + f"pO{i}") for i in range(2)]
    pU = [psb(P, tag + f"pU{i}") for i in range(2)]

    P.pool(lambda e: e.memset(S[:], 0.0), writes=[tag + "S"])
    P.pool(lambda e: e.memset(Sbf[:], 0.0), writes=[tag + "Sbf"])
    pv = projT.rearrange("(c p) t -> p c t", p=128)
    yv = ycatT.rearrange("(c p) t -> p c t", p=128)
    nU = 0
    for it in range(nt):
        ts = slice(it * T, (it + 1) * T)
        b = it % 2
        P.dma(q[b][:], pv[:, 6:8, ts], reads=[("projT", it)], writes=[(tag + "q", b)])
        P.dma(f[b][:], pv[:, 8:10, ts], reads=[("projT", it)], writes=[(tag + "f", b)])
        P.dma(vi[b][:], pv[:, 10:12, ts], reads=[("projT", it)], writes=[(tag + "vi", b)])
        P.dma(g[b][:], pv[:, 12:14, ts], reads=[("projT", it)], writes=[(tag + "g", b)])
        fb, qb_, vib, gb_ = f[b], q[b], vi[b], g[b]
        kf, kq, kv, kg = (tag + "f", b), (tag + "q", b), (tag + "vi", b), (tag + "g", b)
        P.act(lambda e, fb=fb: e.activation(out=fb[:], in_=fb[:], func=AF.Sigmoid), reads=[kf], writes=[kf])
        for c in range(2):
            P.dve(lambda e, c=c, fb=fb: e.tensor_scalar(out=kk[:, c, :], in0=fb[:, c, :], scalar1=LB[:, 2, c:c + 1],
                                                        scalar2=LB[:, 1, c:c + 1], op0=ALU.mult, op1=ALU.add),
                  reads=[kf, "LB"], writes=[tag + "kk"])
            P.dve(lambda e, c=c, fb=fb: e.tensor_scalar(out=fb[:, c, :], in0=fb[:, c, :], scalar1=LB[:, 1, c:c + 1],
                                                        scalar2=LB[:, 0, c:c + 1], op0=ALU.mult, op1=ALU.add),
                  reads=[kf, "LB", tag + "kk"], writes=[kf])
        P.act(lambda e, fb=fb: e.activation(out=fb[:], in_=fb[:], func=AF.Ln), reads=[kf], writes=[kf])
        for c in range(2):
            P.dve(lambda e, c=c, fb=fb: e.tensor_tensor_scan(out=bb[:, c, :], data0=rmask[:], data1=fb[:, c, :],
                                                             initial=0.0, op0=ALU.mult, op1=ALU.add),
                  reads=[kf, "KC"], writes=[tag + "bb"])
        b4 = bb[:].rearrange("p c (n s) -> p c n s", s=64)
        mid = b4[:, :, :, 31:32].to_broadcast([128, 2, 8, 64])
        last = b4[:, :, :, 63:64].to_broadcast([128, 2, 8, 64])
        t14 = t1[:].rearrange("p c (n s) -> p c n s", s=64)
        t44 = t4[:].rearrange("p c (n s) -> p c n s", s=64)
        P.dve(lambda e: e.tensor_tensor(out=t14, in0=b4, in1=mid, op=ALU.subtract), reads=[tag + "bb"], writes=[tag + "t1"])
        P.dve(lambda e: e.tensor_tensor(out=t44, in0=last, in1=b4, op=ALU.subtract), reads=[tag + "bb"], writes=[tag + "t4"])
        P.act(lambda e: e.activation(out=e3[:], in_=bb[:], func=AF.Exp), reads=[tag + "bb"], writes=[tag + "e3"])
        P.dve(lambda e, qb_=qb_: e.tensor_tensor(out=qe[:], in0=qb_[:], in1=e3[:], op=ALU.mult),
              reads=[kq, tag + "e3"], writes=[tag + "qe"])
        P.act(lambda e: e.activation(out=ee[:], in_=t1[:], func=AF.Exp), reads=[tag + "t1"], writes=[tag + "ee"])
        P.dve(lambda e, qb_=qb_: e.tensor_tensor(out=qt[:], in0=qb_[:], in1=ee[:], op=ALU.mult),
              reads=[kq, tag + "ee"], writes=[tag + "qt"])
        P.act(lambda e: e.activation(out=ee[:], in_=t1[:], func=AF.Exp, scale=-1.0), reads=[tag + "t1", tag + "qt"],
              writes=[tag + "ee"])
        P.dve(lambda e: e.tensor_tensor(out=ktl[:], in0=kk[:], in1=ee[:], op=ALU.mult),
              reads=[tag + "kk", tag + "ee"], writes=[tag + "ktl"])
        P.act(lambda e: e.activation(out=t4[:], in_=t4[:], func=AF.Exp), reads=[tag + "t4"], writes=[tag + "t4"])
        P.dve(lambda e: e.tensor_tensor(out=kh[:], in0=kk[:], in1=t4[:], op=ALU.mult),
               reads=[tag + "kk", tag + "t4"], writes=[tag + "kh"])
        P.pool(lambda e, vib=vib: e.tensor_copy(out=vb[:], in_=vib[:]), reads=[kv], writes=[tag + "vb"])
        P.act(lambda e, gb_=gb_: e.activation(out=gb_[:], in_=gb_[:], func=AF.Silu), reads=[kg], writes=[kg])
        for (src, ksrc, pst, kps, dst, kdst) in ((kh, tag + "kh", pkT, tag + "pkT", khT, tag + "khT"),
                                                  (vb, tag + "vb", pvT, tag + "pvT", vT, tag + "vT")):
            for j in range(4):
                for c in range(2):
                    P.pe(lambda e, src=src, pst=pst, j=j, c=c: e.matmul(
                        pst[j // 2][:, (j % 2) * 256 + c * 128:(j % 2) * 256 + c * 128 + 128],
                        lhsT=src[:, c, j * 128:(j + 1) * 128], rhs=ident[:], start=True, stop=True),
                        reads=[ksrc, "KC"], writes=[(kps, j // 2)])
            for hf in range(2):
                eng = P.act if hf == 0 else P.dve
                if hf == 0:
                    P.act(lambda e, pst=pst, dst=dst: e.copy(out=dst[:, 0:2, :].rearrange("p j n -> p (j n)"), in_=pst[0][:]),
                          reads=[(kps, 0)], writes=[kdst])
                else:
                    P.dve(lambda e, pst=pst, dst=dst: e.tensor_copy(out=dst[:, 2:4, :].rearrange("p j n -> p (j n)"), in_=pst[1][:]),
                          reads=[(kps, 1)], writes=[kdst])
        for e_ in range(2):
            P.dve(lambda e, e_=e_: e.tensor_scalar(out=qtm[e_][:], in0=qt[:], scalar1=rm[:, e_:e_ + 1], scalar2=None,
                                                   op0=ALU.mult), reads=[tag + "qt", "KC"], writes=[(tag + "qtm", e_)])
            P.dve(lambda e, e_=e_: e.tensor_scalar(out=qem[e_][:], in0=qe[:], scalar1=rm[:, e_:e_ + 1], scalar2=None,
                                                   op0=ALU.mult), reads=[tag + "qe", "KC"], writes=[(tag + "qem", e_)])
            P.dve(lambda e, e_=e_: e.tensor_scalar(out=vTm[e_][:], in0=vT[:], scalar1=rm[:, e_:e_ + 1], scalar2=None,
                                                   op0=ALU.mult), reads=[tag + "vT", "KC"], writes=[(tag + "vTm", e_)])
        for j in range(4):
            blk = slice(j * 128, (j + 1) * 128)
            for h in range(4):
                c2, e_ = h // 2, h % 2
                P.pe(lambda e, j=j, h=h, c2=c2, e_=e_, blk=blk: e.matmul(
                    pS[j][:, h * 128:(h + 1) * 128], lhsT=ktl[:, c2, blk], rhs=qtm[e_][:, c2, blk], start=True, stop=True),
                    reads=[tag + "ktl", (tag + "qtm", e_)], writes=[(tag + "pS", j)])
            P.dve(lambda e, j=j: e.tensor_tensor(out=Asb[j][:], in0=pS[j][:].rearrange("p (h t) -> p h t", h=4),
                                                 in1=tri2.rearrange("p (o t) -> p o t", o=1).to_broadcast([128, 4, 128]),
                                                 op=ALU.mult),
                  reads=[(tag + "pS", j), "KC"], writes=[(tag + "Asb", j)])
        for j in range(4):
            blk = slice(j * 128, (j + 1) * 128)
            for h in range(4):
                c2, hp = h // 2, (h % 2) * 64
                P.pe(lambda e, j=j, h=h, c2=c2, hp=hp, blk=blk: e.matmul(
                    pO[c2][hp:hp + 64, blk], lhsT=vT[:, j, h * 64:(h + 1) * 64], rhs=Asb[j][:, h, :],
                    start=True, stop=False), reads=[tag + "vT", (tag + "Asb", j)], writes=[(tag + "pO", c2)])
            for par in range(2):
                c = 2 * j + par
                cols = slice(c * 64, c * 64 + 64)
                ub = nU % 2
                nU += 1
                for h in range(4):
                    c2, hp, e_ = h // 2, (h % 2) * 64, h % 2
                    P.pe(lambda e, c2=c2, hp=hp, e_=e_, cols=cols, par=par: e.matmul(
                        pO[c2][hp:hp + 64, cols], lhsT=Sbf[:, c2 * 64:(c2 + 1) * 64], rhs=qem[e_][:, c2, cols],
                        start=False, stop=(par == 1)), reads=[tag + "Sbf", (tag + "qem", e_)], writes=[(tag + "pO", c2)])
                for h in range(4):
                    c2, hp = h // 2, (h % 2) * 64
                    P.pe(lambda e, c2=c2, hp=hp, j=j, h=h, ub=ub, par=par: e.matmul(
                        pU[ub][hp:hp + 64, c2 * 64:(c2 + 1) * 64], lhsT=khT[:, j, h * 64:(h + 1) * 64],
                        rhs=vTm[par][:, j, h * 64:(h + 1) * 64], start=True, stop=True),
                        reads=[tag + "khT", (tag + "vTm", par)], writes=[(tag + "pU", ub)])
                for c2 in range(2):
                    P.dve(lambda e, c2=c2, ub=ub, c=c: e.scalar_tensor_tensor(
                        out=S[:, c2 * 64:(c2 + 1) * 64], in0=S[:, c2 * 64:(c2 + 1) * 64],
                        scalar=e3[:, c2, c * 64 + 63:c * 64 + 64], in1=pU[ub][:, c2 * 64:(c2 + 1) * 64],
                        op0=ALU.mult, op1=ALU.add), reads=[tag + "S", tag + "e3", (tag + "pU", ub)], writes=[tag + "S"])
                P.act(lambda e: e.copy(out=Sbf[:], in_=S[:]), reads=[tag + "S"], writes=[tag + "Sbf"])
        for c2 in range(2):
            P.act(lambda e, c2=c2: e.activation(out=osq[:, c2, :], in_=pO[c2][:], func=AF.Square),
                  reads=[(tag + "pO", c2)], writes=[(tag + "osq", c2)])
            P.pe(lambda e, c2=c2: e.matmul(pS[c2][:], lhsT=bd[:], rhs=osq[:, c2, :], start=True, stop=True),
                 reads=["KC", (tag + "osq", c2)], writes=[(tag + "pS", c2)])
            P.dve(lambda e, c2=c2: e.tensor_scalar(out=rs[:, c2, :], in0=pS[c2][:], scalar1=1.0 / 64, scalar2=1e-6,
                                                   op0=ALU.mult, op1=ALU.add), reads=[(tag + "pS", c2)], writes=[(tag + "rs", c2)])
            P.act(lambda e, c2=c2: e.activation(out=rs[:, c2, :], in_=rs[:, c2, :], func=AF.Sqrt),
                  reads=[(tag + "rs", c2)], writes=[(tag + "rs", c2)])
            P.dve(lambda e, c2=c2: e.reciprocal(out=rs[:, c2, :], in_=rs[:, c2, :]), reads=[(tag + "rs", c2)],
                  writes=[(tag + "rs", c2)])
            P.dve(lambda e, c2=c2: e.scalar_tensor_tensor(out=rs[:, c2, :], in0=rs[:, c2, :], scalar=vcol(V, "b_norm_g", c2),
                                                          in1=pO[c2][:], op0=ALU.mult, op1=ALU.mult),
                  reads=[(tag + "rs", c2), (tag + "pO", c2), "V"], writes=[(tag + "rs", c2)])
            P.dve(lambda e, c2=c2, b=b, gb_=gb_: e.tensor_tensor(out=yb[b][:, c2, :], in0=rs[:, c2, :], in1=gb_[:, c2, :],
                                                                 op=ALU.mult),
                  reads=[(tag + "rs", c2), kg], writes=[(tag + "yb", b)])
        P.dma(yv[:, 2:4, ts], yb[b][:], reads=[(tag + "yb", b)], writes=[("ycatB", it)])
        yield


def make_kc_host():
    ident = np.eye(128, dtype=np.float32)
    bd = np.kron(np.eye(2, dtype=np.float32), np.ones((64, 64), np.float32))
    s = np.arange(128) % 64
    tri = (s[:, None] <= np.arange(64)[None, :]).astype(np.float32)
    rmask = np.ones((128, T), np.float32)
    rmask[:, ::64] = 0.0
    rm = np.zeros((128, 2), np.float32)
    rm[0:64, 0] = 1.0
    rm[64:128, 1] = 1.0
    pp = np.arange(128)
    tri2 = ((pp[:, None] // 64 == pp[None, :] // 64) & (pp[:, None] % 64 <= pp[None, :] % 64)).astype(np.float32)
    return np.ascontiguousarray(np.concatenate([ident, bd, tri, rmask, rm, tri2], axis=1))


def load_kc(nc, P, es, d_kc):
    raw = sb(nc, es, "kc_raw", [128, 962], F32)
    ident = sb(nc, es, "kc_ident", [128, 128], BF16)
    P.dma(raw[:], d_kc[:, :], writes=["KC"])
    P.dve(lambda e: e.tensor_copy(out=ident[:], in_=raw[:, 0:128]), reads=["KC"], writes=["KC"])
    return {"ident": ident, "bd": raw[:, 128:256], "tri": raw[:, 256:320], "rmask": raw[:, 320:832], "rm": raw[:, 832:834],
            "tri2": raw[:, 834:962], "raw": raw}


def make_kc2_host():
    j = np.arange(128)
    su = (j[:, None] > j[None, :]).astype(np.float32)
    lt = (j[:, None] <= j[None, :]).astype(np.float32)
    negi = (-30000.0 * np.eye(128)).astype(np.float32)
    ones = np.ones((128, 128), np.float32)
    return np.ascontiguousarray(np.concatenate([su, lt, su, negi, ones, np.eye(128, dtype=np.float32)], axis=1))


class _Stop(Exception):
    pass


def _ck(k):
    import os
    if os.environ.get("CSTOP") == str(k):
        raise _Stop()


def emit_C(nc, P, es, projT, V, KC, KC2, ycatT, ntok, tag="C"):
    nt = ntok // T
    ident = KC["ident"]
    SU, LT, GT, NEGI, ONES, IDF = (KC2[:, i * 128:(i + 1) * 128] for i in range(6))
    xb = sb(nc, es, tag + "xb", [128, 6, 4 + T], F32)
    xc = sb(nc, es, tag + "xc", [128, 6, T], F32)
    bcb = sb(nc, es, tag + "bcb", [128, 6, T], BF16)
    z = [sb(nc, es, tag + f"z{i}", [128, 2, T], F32) for i in range(2)]
    dta = sb(nc, es, tag + "dta", [128, T], F32)
    negA = sb(nc, es, tag + "negA", [128, 1], F32)
    atm = sb(nc, es, tag + "atm", [128, 16], F32)
    d2a = sb(nc, es, tag + "d2a", [128, 4, 4], F32)
    datm = sb(nc, es, tag + "datm", [128, 4, 8], F32)
    Lh = sb(nc, es, tag + "Lh", [128, 4, 128], F32)
    AB = sb(nc, es, tag + "AB", [128, 4, 128], F32)
    Lm = sb(nc, es, tag + "Lm", [128, 4, 128], F32)
    Sc = sb(nc, es, tag + "Sc", [128, 4, 128], BF16)
    DEC = sb(nc, es, tag + "DEC", [128, 2, T], F32)
    dl = sb(nc, es, tag + "dl", [128, 4], F32)
    d2 = sb(nc, es, tag + "d2", [128, 4], F32)
    xdtf = sb(nc, es, tag + "xdtf", [128, 256], F32)
    xdtb = sb(nc, es, tag + "xdtb", [128, 256], BF16)
    wxb = sb(nc, es, tag + "wxb", [128, 256], BF16)
    Btm = sb(nc, es, tag + "Btm", [128, 256], BF16)
    st = sb(nc, es, tag + "st", [128, 4, 64], F32)
    stb = sb(nc, es, tag + "stb", [128, 4, 64], BF16)
    yt = sb(nc, es, tag + "yt", [128, 2, T], F32)
    ysq = sb(nc, es, tag + "ysq", [128, 2, T], F32)
    rs = sb(nc, es, tag + "rs", [128, 2, T], F32)
    yc = [sb(nc, es, tag + f"yc{i}", [128, 2, T], BF16) for i in range(2)]
    p_da = psb(P, tag + "p_da")
    p_seg = psb(P, tag + "p_seg")
    p_acs = psb(P, tag + "p_acs")
    p_m1 = psb(P, tag + "p_m1")
    p_m2 = psb(P, tag + "p_m2")
    p_y = [psb(P, tag + f"p_y{i}") for i in range(2)]
    p_ss = psb(P, tag + "p_ss")

    P.pool(lambda e: e.memset(xb[:, :, 0:4], 0.0), writes=[tag + "xb"])
    P.pool(lambda e: e.memset(st[:], 0.0), writes=[tag + "st"])
    P.pool(lambda e: e.memset(stb[:], 0.0), writes=[tag + "stb"])
    P.pool(lambda e: e.memset(dta[:], 0.0), writes=[tag + "dta"])
    P.act(lambda e: e.activation(out=negA[:], in_=vcol(V, "c_a_log", rows=128), func=AF.Exp), reads=["V"], writes=[tag + "negA"])
    P.dve(lambda e: e.tensor_scalar(out=negA[:], in0=negA[:], scalar1=-1.0, scalar2=None, op0=ALU.mult),
          reads=[tag + "negA"], writes=[tag + "negA"])
    pv = projT.rearrange("(c p) t -> p c t", p=128)
    yv = ycatT.rearrange("(c p) t -> p c t", p=128)
    ny = 0
    for it in range(nt):
        ts = slice(it * T, (it + 1) * T)
        b = it % 2
        P.dma(xb[:, :, 4:4 + T], pv[:, 14:20, ts], reads=[("projT", it)], writes=[tag + "xb"])
        P.dma(z[b][:], pv[:, 20:22, ts], reads=[("projT", it)], writes=[(tag + "z", b)])
        P.dma(dta[0:4, :], projT[R_DT:R_DT + 4, ts], reads=[("projT", it)], writes=[tag + "dta"])
        P.dma(dta[64:68, :], projT[R_DT:R_DT + 4, ts], reads=[("projT", it)], writes=[tag + "dta"])
        for c in range(6):
            eng = P.dve
            for j in range(4):
                src = xb[:, c, 1 + j:1 + j + T]
                wj = vcol(V, "c_conv_w", c * 4 + j)
                if j == 0:
                    eng(lambda e, c=c, src=src, wj=wj: e.tensor_scalar(out=xc[:, c, :], in0=src, scalar1=wj,
                                                                      scalar2=vcol(V, "c_conv_b", c), op0=ALU.mult, op1=ALU.add),
                        reads=[tag + "xb", "V"], writes=[(tag + "xc", c)])
                else:
                    eng(lambda e, c=c, src=src, wj=wj: e.scalar_tensor_tensor(out=xc[:, c, :], in0=src, scalar=wj,
                                                                             in1=xc[:, c, :], op0=ALU.mult, op1=ALU.add),
                        reads=[tag + "xb", "V", (tag + "xc", c)], writes=[(tag + "xc", c)])
        allxc = [(tag + "xc", c) for c in range(6)]
        P.act(lambda e: e.copy(out=xb[:, :, 0:4], in_=xb[:, :, T:T + 4]), reads=[tag + "xb"] + allxc, writes=[tag + "xb"])
        P.act(lambda e: e.activation(out=xc[:], in_=xc[:], func=AF.Silu), reads=allxc, writes=allxc)
        P.dve(lambda e: e.tensor_copy(out=bcb[:], in_=xc[:]), reads=allxc, writes=[tag + "bcb"])
        P.act(lambda e, b=b: e.activation(out=z[b][:], in_=z[b][:], func=AF.Silu), reads=[(tag + "z", b)], writes=[(tag + "z", b)])
        _ck(1)
        P.act(lambda e: e.activation(out=dta[:], in_=dta[:], func=AF.Exp, bias=vcol(V, "c_dt_bias", rows=128)),
              reads=[tag + "dta", "V"], writes=[tag + "dta"])
        P.dve(lambda e: e.tensor_scalar(out=dta[:], in0=dta[:], scalar1=1.0, scalar2=None, op0=ALU.add),
              reads=[tag + "dta"], writes=[tag + "dta"])
        P.act(lambda e: e.activation(out=dta[:], in_=dta[:], func=AF.Ln), reads=[tag + "dta"], writes=[tag + "dta"])
        P.dve(lambda e: e.tensor_scalar(out=dta[64:128, :], in0=dta[64:128, :], scalar1=negA[64:128, 0:1], scalar2=None,
                                        op0=ALU.mult), reads=[tag + "dta", tag + "negA"], writes=[tag + "dta"])
        for j in range(4):
            P.pe(lambda e, j=j: e.matmul(p_da[:, j * 128:(j + 1) * 128], lhsT=dta[:, j * 128:(j + 1) * 128], rhs=IDF,
                                         start=True, stop=True), reads=[tag + "dta", "KC2"], writes=[tag + "p_da"])
        for j in range(4):
            P.dve(lambda e, j=j: e.tensor_copy(out=datm[:, j, 0:4], in_=p_da[:, j * 128:j * 128 + 4]),
                  reads=[tag + "p_da"], writes=[tag + "datm"])
            P.dve(lambda e, j=j: e.tensor_copy(out=datm[:, j, 4:8], in_=p_da[:, j * 128 + 64:j * 128 + 68]),
                  reads=[tag + "p_da"], writes=[tag + "datm"])
            P.dve(lambda e, j=j: e.tensor_copy(out=atm[:, j * 4:j * 4 + 4], in_=p_da[:, j * 128 + 64:j * 128 + 68]),
                  reads=[tag + "p_da"], writes=[tag + "atm"])
        P.pe(lambda e: e.matmul(p_da[:, 0:16], lhsT=GT, rhs=atm[:], start=True, stop=True),
             reads=["KC2", tag + "atm"], writes=[tag + "p_da"])
        P.act(lambda e: e.activation(out=d2a[:].rearrange("p j h -> p (j h)"), in_=p_da[:, 0:16], func=AF.Exp),
              reads=[tag + "p_da"], writes=[tag + "d2a"])
        _ck(2)
        for j in range(4):
            blk = slice(j * 128, (j + 1) * 128)
            yb_ = ny % 2
            ny += 1
            py = p_y[yb_]
            kpy = (tag + "p_y", yb_)
            for h in range(4):
                asc = datm[:, j, 4 + h:5 + h]
                P.dve(lambda e, h=h, asc=asc: e.tensor_scalar(out=Lh[:, h, :], in0=SU, scalar1=asc, scalar2=None, op0=ALU.mult),
                      reads=[tag + "datm", "KC2"], writes=[(tag + "Lh", h)])
                P.dve(lambda e, h=h, asc=asc: e.tensor_scalar(out=AB[:, h, :], in0=ONES, scalar1=asc, scalar2=None, op0=ALU.mult),
                       reads=[tag + "datm", "KC2"], writes=[(tag + "AB", h)])
            for h in range(4):
                P.pe(lambda e, h=h: e.matmul(p_seg[:, h * 128:(h + 1) * 128], lhsT=Lh[:, h, :], rhs=LT, start=True, stop=False),
                     reads=[(tag + "Lh", h), "KC2"], writes=[tag + "p_seg"])
                P.pe(lambda e, h=h: e.matmul(p_seg[:, h * 128:(h + 1) * 128], lhsT=NEGI, rhs=GT, start=False, stop=True),
                     reads=["KC2"], writes=[tag + "p_seg"])
            for h in range(4):
                P.pe(lambda e, h=h: e.matmul(p_acs[:, h * 128:(h + 1) * 128], lhsT=AB[:, h, :], rhs=LT, start=True, stop=True),
                     reads=[(tag + "AB", h), "KC2"], writes=[tag + "p_acs"])
            P.act(lambda e: e.activation(out=Lm[:].rearrange("p h l -> p (h l)"), in_=p_seg[:], func=AF.Exp),
                  reads=[tag + "p_seg"], writes=[tag + "Lm"])
            for h in range(4):
                P.act(lambda e, h=h: e.activation(out=dl[:, h:h + 1], in_=p_acs[:, h * 128 + 127:h * 128 + 128], func=AF.Exp),
                      reads=[tag + "p_acs"], writes=[tag + "dl"])
            for h in range(4):
                g_, hp = h // 2, (h % 2) * 64
                P.act(lambda e, h=h, g_=g_, hp=hp, blk=blk: e.activation(out=DEC[hp:hp + 64, g_, blk],
                                                                        in_=p_acs[hp:hp + 64, h * 128:(h + 1) * 128], func=AF.Exp),
                      reads=[tag + "p_acs"], writes=[tag + "DEC"])
            _ck(3)
            for g_ in range(2):
                P.pe(lambda e, g_=g_, blk=blk: e.matmul(p_m1[:, g_ * 128:(g_ + 1) * 128], lhsT=bcb[:, g_, blk], rhs=ident[:],
                                                         start=True, stop=True), reads=[tag + "bcb", "KC"], writes=[tag + "p_m1"])
                P.pe(lambda e, g_=g_, blk=blk: e.matmul(p_m1[:, 256 + g_ * 128:256 + (g_ + 1) * 128], lhsT=bcb[:, 2 + g_, blk],
                                                         rhs=ident[:], start=True, stop=True), reads=[tag + "bcb", "KC"], writes=[tag + "p_m1"])
            for h in range(4):
                P.dve(lambda e, h=h, j=j: e.tensor_scalar(out=xdtf[:, h * 64:(h + 1) * 64], in0=p_m1[:, h * 64:(h + 1) * 64],
                                                          scalar1=datm[:, j, h:h + 1], scalar2=None, op0=ALU.mult),
                      reads=[tag + "p_m1", tag + "datm"], writes=[tag + "xdtf"])
            P.act(lambda e: e.copy(out=Btm[:], in_=p_m1[:, 256:512]), reads=[tag + "p_m1"], writes=[tag + "Btm"])
            P.act(lambda e: e.copy(out=xdtb[:], in_=xdtf[:]), reads=[tag + "xdtf"], writes=[tag + "xdtb"])
            for h in range(4):
                P.dve(lambda e, h=h, j=j: e.tensor_scalar(out=wxb[:, h * 64:(h + 1) * 64], in0=xdtf[:, h * 64:(h + 1) * 64],
                                                          scalar1=d2a[:, j, h:h + 1], scalar2=None, op0=ALU.mult),
                      reads=[tag + "xdtf", tag + "d2a"], writes=[tag + "wxb"])
            _ck(4)
            for g_ in range(2):
                P.pe(lambda e, g_=g_, blk=blk: e.matmul(p_m2[:, g_ * 128:(g_ + 1) * 128], lhsT=bcb[:, 2 + g_, blk],
                                                         rhs=bcb[:, 4 + g_, blk], start=True, stop=True),
                     reads=[tag + "bcb"], writes=[tag + "p_m2"])
            _ck(40)
            for h in range(4):
                g_ = h // 2
                P.dve(lambda e, h=h, g_=g_: e.tensor_tensor(out=Sc[:, h, :], in0=p_m2[:, g_ * 128:(g_ + 1) * 128], in1=Lm[:, h, :],
                                                            op=ALU.mult), reads=[tag + "p_m2", tag + "Lm"], writes=[(tag + "Sc", h)])
            _ck(41)
            for h in range(4):
                g_, hp = h // 2, (h % 2) * 64
                P.pe(lambda e, h=h, g_=g_, hp=hp, py=py: e.matmul(py[hp:hp + 64, g_ * 128:(g_ + 1) * 128],
                                                                  lhsT=xdtb[:, h * 64:(h + 1) * 64], rhs=Sc[:, h, :],
                                                                  start=True, stop=True),
                     reads=[tag + "xdtb", (tag + "Sc", h)], writes=[kpy])
                P.pe(lambda e, h=h, g_=g_, hp=hp, py=py, blk=blk: e.matmul(py[hp:hp + 64, 256 + g_ * 128:256 + (g_ + 1) * 128],
                                                                           lhsT=stb[:, h, :], rhs=bcb[:, 4 + g_, blk],
                                                                           start=True, stop=True),
                     reads=[tag + "stb", tag + "bcb"], writes=[kpy])
            _ck(42)
            for h in range(4):
                g_ = h // 2
                P.pe(lambda e, h=h, g_=g_: e.matmul(p_m2[:, 256 + h * 64:256 + (h + 1) * 64], lhsT=Btm[:, g_ * 128:(g_ + 1) * 128],
                                                    rhs=wxb[:, h * 64:(h + 1) * 64], start=True, stop=True),
                     reads=[tag + "Btm", tag + "wxb"], writes=[tag + "p_m2"])
            _ck(43)
            for h in range(4):
                P.dve(lambda e, h=h: e.scalar_tensor_tensor(out=st[:, h, :], in0=st[:, h, :], scalar=dl[:, h:h + 1],
                                                            in1=p_m2[:, 256 + h * 64:256 + (h + 1) * 64], op0=ALU.mult, op1=ALU.add),
                      reads=[tag + "st", tag + "dl", tag + "p_m2"], writes=[tag + "st"])
            P.act(lambda e: e.copy(out=stb[:], in_=st[:]), reads=[tag + "st"], writes=[tag + "stb"])
            _ck(5)
            yt3 = yt[:, :, blk]
            P.dve(lambda e, py=py, blk=blk, yt3=yt3: e.tensor_tensor(out=yt3, in0=py[:, 256:512].rearrange("p (g l) -> p g l", g=2),
                                                                     in1=DEC[:, :, blk], op=ALU.mult),
                  reads=[kpy, tag + "DEC"], writes=[tag + "yt"])
            P.dve(lambda e, py=py, yt3=yt3: e.tensor_tensor(out=yt3, in0=py[:, 0:256].rearrange("p (g l) -> p g l", g=2),
                                                            in1=yt3, op=ALU.add), reads=[kpy, tag + "yt"], writes=[tag + "yt"])
        _ck(6)
        for g_ in range(2):
            P.dve(lambda e, g_=g_: e.scalar_tensor_tensor(out=yt[:, g_, :], in0=xc[:, g_, :], scalar=vcol(V, "c_d", g_),
                                                          in1=yt[:, g_, :], op0=ALU.mult, op1=ALU.add),
                  reads=[(tag + "xc", g_), "V", tag + "yt"], writes=[tag + "yt"])
        P.dve(lambda e, b=b: e.tensor_tensor(out=yt[:], in0=yt[:], in1=z[b][:], op=ALU.mult),
              reads=[tag + "yt", (tag + "z", b)], writes=[tag + "yt"])
        P.act(lambda e: e.activation(out=ysq[:], in_=yt[:], func=AF.Square), reads=[tag + "yt"], writes=[tag + "ysq"])
        for g_ in range(2):
            P.pe(lambda e, g_=g_: e.matmul(p_ss[:], lhsT=ONES, rhs=ysq[:, g_, :], start=True, stop=True),
                 reads=["KC2", tag + "ysq"], writes=[tag + "p_ss"])
            P.dve(lambda e, g_=g_: e.tensor_scalar(out=rs[:, g_, :], in0=p_ss[:], scalar1=1.0 / 128, scalar2=1e-6,
                                                   op0=ALU.mult, op1=ALU.add), reads=[tag + "p_ss"], writes=[(tag + "rs", g_)])
            P.act(lambda e, g_=g_: e.activation(out=rs[:, g_, :], in_=rs[:, g_, :], func=AF.Sqrt), reads=[(tag + "rs", g_)],
                  writes=[(tag + "rs", g_)])
            P.dve(lambda e, g_=g_: e.reciprocal(out=rs[:, g_, :], in_=rs[:, g_, :]), reads=[(tag + "rs", g_)], writes=[(tag + "rs", g_)])
            P.dve(lambda e, g_=g_, b=b: e.scalar_tensor_tensor(out=yc[b][:, g_, :], in0=yt[:, g_, :], scalar=vcol(V, "c_norm_g", g_),
                                                               in1=rs[:, g_, :], op0=ALU.mult, op1=ALU.mult),
                  reads=[tag + "yt", (tag + "rs", g_), "V"], writes=[(tag + "yc", b)])
        P.dma(yv[:, 4:6, ts], yc[b][:], reads=[(tag + "yc", b)], writes=[("ycatC", it)])
        yield


def build_full(ntok=SEQ, depth=DEPTH):
    nc = bass.Bass("TRN2", target_bir_lowering=False)
    di = lambda name, shape, dt=F32: nc.dram_tensor(name, list(shape), dt, kind="ExternalInput").ap()
    dn = lambda name, shape, dt=F32: nc.dram_tensor(name, list(shape), dt, kind="Internal").ap()
    xT = di("xT", [D_MODEL, ntok])
    pos = di("pos", [1, ntok], I32)
    Vd = di("vecs", [depth, 128, NV])
    w_in = di("w_in", [depth, D_MODEL, NPROJ])
    w_out = di("w_out", [depth, D_MODEL, D_MODEL])
    pw = di("a_pw", [depth, 256, 256])
    qbw = di("qbw", [depth, 256, 768])
    kvbw = di("kvbw", [depth, 128, 512])
    cp_d = di("cp", [128, NCP])
    kc_d = di("kc", [128, 962])
    kc2_d = di("kc2", [128, 768])
    xTo = nc.dram_tensor("xTo", [D_MODEL, ntok], F32, kind="ExternalOutput").ap()
    projT = dn("projT", [NPROJ, ntok])
    ycatT = dn("ycatT", [D_MODEL, ntok], BF16)
    xbuf = [dn("xbuf0", [D_MODEL, ntok]), dn("xbuf1", [D_MODEL, ntok])]
    QT = dn("QT", [4, 96, ntok], BF16)
    KT = dn("KT", [4, 96, ntok], BF16)
    VD = dn("VD", [4, 128, ntok // 128, 65], BF16)
    OD = dn("OD", [4, 65, ntok])
    with ExitStack() as es:
        P = Prog(nc, es)
        Vs = [sb(nc, es, f"V_sb{l}", [128, NV], F32) for l in range(depth)]
        CP = sb(nc, es, "CP_sb", [128, NCP], F32)
        KC2 = sb(nc, es, "KC2_sb", [128, 768], F32)
        LB = sb(nc, es, "LB_sb", [128, 3, 2], F32)
        for l in range(depth):
            P.dma(Vs[l][:], Vd[l], writes=["V"])
        P.dma(CP[:], cp_d[:, :], writes=["CP"])
        P.dma(KC2[:], kc2_d[:, :], writes=["KC2"])
        KC = load_kc(nc, P, es, kc_d)
        P.phase_end()
        for l in range(depth):
            V = Vs[l]
            x_in = xT if l == 0 else xbuf[(l - 1) % 2]
            x_out = xTo if l == depth - 1 else xbuf[l % 2]
            with ExitStack() as pes:
                emit_lb(nc, P, pes, V, LB, l)
                emit_l1(nc, P, pes, x_in, w_in[l], V, projT, ntok=ntok)
                P.phase_end()
            with ExitStack() as pes:
                emit_A(nc, P, pes, projT, V, pw[l], ycatT, ntok)
                P.phase_end()
            with ExitStack() as pes:
                gB = emit_B(nc, P, pes, projT, V, LB, KC, ycatT, ntok)
                gC = emit_C(nc, P, pes, projT, V, KC, KC2, ycatT, ntok)
                doneB = doneC = False
                while not (doneB and doneC):
                    if not doneB:
                        doneB = next(gB, "end") == "end"
                    if not doneC:
                        doneC = next(gC, "end") == "end"
                P.phase_end()
            with ExitStack() as pes:
                emit_D(nc, P, pes, projT, pos, V, CP, qbw[l], kvbw[l], QT, KT, VD, OD, ycatT, ntok)
                P.phase_end()
            with ExitStack() as pes:
                emit_O(nc, P, pes, ycatT, x_in, V, w_out[l], x_out, ntok)
                P.phase_end()
    return nc


def make_in_map(x_b, pos_b, p, depth=DEPTH):
    return {
        "xT": np.ascontiguousarray(np.asarray(x_b, np.float32).T),
        "pos": np.ascontiguousarray(np.asarray(pos_b, np.int32).reshape(1, -1)),
        "vecs": np.stack([pack_vecs(p, l) for l in range(depth)]),
        "w_in": np.stack([pack_w_in(np.asarray(p["w_in"][l], np.float32)) for l in range(depth)]),
        "w_out": np.ascontiguousarray(np.asarray(p["w_out"], np.float32)[:depth]),
        "a_pw": np.ascontiguousarray(np.asarray(p["a_pw_w"], np.float32)[:depth]),
        "qbw": np.stack([pack_qb(p["d_qb_w"][l]).reshape(256, 768) for l in range(depth)]),
        "kvbw": np.stack([pack_kvb(p["d_kvb_w"][l]).reshape(128, 512) for l in range(depth)]),
        "cp": make_cp(), "kc": make_kc_host(), "kc2": make_kc2_host(),
    }


def kernel(**inputs):
    x = np.asarray(inputs["x"], np.float32)
    positions = np.asarray(inputs["positions"], np.int32)
    p = {k: np.asarray(v) for k, v in inputs.items() if k not in ("x", "positions")}
    nb, ntok, _ = x.shape
    nc = build_full(ntok, DEPTH)
    in_maps = [make_in_map(x[b], positions[b], p) for b in range(nb)]
    res = run_bass_kernel_spmd(nc, in_maps, core_ids=list(range(nb)))
    out = np.stack([np.ascontiguousarray(res.results[b]["xTo"].T) for b in range(nb)])
    return out.astype(np.float32)
```

```python
import numpy as np
from contextlib import ExitStack
import concourse.bass as bass
import concourse.mybir as mybir
from concourse.bass_utils import run_bass_kernel_spmd

F32 = mybir.dt.float32
BF16 = mybir.dt.bfloat16
I32 = mybir.dt.int32
AF = mybir.ActivationFunctionType
ALU = mybir.AluOpType
AX = mybir.AxisListType

D_MODEL = 1024
SEQ = 16384
DEPTH = 4
NCORE = 8
SEG = 2048
NSEG = 8
TOK = 4096
T = 512
NT = TOK // T
NPROJ = 3584
SEM_ROT = 24000


class Op:
    __slots__ = ("eng", "fn", "waits", "sem", "val", "dma", "idx")


class Prog:
    ENGS = ("pe", "act", "dve", "pool", "sp")

    def __init__(self, nc, es, ndma_sems=14):
        self.nc = nc
        self.es = es
        self.ops = {e: [] for e in self.ENGS}
        self.count = {e: 0 for e in self.ENGS}
        self.last_op = {e: None for e in self.ENGS}
        self.last_w = {}
        self.readers = {}
        self.waited = {e: {} for e in self.ENGS}
        self.eng_sems = {e: [] for e in self.ENGS}
        self.dsems = [es.enter_context(nc.semaphore(f"dma{i}")) for i in range(ndma_sems)]
        self.ndma = 0
        self.dma_ops = []
        self.banks = [es.enter_context(nc.psum_tensor(f"psbank{i}", [128, T], F32)) for i in range(8)]
        self.alias = {}
        self.nbank = 0

    def ps_reset(self):
        self.nbank = 0

    def bank(self, name):
        k = self.nbank % 8
        self.nbank += 1
        self.alias[name] = ("PS", k)
        return self.banks[k]

    def canon(self, k):
        if isinstance(k, str):
            return self.alias.get(k, k)
        if isinstance(k, tuple) and len(k) == 2 and isinstance(k[0], str):
            return self.alias.get(k[0] + str(k[1]), k)
        return k

    def _eng_sem(self, eng, k):
        lst = self.eng_sems[eng]
        while len(lst) <= k:
            lst.append(self.es.enter_context(self.nc.semaphore(f"s_{eng}{len(lst)}")))
        return lst[k]

    def _need(self, op, d, raw):
        if d is None or d is op:
            return
        if not d.dma and d.eng == op.eng:
            if op.eng == "pe" or op.eng == "sp":
                return
        key = id(d.sem)
        if self.waited[op.eng].get(key, 0) >= d.val:
            return
        self.waited[op.eng][key] = d.val
        op.waits.append((d.sem, d.val))

    def add(self, eng, fn, reads=(), writes=(), dma=False):
        op = Op()
        op.eng, op.fn, op.waits, op.dma = eng, fn, [], dma
        op.idx = self.count[eng]
        self.count[eng] += 1
        reads = [self.canon(k) for k in reads]
        writes = [self.canon(k) for k in writes]
        if dma:
            n = self.ndma
            R = len(self.dsems)
            op.sem = self.dsems[n % R]
            op.val = 16 * (n // R + 1)
            if n >= R:
                prev = self.dma_ops[n - R]
                self._need(op, prev, True)
            self.ndma += 1
            self.dma_ops.append(op)
        else:
            k = op.idx // SEM_ROT
            op.sem = self._eng_sem(eng, k)
            op.val = op.idx % SEM_ROT + 1
        for k in reads:
            self._need(op, self.last_w.get(k), True)
        for k in writes:
            self._need(op, self.last_w.get(k), False)
            for r in self.readers.get(k, ()):
                self._need(op, r, False)
        for k in reads:
            lst = self.readers.setdefault(k, [])
            if not dma:
                lst[:] = [r for r in lst if r.dma or r.eng != eng]
            lst.append(op)
        for k in writes:
            self.last_w[k] = op
            self.readers[k] = []
        self.ops[eng].append(op)
        self.last_op[eng] = op
        return op

    def phase_end(self):
        R = len(self.dsems)
        for eng in self.ENGS:
            op = Op()
            op.eng, op.fn, op.waits, op.dma, op.idx = eng, None, [], False, -1
            for x in self.ENGS:
                if x != eng and x != "sp" and self.last_op[x] is not None:
                    self._need(op, self.last_op[x], True)
            for d in self.dma_ops[-R:]:
                self._need(op, d, True)
            self.ops[eng].append(op)
        self.emit()
        self.ops = {e: [] for e in self.ENGS}
        self.last_w = {}
        self.readers = {}
        self.alias = {}
        self.nbank = 0

    def pe(self, fn, reads=(), writes=()):
        return self.add("pe", fn, reads, writes)

    def act(self, fn, reads=(), writes=()):
        return self.add("act", fn, reads, writes)

    def dve(self, fn, reads=(), writes=()):
        return self.add("dve", fn, reads, writes)

    def pool(self, fn, reads=(), writes=()):
        return self.add("pool", fn, reads, writes)

    def dma(self, out, in_, reads=(), writes=(), **kw):
        return self.add("sp", lambda e: e.dma_start(out=out, in_=in_, **kw), reads, writes, dma=True)

    def finish(self):
        pass

    def emit(self):
        nc = self.nc
        with nc.Block() as block:
            def replay(name, e):
                for op in self.ops[name]:
                    for (s, v) in op.waits:
                        e.wait_ge(s, v)
                    if op.fn is None:
                        continue
                    ins = op.fn(e)
                    ins.then_inc(op.sem, 16 if op.dma else 1)

            @block.tensor
            def _(e):
                replay("pe", e)

            @block.scalar
            def _(e):
                replay("act", e)

            @block.vector
            def _(e):
                replay("dve", e)

            @block.gpsimd
            def _(e):
                replay("pool", e)

            @block.sync
            def _(e):
                replay("sp", e)


_UNIQ = [0]


def sb(nc, es, name, shape, dt):
    _UNIQ[0] += 1
    return es.enter_context(nc.sbuf_tensor(f"{name}_{_UNIQ[0]}", list(shape), dt))


def psb(P, name):
    return P.bank(name)


def emit_l1(nc, P, es, xT, w_in, V, projT, tag="l1", ntok=TOK):
    NCC = NPROJ // 128
    Wb = sb(nc, es, tag + "Wb", [128, 8, NPROJ], BF16)
    Wst = [sb(nc, es, tag + f"Wst{i}", [128, NPROJ], F32) for i in range(2)]
    X = [sb(nc, es, tag + f"X{i}", [128, 8, T], F32) for i in range(2)]
    Xsq = sb(nc, es, tag + "Xsq", [128, 8, T], BF16)
    H = [sb(nc, es, tag + f"H{i}", [128, 8, T], BF16) for i in range(2)]
    ones = sb(nc, es, tag + "ones", [128, 128], BF16)
    rs = sb(nc, es, tag + "rs", [128, T], F32)
    O = [sb(nc, es, tag + f"O{i}", [128, T], F32) for i in range(4)]
    ps_ss = psb(P, tag + "ps_ss")
    ps = [psb(P, tag + f"ps{i}") for i in range(4)]

    P.pool(lambda e: e.memset(ones[:], 1.0), writes=["ones"])
    w_v = w_in.rearrange("(kc p) n -> kc p n", p=128)
    for kc in range(8):
        st = Wst[kc % 2]
        P.dma(st[:], w_v[kc], writes=[("Wst", kc % 2)])
        P.pool(lambda e, st=st, kc=kc: e.tensor_copy(out=Wb[:, kc, :], in_=st[:]),
               reads=[("Wst", kc % 2)], writes=[("Wb", kc)])
    x_v = xT.rearrange("(kc p) t -> p kc t", p=128)
    nout = 0
    for it in range(ntok // T):
        xs = X[it % 2]
        hs = H[it % 2]
        ts = slice(it * T, (it + 1) * T)
        P.dma(xs[:], x_v[:, :, ts], reads=[("xT", it)], writes=[("X", it % 2)])
        P.act(lambda e, xs=xs: e.activation(out=Xsq[:], in_=xs[:], func=AF.Square),
              reads=[("X", it % 2)], writes=["Xsq"])
        for kc in range(8):
            P.pe(lambda e, kc=kc: e.matmul(ps_ss[:], lhsT=ones[:], rhs=Xsq[:, kc, :],
                                           start=(kc == 0), stop=(kc == 7)),
                 reads=["ones", "Xsq"], writes=[tag + "ps_ss"])
        P.dve(lambda e: e.tensor_scalar(out=rs[:], in0=ps_ss[:], scalar1=1.0 / D_MODEL, scalar2=1e-6,
                                        op0=ALU.mult, op1=ALU.add),
              reads=[tag + "ps_ss"], writes=["rs"])
        P.act(lambda e: e.activation(out=rs[:], in_=rs[:], func=AF.Sqrt), reads=["rs"], writes=["rs"])
        P.dve(lambda e: e.reciprocal(out=rs[:], in_=rs[:]), reads=["rs"], writes=["rs"])
        for kc in range(8):
            P.dve(lambda e, kc=kc, xs=xs, hs=hs: e.scalar_tensor_tensor(
                out=hs[:, kc, :], in0=xs[:, kc, :], scalar=vcol(V, "pre_g", kc), in1=rs[:],
                op0=ALU.mult, op1=ALU.mult),
                reads=[("X", it % 2), "V", "rs"], writes=[("H", it % 2, kc)])
        for cc in range(NCC):
            pb = ps[cc % 4]
            for kc in range(8):
                P.pe(lambda e, kc=kc, cc=cc, pb=pb, hs=hs: e.matmul(
                    pb[:], lhsT=Wb[:, kc, cc * 128:(cc + 1) * 128], rhs=hs[:, kc, :],
                    start=(kc == 0), stop=(kc == 7)),
                    reads=[("Wb", kc), ("H", it % 2, kc)], writes=[(tag + "ps", cc % 4)])
            ob = O[nout % 4]
            if cc % 2 == 0:
                P.act(lambda e, ob=ob, pb=pb: e.copy(out=ob[:], in_=pb[:]),
                      reads=[(tag + "ps", cc % 4)], writes=[("O", nout % 4)])
            else:
                P.dve(lambda e, ob=ob, pb=pb: e.tensor_copy(out=ob[:], in_=pb[:]),
                      reads=[(tag + "ps", cc % 4)], writes=[("O", nout % 4)])
            P.dma(projT[cc * 128:(cc + 1) * 128, ts], ob[:], reads=[("O", nout % 4)],
                  writes=[("projT", it, cc)])
            nout += 1


VEC_SPEC = [("pre_g", 8), ("post_g", 8), ("a_dw_w", 62), ("a_dw_b", 2), ("a_ln_g", 2), ("a_ln_b", 2),
            ("a_pw_b", 2), ("b_lb", 8), ("b_norm_g", 2), ("c_conv_w", 24), ("c_conv_b", 6),
            ("c_norm_g", 2), ("d_qa_g", 2), ("d_kva_g", 1), ("c_dt_bias", 1), ("c_a_log", 1), ("c_d", 2)]
VEC_OFF = {}
_o = 0
for _n, _w in VEC_SPEC:
    VEC_OFF[_n] = (_o, _w)
    _o += _w
NV = _o


def vcol(V, name, j=0, n=1, rows=128):
    o, w = VEC_OFF[name]
    return V[0:rows, o + j:o + j + n]


def emit_A(nc, P, es, projT, V, pw_w, ycatT, ntok, tag="A"):
    HAL = 32
    val = [sb(nc, es, tag + f"val{i}", [128, 2, T], F32) for i in range(2)]
    glu = [sb(nc, es, tag + f"glu{i}", [128, 2, T], F32) for i in range(2)]
    gat = [sb(nc, es, tag + f"gat{i}", [128, 2, T], F32) for i in range(2)]
    gb = sb(nc, es, tag + "gb", [128, 2, HAL + T], F32)
    acc = sb(nc, es, tag + "acc", [128, 2, T], F32)
    sq = sb(nc, es, tag + "sq", [128, 2, T], F32)
    mean = sb(nc, es, tag + "mean", [128, T], F32)
    rstd = sb(nc, es, tag + "rstd", [128, T], F32)
    hs = sb(nc, es, tag + "hs", [128, 2, T], BF16)
    ya = [sb(nc, es, tag + f"ya{i}", [128, 2, T], BF16) for i in range(2)]
    onesf = sb(nc, es, tag + "onesf", [128, 128], F32)
    pwst = sb(nc, es, tag + "pwst", [128, 2, 256], F32)
    pwb = sb(nc, es, tag + "pwb", [128, 2, 256], BF16)
    ps1 = psb(P, tag + "ps1")
    ps2 = psb(P, tag + "ps2")
    pso = [psb(P, tag + f"pso{i}") for i in range(2)]

    P.pool(lambda e: e.memset(onesf[:], 1.0), writes=[tag + "onesf"])
    P.dma(pwst[:], pw_w.rearrange("(c p) n -> p c n", p=128), writes=[tag + "pwst"])
    P.pool(lambda e: e.tensor_copy(out=pwb[:], in_=pwst[:]), reads=[tag + "pwst"], writes=[tag + "pwb"])
    P.pool(lambda e: e.memset(gb[:, :, 0:HAL], 0.0), writes=[tag + "gb"])
    pv = projT.rearrange("(c p) t -> p c t", p=128)
    for it in range(ntok // T):
        ts = slice(it * T, (it + 1) * T)
        b = it % 2
        P.dma(val[b][:], pv[:, 0:2, ts], reads=[("projT", it)], writes=[(tag + "val", b)])
        P.dma(glu[b][:], pv[:, 2:4, ts], reads=[("projT", it)], writes=[(tag + "glu", b)])
        P.dma(gat[b][:], pv[:, 4:6, ts], reads=[("projT", it)], writes=[(tag + "gat", b)])
        P.act(lambda e, b=b: e.activation(out=glu[b][:], in_=glu[b][:], func=AF.Sigmoid),
              reads=[(tag + "glu", b)], writes=[(tag + "glu", b)])
        P.dve(lambda e, b=b: e.tensor_tensor(out=gb[:, :, HAL:HAL + T], in0=val[b][:], in1=glu[b][:], op=ALU.mult),
              reads=[(tag + "glu", b), (tag + "val", b)], writes=[tag + "gb"])
        P.act(lambda e, b=b: e.activation(out=gat[b][:], in_=gat[b][:], func=AF.Silu),
              reads=[(tag + "gat", b)], writes=[(tag + "gat", b)])
        for c in range(2):
            eng = P.dve
            for j in range(31):
                src = gb[:, c, HAL - 30 + j:HAL - 30 + j + T]
                wj = vcol(V, "a_dw_w", c * 31 + j)
                if j == 0:
                    eng(lambda e, c=c, src=src, wj=wj: e.tensor_scalar(
                        out=acc[:, c, :], in0=src, scalar1=wj, scalar2=vcol(V, "a_dw_b", c),
                        op0=ALU.mult, op1=ALU.add),
                        reads=[tag + "gb", "V"], writes=[(tag + "acc", c)])
                else:
                    eng(lambda e, c=c, src=src, wj=wj: e.scalar_tensor_tensor(
                        out=acc[:, c, :], in0=src, scalar=wj, in1=acc[:, c, :], op0=ALU.mult, op1=ALU.add),
                        reads=[tag + "gb", "V", (tag + "acc", c)], writes=[(tag + "acc", c)])
        P.act(lambda e: e.copy(out=gb[:, :, 0:HAL], in_=gb[:, :, T:T + HAL]),
              reads=[tag + "gb"], writes=[tag + "gb"])
        P.act(lambda e: e.activation(out=sq[:], in_=acc[:], func=AF.Square),
              reads=[(tag + "acc", 0), (tag + "acc", 1)], writes=[tag + "sq"])
        for c in range(2):
            P.pe(lambda e, c=c: e.matmul(ps1[:], lhsT=onesf[:], rhs=acc[:, c, :], start=(c == 0), stop=(c == 1)),
                 reads=[tag + "onesf", (tag + "acc", c)], writes=[tag + "ps1"])
        for c in range(2):
            P.pe(lambda e, c=c: e.matmul(ps2[:], lhsT=onesf[:], rhs=sq[:, c, :], start=(c == 0), stop=(c == 1)),
                 reads=[tag + "onesf", tag + "sq"], writes=[tag + "ps2"])
        P.dve(lambda e: e.tensor_scalar(out=mean[:], in0=ps1[:], scalar1=1.0 / 256, scalar2=None, op0=ALU.mult),
              reads=[tag + "ps1"], writes=[tag + "mean"])
        P.dve(lambda e: e.tensor_tensor(out=rstd[:], in0=mean[:], in1=mean[:], op=ALU.mult),
              reads=[tag + "mean"], writes=[tag + "rstd"])
        P.dve(lambda e: e.scalar_tensor_tensor(out=rstd[:], in0=ps2[:], scalar=1.0 / 256, in1=rstd[:],
                                               op0=ALU.mult, op1=ALU.subtract),
              reads=[tag + "ps2", tag + "rstd"], writes=[tag + "rstd"])
        P.dve(lambda e: e.tensor_scalar(out=rstd[:], in0=rstd[:], scalar1=1e-5, scalar2=None, op0=ALU.add),
              reads=[tag + "rstd"], writes=[tag + "rstd"])
        P.act(lambda e: e.activation(out=rstd[:], in_=rstd[:], func=AF.Sqrt), reads=[tag + "rstd"], writes=[tag + "rstd"])
        P.dve(lambda e: e.reciprocal(out=rstd[:], in_=rstd[:]), reads=[tag + "rstd"], writes=[tag + "rstd"])
        for c in range(2):
            P.dve(lambda e, c=c: e.tensor_tensor(out=acc[:, c, :], in0=acc[:, c, :], in1=mean[:], op=ALU.subtract),
                  reads=[(tag + "acc", c), tag + "mean", tag + "ps1"], writes=[(tag + "acc", c)])
            P.dve(lambda e, c=c: e.tensor_tensor(out=acc[:, c, :], in0=acc[:, c, :], in1=rstd[:], op=ALU.mult),
                  reads=[(tag + "acc", c), tag + "rstd"], writes=[(tag + "acc", c)])
            P.act(lambda e, c=c: e.activation(out=hs[:, c, :], in_=acc[:, c, :], func=AF.Silu,
                                              scale=vcol(V, "a_ln_g", c), bias=vcol(V, "a_ln_b", c)),
                  reads=[(tag + "acc", c), "V"], writes=[(tag + "hs", c)])
        for co in range(2):
            for ci in range(2):
                P.pe(lambda e, co=co, ci=ci: e.matmul(pso[co][:], lhsT=pwb[:, ci, co * 128:(co + 1) * 128],
                                                      rhs=hs[:, ci, :], start=(ci == 0), stop=(ci == 1)),
                     reads=[tag + "pwb", (tag + "hs", ci)], writes=[(tag + "pso", co)])
            P.dve(lambda e, co=co, b=b: e.scalar_tensor_tensor(
                out=ya[b][:, co, :], in0=pso[co][:], scalar=vcol(V, "a_pw_b", co), in1=gat[b][:, co, :],
                op0=ALU.add, op1=ALU.mult),
                reads=[(tag + "pso", co), (tag + "gat", b), "V"], writes=[(tag + "ya", b)])
        P.dma(ycatT.rearrange("(c p) t -> p c t", p=128)[:, 0:2, ts], ya[b][:],
              reads=[(tag + "ya", b)], writes=[("ycatA", it)])


COLMAP = [(0, 768), (768, 1792), (1792, 2560), (2564, 2820), (2820, 3076), (3076, 3204),
          (3236, 3492), (3204, 3236), (2560, 2564)]
R_VAL, R_GLU, R_AG = 0, 256, 512
R_BQ, R_BF, R_BI, R_BG = 768, 1024, 1280, 1536
R_XBC, R_Z, R_CQ, R_CKV, R_DG, R_KR, R_DT = 1792, 2560, 2816, 3072, 3200, 3456, 3488


def pack_w_in(w):
    out = np.zeros((D_MODEL, NPROJ), np.float32)
    o = 0
    for a, b in COLMAP:
        out[:, o:o + b - a] = w[:, a:b]
        o += b - a
    return out


def _pc(v):
    return np.ascontiguousarray(np.asarray(v, np.float32).reshape(-1, 128).T)


def pack_vecs(p, l):
    V = np.zeros((128, NV), np.float32)

    def put(name, arr):
        o, w = VEC_OFF[name]
        V[:arr.shape[0], o:o + arr.shape[1]] = arr

    put("pre_g", _pc(p["pre_norm_g"][l]))
    put("post_g", _pc(p["post_norm_g"][l]))
    w = np.asarray(p["a_dw_w"][l], np.float32)
    put("a_dw_w", np.concatenate([w[:, c * 128:(c + 1) * 128].T for c in range(2)], axis=1))
    put("a_dw_b", _pc(p["a_dw_b"][l]))
    put("a_ln_g", _pc(p["a_ln_g"][l]))
    put("a_ln_b", _pc(p["a_ln_b"][l]))
    put("a_pw_b", _pc(p["a_pw_b"][l]))
    lg = np.asarray(p["b_lb_logits"], np.float32)
    put("b_lb", np.concatenate([lg[:, c * 128:(c + 1) * 128].T for c in range(2)], axis=1))
    put("b_norm_g", _pc(p["b_norm_g"][l]))
    cw = np.asarray(p["c_conv_w"][l], np.float32)
    put("c_conv_w", np.concatenate([cw[:, c * 128:(c + 1) * 128].T for c in range(6)], axis=1))
    put("c_conv_b", _pc(p["c_conv_b"][l]))
    put("c_norm_g", _pc(p["c_norm_g"][l]))
    put("d_qa_g", _pc(p["d_qa_g"][l]))
    put("d_kva_g", _pc(p["d_kva_g"][l]))
    for nm in ("c_dt_bias", "c_a_log"):
        col = np.zeros((128, 1), np.float32)
        col[0:4, 0] = np.asarray(p[nm][l], np.float32)
        col[64:68, 0] = np.asarray(p[nm][l], np.float32)
        put(nm, col)
    cd = np.asarray(p["c_d"][l], np.float32)
    put("c_d", np.stack([np.repeat(cd[2 * g:2 * g + 2], 64) for g in range(2)], axis=1))
    return V


def emit_O(nc, P, es, ycatT, xT_in, V, w_out, xT_out, ntok, tag="O"):
    Wo = sb(nc, es, tag + "Wo", [128, 8, D_MODEL], BF16)
    Wst = [sb(nc, es, tag + f"Wst{i}", [128, D_MODEL], F32) for i in range(2)]
    Y = [sb(nc, es, tag + f"Y{i}", [128, 8, T], BF16) for i in range(2)]
    X = [sb(nc, es, tag + f"X{i}", [128, 8, T], F32) for i in range(2)]
    Yo = sb(nc, es, tag + "Yo", [128, 8, T], F32)
    Ysq = sb(nc, es, tag + "Ysq", [128, 8, T], BF16)
    ones = sb(nc, es, tag + "ones", [128, 128], BF16)
    rs = sb(nc, es, tag + "rs", [128, T], F32)
    ps = [psb(P, tag + f"ps{i}") for i in range(4)]
    ps_ss = psb(P, tag + "ps_ss")
    P.pool(lambda e: e.memset(ones[:], 1.0), writes=[tag + "ones"])
    w_v = w_out.rearrange("(kc p) n -> kc p n", p=128)
    for kc in range(8):
        st = Wst[kc % 2]
        P.dma(st[:], w_v[kc], writes=[(tag + "Wst", kc % 2)])
        P.pool(lambda e, st=st, kc=kc: e.tensor_copy(out=Wo[:, kc, :], in_=st[:]),
               reads=[(tag + "Wst", kc % 2)], writes=[(tag + "Wo", kc)])
    yv = ycatT.rearrange("(c p) t -> p c t", p=128)
    xv = xT_in.rearrange("(c p) t -> p c t", p=128)
    xo = xT_out.rearrange("(c p) t -> p c t", p=128)
    for it in range(ntok // T):
        ts = slice(it * T, (it + 1) * T)
        b = it % 2
        P.dma(Y[b][:], yv[:, :, ts], reads=[("ycatA", it), ("ycatB", it), ("ycatC", it), ("ycatD", it)],
              writes=[(tag + "Y", b)])
        P.dma(X[b][:], xv[:, :, ts], reads=[("xT", it)], writes=[(tag + "X", b)])
        for do in range(8):
            pb = ps[do % 4]
            for kc in range(8):
                P.pe(lambda e, kc=kc, do=do, pb=pb, b=b: e.matmul(
                    pb[:], lhsT=Wo[:, kc, do * 128:(do + 1) * 128], rhs=Y[b][:, kc, :],
                    start=(kc == 0), stop=(kc == 7)),
                    reads=[(tag + "Wo", kc), (tag + "Y", b)], writes=[(tag + "ps", do % 4)])
            P.act(lambda e, do=do, pb=pb: e.copy(out=Yo[:, do, :], in_=pb[:]),
                  reads=[(tag + "ps", do % 4)], writes=[(tag + "Yo", do)])
            P.act(lambda e, do=do, pb=pb: e.activation(out=Ysq[:, do, :], in_=pb[:], func=AF.Square),
                  reads=[(tag + "ps", do % 4)], writes=[(tag + "Ysq", do)])
        for do in range(8):
            P.pe(lambda e, do=do: e.matmul(ps_ss[:], lhsT=ones[:], rhs=Ysq[:, do, :],
                                           start=(do == 0), stop=(do == 7)),
                 reads=[tag + "ones", (tag + "Ysq", do)], writes=[tag + "ps_ss"])
        P.dve(lambda e: e.tensor_scalar(out=rs[:], in0=ps_ss[:], scalar1=1.0 / D_MODEL, scalar2=1e-6,
                                        op0=ALU.mult, op1=ALU.add), reads=[tag + "ps_ss"], writes=[tag + "rs"])
        P.act(lambda e: e.activation(out=rs[:], in_=rs[:], func=AF.Sqrt), reads=[tag + "rs"], writes=[tag + "rs"])
        P.dve(lambda e: e.reciprocal(out=rs[:], in_=rs[:]), reads=[tag + "rs"], writes=[tag + "rs"])
        for do in range(8):
            P.dve(lambda e, do=do: e.scalar_tensor_tensor(
                out=Yo[:, do, :], in0=Yo[:, do, :], scalar=vcol(V, "post_g", do), in1=rs[:],
                op0=ALU.mult, op1=ALU.mult), reads=[(tag + "Yo", do), tag + "rs", "V"], writes=[(tag + "Yo", do)])
            P.dve(lambda e, do=do, b=b: e.tensor_tensor(out=Yo[:, do, :], in0=Yo[:, do, :], in1=X[b][:, do, :],
                                                        op=ALU.add),
                   reads=[(tag + "Yo", do), (tag + "X", b)], writes=[(tag + "Yo", do)])
        P.dma(xo[:, :, ts], Yo[:], reads=[(tag + "Yo", do) for do in range(8)], writes=[("xTo", it)])


NCP = 8
TWO_PI = float(2 * np.pi)


def make_cp():
    cp = np.zeros((128, NCP), np.float32)
    inv = (10000.0 ** (-np.arange(0, 32, 2, dtype=np.float32) / 32)).astype(np.float32)
    cp[64:96, 0] = np.concatenate([inv, inv])
    cp[64:96, 1] = np.concatenate([-np.ones(16), np.ones(16)])
    cp[64:96, 2] = np.concatenate([-np.pi * np.ones(16), np.pi * np.ones(16)])
    cp[:, 3] = np.pi
    cp[:, 4] = np.pi / 2
    return cp


def pack_qb(w):
    w = np.asarray(w, np.float32).reshape(256, 4, 96)
    b = w.copy()
    b[:, :, 64:80] = w[:, :, 80:96]
    b[:, :, 80:96] = w[:, :, 64:80]
    return np.ascontiguousarray(np.stack([w, b], axis=2))


def pack_kvb(w):
    w = np.asarray(w, np.float32).reshape(128, 4, 128)
    return np.ascontiguousarray(np.stack([w[:, :, 0:64].reshape(128, 256), w[:, :, 64:128].reshape(128, 256)], axis=1))


def emit_D(nc, P, es, projT, pos, V, CP, qbw, kvbw, QT, KT, VD, OD, ycatT, ntok, tag="D"):
    nt = ntok // T
    SC = float(96 ** -0.5)
    qst = sb(nc, es, tag + "qst", [128, 2, 768], F32)
    qb = sb(nc, es, tag + "qb", [128, 2, 768], BF16)
    kst = sb(nc, es, tag + "kst", [128, 512], F32)
    kb_ = sb(nc, es, tag + "kb", [128, 512], BF16)
    ones = sb(nc, es, tag + "ones", [128, 128], BF16)
    cq = [sb(nc, es, tag + f"cq{i}", [128, 2, T], F32) for i in range(2)]
    ckv = [sb(nc, es, tag + f"ckv{i}", [128, T], F32) for i in range(2)]
    krA = [sb(nc, es, tag + f"krA{i}", [128, T], F32) for i in range(2)]
    krB = [sb(nc, es, tag + f"krB{i}", [128, T], F32) for i in range(2)]
    posi = [sb(nc, es, tag + f"posi{i}", [128, T], I32) for i in range(2)]
    sqb = sb(nc, es, tag + "sqb", [128, 3, T], BF16)
    rs = sb(nc, es, tag + "rs", [128, T], F32)
    rs2 = sb(nc, es, tag + "rs2", [128, T], F32)
    cqn = sb(nc, es, tag + "cqn", [128, 2, T], BF16)
    ckvn = sb(nc, es, tag + "ckvn", [128, T], BF16)
    ang = sb(nc, es, tag + "ang", [128, T], F32)
    cosT = sb(nc, es, tag + "cos", [128, T], F32)
    sinT = sb(nc, es, tag + "sin", [128, T], F32)
    tmp = sb(nc, es, tag + "tmp", [128, T], F32)
    tmp2 = sb(nc, es, tag + "tmp2", [128, T], F32)
    kf = sb(nc, es, tag + "kf", [128, T], F32)
    ki = sb(nc, es, tag + "ki", [128, T], I32)
    qt = [sb(nc, es, tag + f"qt{i}", [96, 4, T], BF16) for i in range(2)]
    kt = [sb(nc, es, tag + f"kt{i}", [96, 4, T], BF16) for i in range(2)]
    vt = [sb(nc, es, tag + f"vt{i}", [128, 4, 4, 65], BF16) for i in range(2)]
    ps_a = psb(P, tag + "ps_a")
    ps_b = psb(P, tag + "ps_b")
    ps_q = [psb(P, tag + f"ps_q{i}") for i in range(2)]
    ps_q2 = [psb(P, tag + f"ps_q2{i}") for i in range(2)]
    ps_v = psb(P, tag + "ps_v")

    P.pool(lambda e: e.memset(ones[:], 1.0), writes=[tag + "ones"])
    P.dma(qst[:], qbw.rearrange("(c p) n -> p c n", p=128), writes=[tag + "qst"])
    P.pool(lambda e: e.tensor_copy(out=qb[:], in_=qst[:]), reads=[tag + "qst"], writes=[tag + "qb"])
    P.dma(kst[:], kvbw[:, :], writes=[tag + "kst"])
    P.pool(lambda e: e.tensor_copy(out=kb_[:], in_=kst[:]), reads=[tag + "kst"], writes=[tag + "kb"])
    for i in range(2):
        P.pool(lambda e, i=i: e.memset(vt[i][:, :, :, 64:65], 1.0), writes=[(tag + "vt", i)])
    pv = projT.rearrange("(c p) t -> p c t", p=128)
    QTv = QT.rearrange("h d t -> d h t")
    KTv = KT.rearrange("h d t -> d h t")
    for it in range(nt):
        ts = slice(it * T, (it + 1) * T)
        b = it % 2
        P.dma(cq[b][:], pv[:, 22:24, ts], reads=[("projT", it)], writes=[(tag + "cq", b)])
        P.dma(ckv[b][:], projT[R_CKV:R_CKV + 128, ts], reads=[("projT", it)], writes=[(tag + "ckv", b)])
        P.dma(krA[b][64:96, :], projT[R_KR:R_KR + 32, ts], reads=[("projT", it)], writes=[(tag + "krA", b)])
        P.dma(krB[b][64:80, :], projT[R_KR + 16:R_KR + 32, ts], reads=[("projT", it)], writes=[(tag + "krB", b)])
        P.dma(krB[b][80:96, :], projT[R_KR:R_KR + 16, ts], reads=[("projT", it)], writes=[(tag + "krB", b)])
        P.dma(posi[b][64:96, :], pos[0:1, ts].partition_broadcast(32), writes=[(tag + "posi", b)])
        P.act(lambda e, b=b: e.activation(out=sqb[:, 0:2, :], in_=cq[b][:], func=AF.Square),
              reads=[(tag + "cq", b)], writes=[tag + "sqq"])
        P.act(lambda e, b=b: e.activation(out=sqb[:, 2, :], in_=ckv[b][:], func=AF.Square),
              reads=[(tag + "ckv", b)], writes=[tag + "sqk"])
        for c in range(2):
            P.pe(lambda e, c=c: e.matmul(ps_a[:], lhsT=ones[:], rhs=sqb[:, c, :], start=(c == 0), stop=(c == 1)),
                 reads=[tag + "ones", tag + "sqq"], writes=[tag + "ps_a"])
        P.pe(lambda e: e.matmul(ps_b[:], lhsT=ones[:], rhs=sqb[:, 2, :], start=True, stop=True),
             reads=[tag + "ones", tag + "sqk"], writes=[tag + "ps_b"])
        for (pss, r, n) in ((ps_a, rs, 256.0), (ps_b, rs2, 128.0)):
            k1, k2 = (tag + "ps_a", tag + "rs") if pss is ps_a else (tag + "ps_b", tag + "rs2")
            P.dve(lambda e, pss=pss, r=r, n=n: e.tensor_scalar(out=r[:], in0=pss[:], scalar1=1.0 / n, scalar2=1e-6,
                                                             op0=ALU.mult, op1=ALU.add), reads=[k1], writes=[k2])
            P.act(lambda e, r=r: e.activation(out=r[:], in_=r[:], func=AF.Sqrt), reads=[k2], writes=[k2])
            P.dve(lambda e, r=r: e.reciprocal(out=r[:], in_=r[:]), reads=[k2], writes=[k2])
        for c in range(2):
            P.dve(lambda e, c=c, b=b: e.scalar_tensor_tensor(out=cqn[:, c, :], in0=cq[b][:, c, :],
                                                            scalar=vcol(V, "d_qa_g", c), in1=rs[:],
                                                            op0=ALU.mult, op1=ALU.mult),
                  reads=[(tag + "cq", b), tag + "rs", "V"], writes=[tag + "cqn"])
        P.dve(lambda e, b=b: e.scalar_tensor_tensor(out=ckvn[:], in0=ckv[b][:], scalar=vcol(V, "d_kva_g"),
                                                     in1=rs2[:], op0=ALU.mult, op1=ALU.mult),
              reads=[(tag + "ckv", b), tag + "rs2", "V"], writes=[tag + "ckvn"])
        R_ = slice(64, 96)
        P.dve(lambda e, b=b: e.tensor_copy(out=ang[R_, :], in_=posi[b][R_, :]), reads=[(tag + "posi", b)],
              writes=[tag + "ang"])
        P.dve(lambda e: e.tensor_scalar(out=ang[R_, :], in0=ang[R_, :], scalar1=CP[R_, 0:1], scalar2=None,
                                        op0=ALU.mult), reads=[tag + "ang", "CP"], writes=[tag + "ang"])
        def reduce_angle(dst, kd, add_half_pi):
            if add_half_pi:
                P.dve(lambda e: e.tensor_scalar(out=dst[R_, :], in0=ang[R_, :], scalar1=CP[R_, 4:5], scalar2=None,
                                                op0=ALU.add), reads=[tag + "ang", "CP"], writes=[kd])
            else:
                P.dve(lambda e: e.tensor_copy(out=dst[R_, :], in_=ang[R_, :]), reads=[tag + "ang"], writes=[kd])
            P.dve(lambda e: e.tensor_scalar(out=kf[R_, :], in0=dst[R_, :], scalar1=1.0 / TWO_PI, scalar2=None,
                                            op0=ALU.mult), reads=[kd], writes=[tag + "kf"])
            P.dve(lambda e: e.tensor_copy(out=ki[R_, :], in_=kf[R_, :]), reads=[tag + "kf"], writes=[tag + "ki"])
            P.dve(lambda e: e.tensor_copy(out=kf[R_, :], in_=ki[R_, :]), reads=[tag + "ki"], writes=[tag + "kf"])
            P.dve(lambda e: e.scalar_tensor_tensor(out=dst[R_, :], in0=kf[R_, :], scalar=-6.28125, in1=dst[R_, :],
                                                   op0=ALU.mult, op1=ALU.add), reads=[tag + "kf", kd], writes=[kd])
            P.dve(lambda e: e.scalar_tensor_tensor(out=dst[R_, :], in0=kf[R_, :], scalar=-(TWO_PI - 6.28125),
                                                   in1=dst[R_, :], op0=ALU.mult, op1=ALU.add),
                  reads=[tag + "kf", kd], writes=[kd])
            P.dve(lambda e: e.tensor_scalar(out=kf[R_, :], in0=dst[R_, :], scalar1=float(np.pi), scalar2=None,
                                            op0=ALU.is_gt), reads=[kd], writes=[tag + "kf"])
            P.dve(lambda e: e.scalar_tensor_tensor(out=dst[R_, :], in0=kf[R_, :], scalar=-TWO_PI, in1=dst[R_, :],
                                                   op0=ALU.mult, op1=ALU.add), reads=[tag + "kf", kd], writes=[kd])

        reduce_angle(tmp, tag + "tmp", False)
        P.act(lambda e: e.activation(out=sinT[R_, :], in_=tmp[R_, :], func=AF.Sin, scale=CP[R_, 1:2]),
              reads=[tag + "tmp", "CP"], writes=[tag + "sin"])
        reduce_angle(tmp2, tag + "tmp2", True)
        P.act(lambda e: e.activation(out=cosT[R_, :], in_=tmp2[R_, :], func=AF.Sin),
              reads=[tag + "tmp2"], writes=[tag + "cos"])
        P.dve(lambda e, b=b: e.tensor_tensor(out=krA[b][R_, :], in0=krA[b][R_, :], in1=cosT[R_, :], op=ALU.mult),
              reads=[(tag + "krA", b), tag + "cos"], writes=[(tag + "krA", b)])
        P.dve(lambda e, b=b: e.tensor_tensor(out=krB[b][R_, :], in0=krB[b][R_, :], in1=sinT[R_, :], op=ALU.mult),
              reads=[(tag + "krB", b), tag + "sin"], writes=[(tag + "krB", b)])
        for h in range(4):
            P.dve(lambda e, b=b, h=h: e.tensor_tensor(out=kt[b][R_, h, :], in0=krA[b][R_, :], in1=krB[b][R_, :],
                                                       op=ALU.add),
                   reads=[(tag + "krA", b), (tag + "krB", b)], writes=[(tag + "kt", b)])
        for h in range(4):
            P.pe(lambda e, h=h: e.matmul(ps_q[h % 2][0:64, :], lhsT=kb_[:, h * 64:(h + 1) * 64], rhs=ckvn[:],
                                         start=True, stop=True),
                 reads=[tag + "kb", tag + "ckvn"], writes=[(tag + "ps_q", h % 2)])
            P.act(lambda e, h=h, b=b: e.copy(out=kt[b][0:64, h, :], in_=ps_q[h % 2][0:64, :]),
                  reads=[(tag + "ps_q", h % 2)], writes=[(tag + "kt", b)])
        for h in range(4):
            pA, pB = ps_q[h % 2], ps_q2[h % 2]
            for c in range(2):
                P.pe(lambda e, h=h, c=c, pA=pA: e.matmul(pA[0:96, :], lhsT=qb[:, c, (h * 2) * 96:(h * 2 + 1) * 96],
                                                         rhs=cqn[:, c, :], start=(c == 0), stop=(c == 1)),
                     reads=[tag + "qb", tag + "cqn"], writes=[(tag + "ps_q", h % 2)])
            for c in range(2):
                P.pe(lambda e, h=h, c=c, pB=pB: e.matmul(pB[0:96, :], lhsT=qb[:, c, (h * 2 + 1) * 96:(h * 2 + 2) * 96],
                                                         rhs=cqn[:, c, :], start=(c == 0), stop=(c == 1)),
                     reads=[tag + "qb", tag + "cqn"], writes=[(tag + "ps_q2", h % 2)])
            P.act(lambda e, h=h, b=b, pA=pA: e.copy(out=qt[b][0:64, h, :], in_=pA[0:64, :]),
                  reads=[(tag + "ps_q", h % 2)], writes=[(tag + "qt", b)])
            P.dve(lambda e, pA=pA: e.tensor_tensor(out=tmp[R_, :], in0=pA[R_, :], in1=cosT[R_, :], op=ALU.mult),
                  reads=[(tag + "ps_q", h % 2), tag + "cos"], writes=[tag + "tmp"])
            P.dve(lambda e, pB=pB: e.tensor_tensor(out=tmp2[R_, :], in0=pB[R_, :], in1=sinT[R_, :], op=ALU.mult),
                  reads=[(tag + "ps_q2", h % 2), tag + "sin"], writes=[tag + "tmp2"])
            P.dve(lambda e, h=h, b=b: e.tensor_tensor(out=qt[b][R_, h, :], in0=tmp[R_, :], in1=tmp2[R_, :], op=ALU.add),
                  reads=[tag + "tmp", tag + "tmp2"], writes=[(tag + "qt", b)])
        for j in range(4):
            P.pe(lambda e, j=j: e.matmul(ps_v[:, 0:256],
                                         lhsT=ckvn[:, j * 128:(j + 1) * 128], rhs=kb_[:, 256:512],
                                         start=True, stop=True),
                 reads=[tag + "ckvn", tag + "kb"], writes=[tag + "ps_v"])
            P.act(lambda e, j=j, b=b: e.copy(out=vt[b][:, j, :, 0:64],
                                             in_=ps_v[:, 0:256].rearrange("p (h v) -> p h v", h=4)),
                  reads=[tag + "ps_v"], writes=[(tag + "vt", b)])
        P.dma(QTv[:, :, ts], qt[b][:], reads=[(tag + "qt", b)], writes=[("QT", it)])
        P.dma(KTv[:, :, ts], kt[b][:], reads=[(tag + "kt", b)], writes=[("KT", it)])
        for h in range(4):
            P.dma(VD[h, :, it * 4:(it + 1) * 4, :], vt[b][:, :, h, :], reads=[(tag + "vt", b)], writes=[("VD", it)])

    P.ps_reset()
    Kall = sb(nc, es, tag + "Kall", [96, ntok], BF16)
    Vall = sb(nc, es, tag + "Vall", [128, ntok // 128, 65], BF16)
    Qg = [sb(nc, es, tag + f"Qg{i}", [96, T], BF16) for i in range(2)]
    Pt = [sb(nc, es, tag + f"Pt{i}", [128, T], BF16) for i in range(5)]
    Ost = [sb(nc, es, tag + f"Ost{i}", [65, T], F32) for i in range(2)]
    S_ps = [psb(P, tag + f"S_ps{i}") for i in range(5)]
    O_ps = [psb(P, tag + f"O_ps{i}") for i in range(2)]
    allk = [("KT", i) for i in range(nt)]
    allv = [("VD", i) for i in range(nt)]
    n = 0
    ng = 0
    for h in range(4):
        P.dma(Kall[:], KT[h], reads=allk, writes=[tag + "Kall"])
        P.dma(Vall[:], VD[h], reads=allv, writes=[tag + "Vall"])
        for qg in range(nt):
            gb = ng % 2
            ng += 1
            P.dma(Qg[gb][:], QT[h, :, qg * T:(qg + 1) * T], reads=[("QT", qg)], writes=[(tag + "Qg", gb)])
            nkb = 4 * qg + 4
            SKEW = 4
            tiles = []
            for step in range(nkb + SKEW):
                if step < nkb:
                    kb = step
                    j = kb - 4 * qg
                    c0 = max(0, j) * 128
                    sp, pt = S_ps[n % 5], Pt[n % 5]
                    kS, kP = (tag + "S_ps", n % 5), (tag + "Pt", n % 5)
                    n += 1
                    tiles.append((kb, c0, pt, kP))
                    P.pe(lambda e, sp=sp, kb=kb, gb=gb, c0=c0: e.matmul(
                        sp[:, c0:T], lhsT=Kall[:, kb * 128:(kb + 1) * 128], rhs=Qg[gb][:, c0:T], start=True, stop=True),
                        reads=[tag + "Kall", (tag + "Qg", gb)], writes=[kS])
                    P.act(lambda e, sp=sp, pt=pt, c0=c0: e.activation(out=pt[:, c0:T], in_=sp[:, c0:T], func=AF.Exp,
                                                                      scale=SC), reads=[kS], writes=[kP])
                    if j >= 0:
                        P.pool(lambda e, pt=pt, c0=c0: e.memset(pt[64:128, c0:c0 + 64], 0.0), reads=[kP], writes=[kP])
                if step >= SKEW:
                    kb, c0, pt, kP = tiles[step - SKEW]
                    P.pe(lambda e, pt=pt, kb=kb, gb=gb, c0=c0, nkb=nkb: e.matmul(
                        O_ps[gb][0:65, c0:T], lhsT=Vall[:, kb, :], rhs=pt[:, c0:T], start=(kb == 0), stop=(kb == nkb - 1)),
                        reads=[tag + "Vall", kP], writes=[(tag + "O_ps", gb)])
            P.dve(lambda e, gb=gb: e.tensor_copy(out=Ost[gb][:], in_=O_ps[gb][0:65, :]),
                  reads=[(tag + "O_ps", gb)], writes=[(tag + "Ost", gb)])
            P.dma(OD[h, :, qg * T:(qg + 1) * T], Ost[gb][:], reads=[(tag + "Ost", gb)], writes=[("OD", qg, h)])

    Oa = [sb(nc, es, tag + f"Oa{i}", [128, 2, T], F32) for i in range(2)]
    La = [sb(nc, es, tag + f"La{i}", [128, 2, T], F32) for i in range(2)]
    Ga = [sb(nc, es, tag + f"Ga{i}", [128, 2, T], F32) for i in range(2)]
    Yd = [sb(nc, es, tag + f"Yd{i}", [128, 2, T], BF16) for i in range(2)]
    yv = ycatT.rearrange("(c p) t -> p c t", p=128)
    for it in range(nt):
        ts = slice(it * T, (it + 1) * T)
        b = it % 2
        odk = [("OD", it, h) for h in range(4)]
        for h in range(4):
            P.dma(Oa[b][(h % 2) * 64:(h % 2) * 64 + 64, h // 2, :], OD[h, 0:64, ts], reads=odk, writes=[(tag + "Oa", b)])
            P.dma(La[b][(h % 2) * 64:(h % 2) * 64 + 64, h // 2, :], OD[h, 64:65, ts].partition_broadcast(64),
                  reads=odk, writes=[(tag + "La", b)])
        P.dma(Ga[b][:], pv[:, 25:27, ts], reads=[("projT", it)], writes=[(tag + "Ga", b)])
        P.act(lambda e, b=b: e.activation(out=Ga[b][:], in_=Ga[b][:], func=AF.Silu), reads=[(tag + "Ga", b)],
              writes=[(tag + "Ga", b)])
        P.dve(lambda e, b=b: e.reciprocal(out=La[b][:], in_=La[b][:]), reads=[(tag + "La", b)], writes=[(tag + "La", b)])
        P.dve(lambda e, b=b: e.tensor_tensor(out=Oa[b][:], in0=Oa[b][:], in1=La[b][:], op=ALU.mult),
              reads=[(tag + "Oa", b), (tag + "La", b)], writes=[(tag + "Oa", b)])
        P.dve(lambda e, b=b: e.tensor_tensor(out=Yd[b][:], in0=Oa[b][:], in1=Ga[b][:], op=ALU.mult),
              reads=[(tag + "Oa", b), (tag + "Ga", b)], writes=[(tag + "Yd", b)])
        P.dma(yv[:, 6:8, ts], Yd[b][:], reads=[(tag + "Yd", b)], writes=[("ycatD", it)])


def emit_lb(nc, P, es, V, LB, l):
    ex = sb(nc, es, "lb_ex", [128, 2, 4], F32)
    sm = sb(nc, es, "lb_sm", [128, 2], F32)
    o, w = VEC_OFF["b_lb"]
    lg = V[:, o:o + 8].rearrange("p (c l) -> p c l", c=2)
    P.act(lambda e: e.activation(out=ex[:], in_=lg, func=AF.Exp), reads=["V"], writes=["lb_ex"])
    P.dve(lambda e: e.tensor_reduce(out=sm[:], in_=ex[:], axis=AX.X, op=ALU.add), reads=["lb_ex"], writes=["lb_sm"])
    P.dve(lambda e: e.reciprocal(out=sm[:], in_=sm[:]), reads=["lb_sm"], writes=["lb_sm"])
    P.pool(lambda e: e.memset(LB[:, 0, :], 0.0), writes=["LB"])
    for j in range(1, l + 1):
        P.dve(lambda e, j=j: e.tensor_tensor(out=LB[:, 0, :], in0=LB[:, 0, :], in1=ex[:, :, j], op=ALU.add),
              reads=["LB", "lb_ex"], writes=["LB"])
    P.dve(lambda e: e.tensor_tensor(out=LB[:, 0, :], in0=LB[:, 0, :], in1=sm[:], op=ALU.mult),
          reads=["LB", "lb_sm"], writes=["LB"])
    P.dve(lambda e: e.tensor_scalar(out=LB[:, 1, :], in0=LB[:, 0, :], scalar1=-1.0, scalar2=1.0, op0=ALU.mult,
                                    op1=ALU.add), reads=["LB"], writes=["LB"])
    P.dve(lambda e: e.tensor_scalar(out=LB[:, 2, :], in0=LB[:, 1, :], scalar1=-1.0, scalar2=None, op0=ALU.mult),
          reads=["LB"], writes=["LB"])


def emit_B(nc, P, es, projT, V, LB, KC, ycatT, ntok, tag="B"):
    nt = ntok // T
    ident, bd, rmask, rm, tri2 = KC["ident"], KC["bd"], KC["rmask"], KC["rm"], KC["tri2"]
    q = [sb(nc, es, tag + f"q{i}", [128, 2, T], F32) for i in range(2)]
    f = [sb(nc, es, tag + f"f{i}", [128, 2, T], F32) for i in range(2)]
    vi = [sb(nc, es, tag + f"vi{i}", [128, 2, T], F32) for i in range(2)]
    g = [sb(nc, es, tag + f"g{i}", [128, 2, T], F32) for i in range(2)]
    kk = sb(nc, es, tag + "kk", [128, 2, T], F32)
    bb = sb(nc, es, tag + "bb", [128, 2, T], F32)
    t1 = sb(nc, es, tag + "t1", [128, 2, T], F32)
    t4 = sb(nc, es, tag + "t4", [128, 2, T], F32)
    e3 = sb(nc, es, tag + "e3", [128, 2, T], F32)
    ee = sb(nc, es, tag + "ee", [128, 2, T], F32)
    qt = sb(nc, es, tag + "qt", [128, 2, T], BF16)
    ktl = sb(nc, es, tag + "ktl", [128, 2, T], BF16)
    qe = sb(nc, es, tag + "qe", [128, 2, T], BF16)
    kh = sb(nc, es, tag + "kh", [128, 2, T], BF16)
    vb = sb(nc, es, tag + "vb", [128, 2, T], BF16)
    khT = sb(nc, es, tag + "khT", [128, 4, 256], BF16)
    vT = sb(nc, es, tag + "vT", [128, 4, 256], BF16)
    Asb = [sb(nc, es, tag + f"Asb{i}", [128, 4, 128], BF16) for i in range(4)]
    qtm = [sb(nc, es, tag + f"qtm{i}", [128, 2, T], BF16) for i in range(2)]
    qem = [sb(nc, es, tag + f"qem{i}", [128, 2, T], BF16) for i in range(2)]
    vTm = [sb(nc, es, tag + f"vTm{i}", [128, 4, 256], BF16) for i in range(2)]
    S = sb(nc, es, tag + "S", [128, 128], F32)
    Sbf = sb(nc, es, tag + "Sbf", [128, 128], BF16)
    osq = sb(nc, es, tag + "osq", [128, 2, T], F32)
    rs = sb(nc, es, tag + "rs", [128, 2, T], F32)
    yb = [sb(nc, es, tag + f"yb{i}", [128, 2, T], BF16) for i in range(2)]
    pkT = [psb(P, tag + f"pkT{i}") for i in range(2)]
    pvT = [psb(P, tag + f"pvT{i}") for i in range(2)]
    pS = [psb(P, tag + f"pS{i}") for i in range(4)]
    pO = [psb(P, tag + f"pO{i}") for i in range(2)]
    pU = [psb(P, tag + f"pU{i}") for i in range(2)]

    P.pool(lambda e: e.memset(S[:], 0.0), writes=[tag + "S"])
    P.pool(lambda e: e.memset(Sbf[:], 0.0), writes=[tag + "Sbf"])
    pv = projT.rearrange("(c p) t -> p c t", p=128)
    yv = ycatT.rearrange("(c p) t -> p c t", p=128)
    nU = 0
    for it in range(nt):
        ts = slice(it * T, (it + 1) * T)
        b = it % 2
        P.dma(q[b][:], pv[:, 6:8, ts], reads=[("projT", it)], writes=[(tag + "q", b)])
        P.dma(f[b][:], pv[:, 8:10, ts], reads=[("projT", it)], writes=[(tag + "f", b)])
        P.dma(vi[b][:], pv[:, 10:12, ts], reads=[("projT", it)], writes=[(tag + "vi", b)])
        P.dma(g[b][:], pv[:, 12:14, ts], reads=[("projT", it)], writes=[(tag + "g", b)])
        fb, qb_, vib, gb_ = f[b], q[b], vi[b], g[b]
        kf, kq, kv, kg = (tag + "f", b), (tag + "q", b), (tag + "vi", b), (tag + "g", b)
        P.act(lambda e, fb=fb: e.activation(out=fb[:], in_=fb[:], func=AF.Sigmoid), reads=[kf], writes=[kf])
        for c in range(2):
            P.dve(lambda e, c=c, fb=fb: e.tensor_scalar(out=kk[:, c, :], in0=fb[:, c, :], scalar1=LB[:, 2, c:c + 1],
                                                        scalar2=LB[:, 1, c:c + 1], op0=ALU.mult, op1=ALU.add),
                  reads=[kf, "LB"], writes=[tag + "kk"])
            P.dve(lambda e, c=c, fb=fb: e.tensor_scalar(out=fb[:, c, :], in0=fb[:, c, :], scalar1=LB[:, 1, c:c + 1],
                                                        scalar2=LB[:, 0, c:c + 1], op0=ALU.mult, op1=ALU.add),
                  reads=[kf, "LB", tag + "kk"], writes=[kf])
        P.act(lambda e, fb=fb: e.activation(out=fb[:], in_=fb[:], func=AF.Ln), reads=[kf], writes=[kf])
        for c in range(2):
            P.dve(lambda e, c=c, fb=fb: e.tensor_tensor_scan(out=bb[:, c, :], data0=rmask[:], data1=fb[:, c, :],
                                                             initial=0.0, op0=ALU.mult, op1=ALU.add),
                  reads=[kf, "KC"], writes=[tag + "bb"])
        b4 = bb[:].rearrange("p c (n s) -> p c n s", s=64)
        mid = b4[:, :, :, 31:32].to_broadcast([128, 2, 8, 64])
        last = b4[:, :, :, 63:64].to_broadcast([128, 2, 8, 64])
        t14 = t1[:].rearrange("p c (n s) -> p c n s", s=64)
        t44 = t4[:].rearrange("p c (n s) -> p c n s", s=64)
        P.dve(lambda e: e.tensor_tensor(out=t14, in0=b4, in1=mid, op=ALU.subtract), reads=[tag + "bb"], writes=[tag + "t1"])
        P.dve(lambda e: e.tensor_tensor(out=t44, in0=last, in1=b4, op=ALU.subtract), reads=[tag + "bb"], writes=[tag + "t4"])
        P.act(lambda e: e.activation(out=e3[:], in_=bb[:], func=AF.Exp), reads=[tag + "bb"], writes=[tag + "e3"])
        P.dve(lambda e, qb_=qb_: e.tensor_tensor(out=qe[:], in0=qb_[:], in1=e3[:], op=ALU.mult),
              reads=[kq, tag + "e3"], writes=[tag + "qe"])
        P.act(lambda e: e.activation(out=ee[:], in_=t1[:], func=AF.Exp), reads=[tag + "t1"], writes=[tag + "ee"])
        P.dve(lambda e, qb_=qb_: e.tensor_tensor(out=qt[:], in0=qb_[:], in1=ee[:], op=ALU.mult),
              reads=[kq, tag + "ee"], writes=[tag + "qt"])
        P.act(lambda e: e.activation(out=ee[:], in_=t1[:], func=AF.Exp, scale=-1.0), reads=[tag + "t1", tag + "qt"],
              writes=[tag + "ee"])
        P.dve(lambda e: e.tensor_tensor(out=ktl[:], in0=kk[:], in1=ee[:], op=ALU.mult),
              reads=[tag + "kk", tag + "ee"], writes=[tag + "ktl"])
        P.act(lambda e: e.activation(out=t4[:], in_=t4[:], func=AF.Exp), reads=[tag + "t4"], writes=[tag + "t4"])
        P.dve(lambda e: e.tensor_tensor(out=kh[:], in0=kk[:], in1=t4[:], op=ALU.mult),
               reads=[tag + "kk", tag + "t4"], writes=[tag + "kh"])
        P.pool(lambda e, vib=vib: e.tensor_copy(out=vb[:], in_=vib[:]), reads=[kv], writes=[tag + "vb"])
        P.act(lambda e, gb_=gb_: e.activation(out=gb_[:], in_=gb_[:], func=AF.Silu), reads=[kg], writes=[kg])
        for (src, ksrc, pst, kps, dst, kdst) in ((kh, tag + "kh", pkT, tag + "pkT", khT, tag + "khT"),
                                                  (vb, tag + "vb", pvT, tag + "pvT", vT, tag + "vT")):
            for j in range(4):
                for c in range(2):
                    P.pe(lambda e, src=src, pst=pst, j=j, c=c: e.matmul(
                        pst[j // 2][:, (j % 2) * 256 + c * 128:(j % 2) * 256 + c * 128 + 128],
                        lhsT=src[:, c, j * 128:(j + 1) * 128], rhs=ident[:], start=True, stop=True),
                        reads=[ksrc, "KC"], writes=[(kps, j // 2)])
            for hf in range(2):
                eng = P.act if hf == 0 else P.dve
                if hf == 0:
                    P.act(lambda e, pst=pst, dst=dst: e.copy(out=dst[:, 0:2, :].rearrange("p j n -> p (j n)"), in_=pst[0][:]),
                          reads=[(kps, 0)], writes=[kdst])
                else:
                    P.dve(lambda e, pst=pst, dst=dst: e.tensor_copy(out=dst[:, 2:4, :].rearrange("p j n -> p (j n)"), in_=pst[1][:]),
                          reads=[(kps, 1)], writes=[kdst])
        for e_ in range(2):
            P.dve(lambda e, e_=e_: e.tensor_scalar(out=qtm[e_][:], in0=qt[:], scalar1=rm[:, e_:e_ + 1], scalar2=None,
                                                   op0=ALU.mult), reads=[tag + "qt", "KC"], writes=[(tag + "qtm", e_)])
            P.dve(lambda e, e_=e_: e.tensor_scalar(out=qem[e_][:], in0=qe[:], scalar1=rm[:, e_:e_ + 1], scalar2=None,
                                                   op0=ALU.mult), reads=[tag + "qe", "KC"], writes=[(tag + "qem", e_)])
            P.dve(lambda e, e_=e_: e.tensor_scalar(out=vTm[e_][:], in0=vT[:], scalar1=rm[:, e_:e_ + 1], scalar2=None,
                                                   op0=ALU.mult), reads=[tag + "vT", "KC"], writes=[(tag + "vTm", e_)])
        for j in range(4):
            blk = slice(j * 128, (j + 1) * 128)
            for h in range(4):
                c2, e_ = h // 2, h % 2
                P.pe(lambda e, j=j, h=h, c2=c2, e_=e_, blk=blk: e.matmul(
                    pS[j][:, h * 128:(h + 1) * 128], lhsT=ktl[:, c2, blk], rhs=qtm[e_][:, c2, blk], start=True, stop=True),
                    reads=[tag + "ktl", (tag + "qtm", e_)], writes=[(tag + "pS", j)])
            P.dve(lambda e, j=j: e.tensor_tensor(out=Asb[j][:], in0=pS[j][:].rearrange("p (h t) -> p h t", h=4),
                                                 in1=tri2.rearrange("p (o t) -> p o t", o=1).to_broadcast([128, 4, 128]),
                                                 op=ALU.mult),
                  reads=[(tag + "pS", j), "KC"], writes=[(tag + "Asb", j)])
        for j in range(4):
            blk = slice(j * 128, (j + 1) * 128)
            for h in range(4):
                c2, hp = h // 2, (h % 2) * 64
                P.pe(lambda e, j=j, h=h, c2=c2, hp=hp, blk=blk: e.matmul(
                    pO[c2][hp:hp + 64, blk], lhsT=vT[:, j, h * 64:(h + 1) * 64], rhs=Asb[j][:, h, :],
                    start=True, stop=False), reads=[tag + "vT", (tag + "Asb", j)], writes=[(tag + "pO", c2)])
            for par in range(2):
                c = 2 * j + par
                cols = slice(c * 64, c * 64 + 64)
                ub = nU % 2
                nU += 1
                for h in range(4):
                    c2, hp, e_ = h // 2, (h % 2) * 64, h % 2
                    P.pe(lambda e, c2=c2, hp=hp, e_=e_, cols=cols, par=par: e.matmul(
                        pO[c2][hp:hp + 64, cols], lhsT=Sbf[:, c2 * 64:(c2 + 1) * 64], rhs=qem[e_][:, c2, cols],
                        start=False, stop=(par == 1)), reads=[tag + "Sbf", (tag + "qem", e_)], writes=[(tag + "pO", c2)])
                for h in range(4):
                    c2, hp = h // 2, (h % 2) * 64
                    P.pe(lambda e, c2=c2, hp=hp, j=j, h=h, ub=ub, par=par: e.matmul(
                        pU[ub][hp:hp + 64, c2 * 64:(c2 + 1) * 64], lhsT=khT[:, j, h * 64:(h + 1) * 64],
                        rhs=vTm[par][:, j, h * 64:(h + 1) * 64], start=True, stop=True),
                        reads=[tag + "khT", (tag + "vTm", par)], writes=[(tag + "pU", ub)])
                for c2 in range(2):
                    P.dve(lambda e, c2=c2, ub=ub, c=c: e.scalar_tensor_tensor(
                        out=S[:, c2 * 64:(c2 + 1) * 64], in0=S[:, c2 * 64:(c2 + 1) * 64],
                        scalar=e3[:, c2, c * 64 + 63:c * 64 + 64], in1=pU[ub][:, c2 * 64:(c2 + 1) * 64],
                        op0=ALU.mult, op1=ALU.add), reads=[tag + "S", tag + "e3", (tag + "pU", ub)], writes=[tag + "S"])
                P.act(lambda e: e.copy(out=Sbf[:], in_=S[:]), reads=[tag + "S"], writes=[tag + "Sbf"])
        for c2 in range(2):
            P.act(lambda e, c2=c2: e.activation(out=osq[:, c2, :], in_=pO[c2][:], func=AF.Square),
                  reads=[(tag + "pO", c2)], writes=[(tag + "osq", c2)])
            P.pe(lambda e, c2=c2: e.matmul(pS[c2][:], lhsT=bd[:], rhs=osq[:, c2, :], start=True, stop=True),
                 reads=["KC", (tag + "osq", c2)], writes=[(tag + "pS", c2)])
            P.dve(lambda e, c2=c2: e.tensor_scalar(out=rs[:, c2, :], in0=pS[c2][:], scalar1=1.0 / 64, scalar2=1e-6,
                                                   op0=ALU.mult, op1=ALU.add), reads=[(tag + "pS", c2)], writes=[(tag + "rs", c2)])
            P.act(lambda e, c2=c2: e.activation(out=rs[:, c2, :], in_=rs[:, c2, :], func=AF.Sqrt),
                  reads=[(tag + "rs", c2)], writes=[(tag + "rs", c2)])
            P.dve(lambda e, c2=c2: e.reciprocal(out=rs[:, c2, :], in_=rs[:, c2, :]), reads=[(tag + "rs", c2)],
                  writes=[(tag + "rs", c2)])
            P.dve(lambda e, c2=c2: e.scalar_tensor_tensor(out=rs[:, c2, :], in0=rs[:, c2, :], scalar=vcol(V, "b_norm_g", c2),
                                                          in1=pO[c2][:], op0=ALU.mult, op1=ALU.mult),
                  reads=[(tag + "rs", c2), (tag + "pO", c2), "V"], writes=[(tag + "rs", c2)])
            P.dve(lambda e, c2=c2, b=b, gb_=gb_: e.tensor_tensor(out=yb[b][:, c2, :], in0=rs[:, c2, :], in1=gb_[:, c2, :],
                                                                 op=ALU.mult),
                  reads=[(tag + "rs", c2), kg], writes=[(tag + "yb", b)])
        P.dma(yv[:, 2:4, ts], yb[b][:], reads=[(tag + "yb", b)], writes=[("ycatB", it)])
        yield


def make_kc_host():
    ident = np.eye(128, dtype=np.float32)
    bd = np.kron(np.eye(2, dtype=np.float32), np.ones((64, 64), np.float32))
    s = np.arange(128) % 64
    tri = (s[:, None] <= np.arange(64)[None, :]).astype(np.float32)
    rmask = np.ones((128, T), np.float32)
    rmask[:, ::64] = 0.0
    rm = np.zeros((128, 2), np.float32)
    rm[0:64, 0] = 1.0
    rm[64:128, 1] = 1.0
    pp = np.arange(128)
    tri2 = ((pp[:, None] // 64 == pp[None, :] // 64) & (pp[:, None] % 64 <= pp[None, :] % 64)).astype(np.float32)
    return np.ascontiguousarray(np.concatenate([ident, bd, tri, rmask, rm, tri2], axis=1))


def load_kc(nc, P, es, d_kc):
    raw = sb(nc, es, "kc_raw", [128, 962], F32)
    ident = sb(nc, es, "kc_ident", [128, 128], BF16)
    P.dma(raw[:], d_kc[:, :], writes=["KC"])
    P.dve(lambda e: e.tensor_copy(out=ident[:], in_=raw[:, 0:128]), reads=["KC"], writes=["KC"])
    return {"ident": ident, "bd": raw[:, 128:256], "tri": raw[:, 256:320], "rmask": raw[:, 320:832], "rm": raw[:, 832:834],
            "tri2": raw[:, 834:962], "raw": raw}


def make_kc2_host():
    j = np.arange(128)
    su = (j[:, None] > j[None, :]).astype(np.float32)
    lt = (j[:, None] <= j[None, :]).astype(np.float32)
    negi = (-30000.0 * np.eye(128)).astype(np.float32)
    ones = np.ones((128, 128), np.float32)
    return np.ascontiguousarray(np.concatenate([su, lt, su, negi, ones, np.eye(128, dtype=np.float32)], axis=1))


class _Stop(Exception):
    pass


def _ck(k):
    import os
    if os.environ.get("CSTOP") == str(k):
        raise _Stop()


def emit_C(nc, P, es, projT, V, KC, KC2, ycatT, ntok, tag="C"):
    nt = ntok // T
    ident = KC["ident"]
    SU, LT, GT, NEGI, ONES, IDF = (KC2[:, i * 128:(i + 1) * 128] for i in range(6))
    xb = sb(nc, es, tag + "xb", [128, 6, 4 + T], F32)
    xc = sb(nc, es, tag + "xc", [128, 6, T], F32)
    bcb = sb(nc, es, tag + "bcb", [128, 6, T], BF16)
    z = [sb(nc, es, tag + f"z{i}", [128, 2, T], F32) for i in range(2)]
    dta = sb(nc, es, tag + "dta", [128, T], F32)
    negA = sb(nc, es, tag + "negA", [128, 1], F32)
    atm = sb(nc, es, tag + "atm", [128, 16], F32)
    d2a = sb(nc, es, tag + "d2a", [128, 4, 4], F32)
    datm = sb(nc, es, tag + "datm", [128, 4, 8], F32)
    Lh = sb(nc, es, tag + "Lh", [128, 4, 128], F32)
    AB = sb(nc, es, tag + "AB", [128, 4, 128], F32)
    Lm = sb(nc, es, tag + "Lm", [128, 4, 128], F32)
    Sc = sb(nc, es, tag + "Sc", [128, 4, 128], BF16)
    DEC = sb(nc, es, tag + "DEC", [128, 2, T], F32)
    dl = sb(nc, es, tag + "dl", [128, 4], F32)
    d2 = sb(nc, es, tag + "d2", [128, 4], F32)
    xdtf = sb(nc, es, tag + "xdtf", [128, 256], F32)
    xdtb = sb(nc, es, tag + "xdtb", [128, 256], BF16)
    wxb = sb(nc, es, tag + "wxb", [128, 256], BF16)
    Btm = sb(nc, es, tag + "Btm", [128, 256], BF16)
    st = sb(nc, es, tag + "st", [128, 4, 64], F32)
    stb = sb(nc, es, tag + "stb", [128, 4, 64], BF16)
    yt = sb(nc, es, tag + "yt", [128, 2, T], F32)
    ysq = sb(nc, es, tag + "ysq", [128, 2, T], F32)
    rs = sb(nc, es, tag + "rs", [128, 2, T], F32)
    yc = [sb(nc, es, tag + f"yc{i}", [128, 2, T], BF16) for i in range(2)]
    p_da = psb(P, tag + "p_da")
    p_seg = psb(P, tag + "p_seg")
    p_acs = psb(P, tag + "p_acs")
    p_m1 = psb(P, tag + "p_m1")
    p_m2 = psb(P, tag + "p_m2")
    p_y = [psb(P, tag + f"p_y{i}") for i in range(2)]
    p_ss = psb(P, tag + "p_ss")

    P.pool(lambda e: e.memset(xb[:, :, 0:4], 0.0), writes=[tag + "xb"])
    P.pool(lambda e: e.memset(st[:], 0.0), writes=[tag + "st"])
    P.pool(lambda e: e.memset(stb[:], 0.0), writes=[tag + "stb"])
    P.pool(lambda e: e.memset(dta[:], 0.0), writes=[tag + "dta"])
    P.act(lambda e: e.activation(out=negA[:], in_=vcol(V, "c_a_log", rows=128), func=AF.Exp), reads=["V"], writes=[tag + "negA"])
    P.dve(lambda e: e.tensor_scalar(out=negA[:], in0=negA[:], scalar1=-1.0, scalar2=None, op0=ALU.mult),
          reads=[tag + "negA"], writes=[tag + "negA"])
    pv = projT.rearrange("(c p) t -> p c t", p=128)
    yv = ycatT.rearrange("(c p) t -> p c t", p=128)
    ny = 0
    for it in range(nt):
        ts = slice(it * T, (it + 1) * T)
        b = it % 2
        P.dma(xb[:, :, 4:4 + T], pv[:, 14:20, ts], reads=[("projT", it)], writes=[tag + "xb"])
        P.dma(z[b][:], pv[:, 20:22, ts], reads=[("projT", it)], writes=[(tag + "z", b)])
        P.dma(dta[0:4, :], projT[R_DT:R_DT + 4, ts], reads=[("projT", it)], writes=[tag + "dta"])
        P.dma(dta[64:68, :], projT[R_DT:R_DT + 4, ts], reads=[("projT", it)], writes=[tag + "dta"])
        for c in range(6):
            eng = P.dve
            for j in range(4):
                src = xb[:, c, 1 + j:1 + j + T]
                wj = vcol(V, "c_conv_w", c * 4 + j)
                if j == 0:
                    eng(lambda e, c=c, src=src, wj=wj: e.tensor_scalar(out=xc[:, c, :], in0=src, scalar1=wj,
                                                                      scalar2=vcol(V, "c_conv_b", c), op0=ALU.mult, op1=ALU.add),
                        reads=[tag + "xb", "V"], writes=[(tag + "xc", c)])
                else:
                    eng(lambda e, c=c, src=src, wj=wj: e.scalar_tensor_tensor(out=xc[:, c, :], in0=src, scalar=wj,
                                                                             in1=xc[:, c, :], op0=ALU.mult, op1=ALU.add),
                        reads=[tag + "xb", "V", (tag + "xc", c)], writes=[(tag + "xc", c)])
        allxc = [(tag + "xc", c) for c in range(6)]
        P.act(lambda e: e.copy(out=xb[:, :, 0:4], in_=xb[:, :, T:T + 4]), reads=[tag + "xb"] + allxc, writes=[tag + "xb"])
        P.act(lambda e: e.activation(out=xc[:], in_=xc[:], func=AF.Silu), reads=allxc, writes=allxc)
        P.dve(lambda e: e.tensor_copy(out=bcb[:], in_=xc[:]), reads=allxc, writes=[tag + "bcb"])
        P.act(lambda e, b=b: e.activation(out=z[b][:], in_=z[b][:], func=AF.Silu), reads=[(tag + "z", b)], writes=[(tag + "z", b)])
        _ck(1)
        P.act(lambda e: e.activation(out=dta[:], in_=dta[:], func=AF.Exp, bias=vcol(V, "c_dt_bias", rows=128)),
              reads=[tag + "dta", "V"], writes=[tag + "dta"])
        P.dve(lambda e: e.tensor_scalar(out=dta[:], in0=dta[:], scalar1=1.0, scalar2=None, op0=ALU.add),
              reads=[tag + "dta"], writes=[tag + "dta"])
        P.act(lambda e: e.activation(out=dta[:], in_=dta[:], func=AF.Ln), reads=[tag + "dta"], writes=[tag + "dta"])
        P.dve(lambda e: e.tensor_scalar(out=dta[64:128, :], in0=dta[64:128, :], scalar1=negA[64:128, 0:1], scalar2=None,
                                        op0=ALU.mult), reads=[tag + "dta", tag + "negA"], writes=[tag + "dta"])
        for j in range(4):
            P.pe(lambda e, j=j: e.matmul(p_da[:, j * 128:(j + 1) * 128], lhsT=dta[:, j * 128:(j + 1) * 128], rhs=IDF,
                                         start=True, stop=True), reads=[tag + "dta", "KC2"], writes=[tag + "p_da"])
        for j in range(4):
            P.dve(lambda e, j=j: e.tensor_copy(out=datm[:, j, 0:4], in_=p_da[:, j * 128:j * 128 + 4]),
                  reads=[tag + "p_da"], writes=[tag + "datm"])
            P.dve(lambda e, j=j: e.tensor_copy(out=datm[:, j, 4:8], in_=p_da[:, j * 128 + 64:j * 128 + 68]),
                  reads=[tag + "p_da"], writes=[tag + "datm"])
            P.dve(lambda e, j=j: e.tensor_copy(out=atm[:, j * 4:j * 4 + 4], in_=p_da[:, j * 128 + 64:j * 128 + 68]),
                  reads=[tag + "p_da"], writes=[tag + "atm"])
        P.pe(lambda e: e.matmul(p_da[:, 0:16], lhsT=GT, rhs=atm[:], start=True, stop=True),
             reads=["KC2", tag + "atm"], writes=[tag + "p_da"])
        P.act(lambda e: e.activation(out=d2a[:].rearrange("p j h -> p (j h)"), in_=p_da[:, 0:16], func=AF.Exp),
              reads=[tag + "p_da"], writes=[tag + "d2a"])
        _ck(2)
        for j in range(4):
            blk = slice(j * 128, (j + 1) * 128)
            yb_ = ny % 2
            ny += 1
            py = p_y[yb_]
            kpy = (tag + "p_y", yb_)
            for h in range(4):
                asc = datm[:, j, 4 + h:5 + h]
                P.dve(lambda e, h=h, asc=asc: e.tensor_scalar(out=Lh[:, h, :], in0=SU, scalar1=asc, scalar2=None, op0=ALU.mult),
                      reads=[tag + "datm", "KC2"], writes=[(tag + "Lh", h)])
                P.dve(lambda e, h=h, asc=asc: e.tensor_scalar(out=AB[:, h, :], in0=ONES, scalar1=asc, scalar2=None, op0=ALU.mult),
                       reads=[tag + "datm", "KC2"], writes=[(tag + "AB", h)])
            for h in range(4):
                P.pe(lambda e, h=h: e.matmul(p_seg[:, h * 128:(h + 1) * 128], lhsT=Lh[:, h, :], rhs=LT, start=True, stop=False),
                     reads=[(tag + "Lh", h), "KC2"], writes=[tag + "p_seg"])
                P.pe(lambda e, h=h: e.matmul(p_seg[:, h * 128:(h + 1) * 128], lhsT=NEGI, rhs=GT, start=False, stop=True),
                     reads=["KC2"], writes=[tag + "p_seg"])
            for h in range(4):
                P.pe(lambda e, h=h: e.matmul(p_acs[:, h * 128:(h + 1) * 128], lhsT=AB[:, h, :], rhs=LT, start=True, stop=True),
                     reads=[(tag + "AB", h), "KC2"], writes=[tag + "p_acs"])
            P.act(lambda e: e.activation(out=Lm[:].rearrange("p h l -> p (h l)"), in_=p_seg[:], func=AF.Exp),
                  reads=[tag + "p_seg"], writes=[tag + "Lm"])
            for h in range(4):
                P.act(lambda e, h=h: e.activation(out=dl[:, h:h + 1], in_=p_acs[:, h * 128 + 127:h * 128 + 128], func=AF.Exp),
                      reads=[tag + "p_acs"], writes=[tag + "dl"])
            for h in range(4):
                g_, hp = h // 2, (h % 2) * 64
                P.act(lambda e, h=h, g_=g_, hp=hp, blk=blk: e.activation(out=DEC[hp:hp + 64, g_, blk],
                                                                        in_=p_acs[hp:hp + 64, h * 128:(h + 1) * 128], func=AF.Exp),
                      reads=[tag + "p_acs"], writes=[tag + "DEC"])
            _ck(3)
            for g_ in range(2):
                P.pe(lambda e, g_=g_, blk=blk: e.matmul(p_m1[:, g_ * 128:(g_ + 1) * 128], lhsT=bcb[:, g_, blk], rhs=ident[:],
                                                         start=True, stop=True), reads=[tag + "bcb", "KC"], writes=[tag + "p_m1"])
                P.pe(lambda e, g_=g_, blk=blk: e.matmul(p_m1[:, 256 + g_ * 128:256 + (g_ + 1) * 128], lhsT=bcb[:, 2 + g_, blk],
                                                         rhs=ident[:], start=True, stop=True), reads=[tag + "bcb", "KC"], writes=[tag + "p_m1"])
            for h in range(4):
                P.dve(lambda e, h=h, j=j: e.tensor_scalar(out=xdtf[:, h * 64:(h + 1) * 64], in0=p_m1[:, h * 64:(h + 1) * 64],
                                                          scalar1=datm[:, j, h:h + 1], scalar2=None, op0=ALU.mult),
                      reads=[tag + "p_m1", tag + "datm"], writes=[tag + "xdtf"])
            P.act(lambda e: e.copy(out=Btm[:], in_=p_m1[:, 256:512]), reads=[tag + "p_m1"], writes=[tag + "Btm"])
            P.act(lambda e: e.copy(out=xdtb[:], in_=xdtf[:]), reads=[tag + "xdtf"], writes=[tag + "xdtb"])
            for h in range(4):
                P.dve(lambda e, h=h, j=j: e.tensor_scalar(out=wxb[:, h * 64:(h + 1) * 64], in0=xdtf[:, h * 64:(h + 1) * 64],
                                                          scalar1=d2a[:, j, h:h + 1], scalar2=None, op0=ALU.mult),
                      reads=[tag + "xdtf", tag + "d2a"], writes=[tag + "wxb"])
            _ck(4)
            for g_ in range(2):
                P.pe(lambda e, g_=g_, blk=blk: e.matmul(p_m2[:, g_ * 128:(g_ + 1) * 128], lhsT=bcb[:, 2 + g_, blk],
                                                         rhs=bcb[:, 4 + g_, blk], start=True, stop=True),
                     reads=[tag + "bcb"], writes=[tag + "p_m2"])
            _ck(40)
            for h in range(4):
                g_ = h // 2
                P.dve(lambda e, h=h, g_=g_: e.tensor_tensor(out=Sc[:, h, :], in0=p_m2[:, g_ * 128:(g_ + 1) * 128], in1=Lm[:, h, :],
                                                            op=ALU.mult), reads=[tag + "p_m2", tag + "Lm"], writes=[(tag + "Sc", h)])
            _ck(41)
            for h in range(4):
                g_, hp = h // 2, (h % 2) * 64
                P.pe(lambda e, h=h, g_=g_, hp=hp, py=py: e.matmul(py[hp:hp + 64, g_ * 128:(g_ + 1) * 128],
                                                                  lhsT=xdtb[:, h * 64:(h + 1) * 64], rhs=Sc[:, h, :],
                                                                  start=True, stop=True),
                     reads=[tag + "xdtb", (tag + "Sc", h)], writes=[kpy])
                P.pe(lambda e, h=h, g_=g_, hp=hp, py=py, blk=blk: e.matmul(py[hp:hp + 64, 256 + g_ * 128:256 + (g_ + 1) * 128],
                                                                           lhsT=stb[:, h, :], rhs=bcb[:, 4 + g_, blk],
                                                                           start=True, stop=True),
                     reads=[tag + "stb", tag + "bcb"], writes=[kpy])
            _ck(42)
            for h in range(4):
                g_ = h // 2
                P.pe(lambda e, h=h, g_=g_: e.matmul(p_m2[:, 256 + h * 64:256 + (h + 1) * 64], lhsT=Btm[:, g_ * 128:(g_ + 1) * 128],
                                                    rhs=wxb[:, h * 64:(h + 1) * 64], start=True, stop=True),
                     reads=[tag + "Btm", tag + "wxb"], writes=[tag + "p_m2"])
            _ck(43)
            for h in range(4):
                P.dve(lambda e, h=h: e.scalar_tensor_tensor(out=st[:, h, :], in0=st[:, h, :], scalar=dl[:, h:h + 1],
                                                            in1=p_m2[:, 256 + h * 64:256 + (h + 1) * 64], op0=ALU.mult, op1=ALU.add),
                      reads=[tag + "st", tag + "dl", tag + "p_m2"], writes=[tag + "st"])
            P.act(lambda e: e.copy(out=stb[:], in_=st[:]), reads=[tag + "st"], writes=[tag + "stb"])
            _ck(5)
            yt3 = yt[:, :, blk]
            P.dve(lambda e, py=py, blk=blk, yt3=yt3: e.tensor_tensor(out=yt3, in0=py[:, 256:512].rearrange("p (g l) -> p g l", g=2),
                                                                     in1=DEC[:, :, blk], op=ALU.mult),
                  reads=[kpy, tag + "DEC"], writes=[tag + "yt"])
            P.dve(lambda e, py=py, yt3=yt3: e.tensor_tensor(out=yt3, in0=py[:, 0:256].rearrange("p (g l) -> p g l", g=2),
                                                            in1=yt3, op=ALU.add), reads=[kpy, tag + "yt"], writes=[tag + "yt"])
        _ck(6)
        for g_ in range(2):
            P.dve(lambda e, g_=g_: e.scalar_tensor_tensor(out=yt[:, g_, :], in0=xc[:, g_, :], scalar=vcol(V, "c_d", g_),
                                                          in1=yt[:, g_, :], op0=ALU.mult, op1=ALU.add),
                  reads=[(tag + "xc", g_), "V", tag + "yt"], writes=[tag + "yt"])
        P.dve(lambda e, b=b: e.tensor_tensor(out=yt[:], in0=yt[:], in1=z[b][:], op=ALU.mult),
              reads=[tag + "yt", (tag + "z", b)], writes=[tag + "yt"])
        P.act(lambda e: e.activation(out=ysq[:], in_=yt[:], func=AF.Square), reads=[tag + "yt"], writes=[tag + "ysq"])
        for g_ in range(2):
            P.pe(lambda e, g_=g_: e.matmul(p_ss[:], lhsT=ONES, rhs=ysq[:, g_, :], start=True, stop=True),
                 reads=["KC2", tag + "ysq"], writes=[tag + "p_ss"])
            P.dve(lambda e, g_=g_: e.tensor_scalar(out=rs[:, g_, :], in0=p_ss[:], scalar1=1.0 / 128, scalar2=1e-6,
                                                   op0=ALU.mult, op1=ALU.add), reads=[tag + "p_ss"], writes=[(tag + "rs", g_)])
            P.act(lambda e, g_=g_: e.activation(out=rs[:, g_, :], in_=rs[:, g_, :], func=AF.Sqrt), reads=[(tag + "rs", g_)],
                  writes=[(tag + "rs", g_)])
            P.dve(lambda e, g_=g_: e.reciprocal(out=rs[:, g_, :], in_=rs[:, g_, :]), reads=[(tag + "rs", g_)], writes=[(tag + "rs", g_)])
            P.dve(lambda e, g_=g_, b=b: e.scalar_tensor_tensor(out=yc[b][:, g_, :], in0=yt[:, g_, :], scalar=vcol(V, "c_norm_g", g_),
                                                               in1=rs[:, g_, :], op0=ALU.mult, op1=ALU.mult),
                  reads=[tag + "yt", (tag + "rs", g_), "V"], writes=[(tag + "yc", b)])
        P.dma(yv[:, 4:6, ts], yc[b][:], reads=[(tag + "yc", b)], writes=[("ycatC", it)])
        yield


def build_full(ntok=SEQ, depth=DEPTH):
    nc = bass.Bass("TRN2", target_bir_lowering=False)
    di = lambda name, shape, dt=F32: nc.dram_tensor(name, list(shape), dt, kind="ExternalInput").ap()
    dn = lambda name, shape, dt=F32: nc.dram_tensor(name, list(shape), dt, kind="Internal").ap()
    xT = di("xT", [D_MODEL, ntok])
    pos = di("pos", [1, ntok], I32)
    Vd = di("vecs", [depth, 128, NV])
    w_in = di("w_in", [depth, D_MODEL, NPROJ])
    w_out = di("w_out", [depth, D_MODEL, D_MODEL])
    pw = di("a_pw", [depth, 256, 256])
    qbw = di("qbw", [depth, 256, 768])
    kvbw = di("kvbw", [depth, 128, 512])
    cp_d = di("cp", [128, NCP])
    kc_d = di("kc", [128, 962])
    kc2_d = di("kc2", [128, 768])
    xTo = nc.dram_tensor("xTo", [D_MODEL, ntok], F32, kind="ExternalOutput").ap()
    projT = dn("projT", [NPROJ, ntok])
    ycatT = dn("ycatT", [D_MODEL, ntok], BF16)
    xbuf = [dn("xbuf0", [D_MODEL, ntok]), dn("xbuf1", [D_MODEL, ntok])]
    QT = dn("QT", [4, 96, ntok], BF16)
    KT = dn("KT", [4, 96, ntok], BF16)
    VD = dn("VD", [4, 128, ntok // 128, 65], BF16)
    OD = dn("OD", [4, 65, ntok])
    with ExitStack() as es:
        P = Prog(nc, es)
        Vs = [sb(nc, es, f"V_sb{l}", [128, NV], F32) for l in range(depth)]
        CP = sb(nc, es, "CP_sb", [128, NCP], F32)
        KC2 = sb(nc, es, "KC2_sb", [128, 768], F32)
        LB = sb(nc, es, "LB_sb", [128, 3, 2], F32)
        for l in range(depth):
            P.dma(Vs[l][:], Vd[l], writes=["V"])
        P.dma(CP[:], cp_d[:, :], writes=["CP"])
        P.dma(KC2[:], kc2_d[:, :], writes=["KC2"])
        KC = load_kc(nc, P, es, kc_d)
        P.phase_end()
        for l in range(depth):
            V = Vs[l]
            x_in = xT if l == 0 else xbuf[(l - 1) % 2]
            x_out = xTo if l == depth - 1 else xbuf[l % 2]
            with ExitStack() as pes:
                emit_lb(nc, P, pes, V, LB, l)
                emit_l1(nc, P, pes, x_in, w_in[l], V, projT, ntok=ntok)
                P.phase_end()
            with ExitStack() as pes:
                emit_A(nc, P, pes, projT, V, pw[l], ycatT, ntok)
                P.phase_end()
            with ExitStack() as pes:
                gB = emit_B(nc, P, pes, projT, V, LB, KC, ycatT, ntok)
                gC = emit_C(nc, P, pes, projT, V, KC, KC2, ycatT, ntok)
                doneB = doneC = False
                while not (doneB and doneC):
                    if not doneB:
                        doneB = next(gB, "end") == "end"
                    if not doneC:
                        doneC = next(gC, "end") == "end"
                P.phase_end()
            with ExitStack() as pes:
                emit_D(nc, P, pes, projT, pos, V, CP, qbw[l], kvbw[l], QT, KT, VD, OD, ycatT, ntok)
                P.phase_end()
            with ExitStack() as pes:
                emit_O(nc, P, pes, ycatT, x_in, V, w_out[l], x_out, ntok)
                P.phase_end()
    return nc


def make_in_map(x_b, pos_b, p, depth=DEPTH):
    return {
        "xT": np.ascontiguousarray(np.asarray(x_b, np.float32).T),
        "pos": np.ascontiguousarray(np.asarray(pos_b, np.int32).reshape(1, -1)),
        "vecs": np.stack([pack_vecs(p, l) for l in range(depth)]),
        "w_in": np.stack([pack_w_in(np.asarray(p["w_in"][l], np.float32)) for l in range(depth)]),
        "w_out": np.ascontiguousarray(np.asarray(p["w_out"], np.float32)[:depth]),
        "a_pw": np.ascontiguousarray(np.asarray(p["a_pw_w"], np.float32)[:depth]),
        "qbw": np.stack([pack_qb(p["d_qb_w"][l]).reshape(256, 768) for l in range(depth)]),
        "kvbw": np.stack([pack_kvb(p["d_kvb_w"][l]).reshape(128, 512) for l in range(depth)]),
        "cp": make_cp(), "kc": make_kc_host(), "kc2": make_kc2_host(),
    }


def kernel(**inputs):
    x = np.asarray(inputs["x"], np.float32)
    positions = np.asarray(inputs["positions"], np.int32)
    p = {k: np.asarray(v) for k, v in inputs.items() if k not in ("x", "positions")}
    nb, ntok, _ = x.shape
    nc = build_full(ntok, DEPTH)
    in_maps = [make_in_map(x[b], positions[b], p) for b in range(nb)]
    res = run_bass_kernel_spmd(nc, in_maps, core_ids=list(range(nb)))
    out = np.stack([np.ascontiguousarray(res.results[b]["xTo"].T) for b in range(nb)])
    return out.astype(np.float32)
```
